# Optimizing a Trainium2 kernel written in Bass

```python
import math
import jax
import jax.numpy as jnp
from jax import lax
import numpy as np

D_MODEL = 1024
BATCH = 8
SEQ = 4096
DEPTH = 4

SSD_HEADS = 16
SSD_HEAD_DIM = 64
SSD_INNER = SSD_HEADS * SSD_HEAD_DIM
SSD_GROUPS = 2
SSD_STATE = 128
SSD_CONV_DIM = SSD_INNER + 2 * SSD_GROUPS * SSD_STATE
CONV_WIDTH = 4
SSD_CHUNK = 128
ATT_HEADS = 8
ATT_KV_HEADS = 2
ATT_HEAD_DIM = 64
ATT_WIDTH = ATT_HEADS * ATT_HEAD_DIM
IDX_HEADS = 4
IDX_HEAD_DIM = 64
TOPK_MAX = 256
Q_BLOCK = 128
POOL_WINDOWS = (2, 4, 8, 16)
POOL_GROUPS = 4
POOL_GROUP_DIM = 128
POOL_WIDTH = POOL_GROUPS * POOL_GROUP_DIM
N_BRANCH = 3
FFN_HIDDEN = ((8 * D_MODEL // 3 + 255) // 256) * 256
EPS = 1e-6
IN_SIZES = (SSD_INNER, SSD_CONV_DIM, SSD_HEADS, ATT_WIDTH, ATT_KV_HEADS * ATT_HEAD_DIM, ATT_KV_HEADS * ATT_HEAD_DIM, IDX_HEADS * IDX_HEAD_DIM, IDX_HEAD_DIM, IDX_HEADS, POOL_WIDTH, N_BRANCH * D_MODEL)
IN_WIDTH = SSD_INNER + SSD_CONV_DIM + SSD_HEADS + ATT_WIDTH + 2 * ATT_KV_HEADS * ATT_HEAD_DIM + IDX_HEADS * IDX_HEAD_DIM + IDX_HEAD_DIM + IDX_HEADS + POOL_WIDTH + N_BRANCH * D_MODEL

kernel_name = "hybrid_ssd_dsa_pool_gated_block"


def _rmsnorm(x, w):
    xf = x.astype(jnp.float32)
    y = xf * lax.rsqrt(jnp.mean(xf * xf, axis=-1, keepdims=True) + EPS)
    return (y * w.astype(jnp.float32)).astype(x.dtype)


def _split_proj(p):
    offs, acc = [], 0
    for s in IN_SIZES[:-1]:
        acc += s
        offs.append(acc)
    return jnp.split(p, offs, axis=-1)


def _causal_dwconv(u, w, b):
    out = lax.conv_general_dilated(u, w[:, None, :].astype(u.dtype), window_strides=(1,), padding=[(CONV_WIDTH - 1, 0)], dimension_numbers=('NWC', 'WIO', 'NWC'), feature_group_count=u.shape[-1])
    return out + b.astype(u.dtype)


def _ssd_scan(xh, dt, A, Bm, Cm):
    out_dtype = xh.dtype
    b, T, g, j, p = xh.shape
    n = Bm.shape[-1]
    c = T // SSD_CHUNK
    f32 = jnp.float32
    x = (xh.astype(f32) * dt.astype(f32)[..., None]).reshape(b, c, SSD_CHUNK, g, j, p)
    a = (dt.astype(f32) * A.astype(f32)).reshape(b, c, SSD_CHUNK, g, j)
    Bc = Bm.astype(f32).reshape(b, c, SSD_CHUNK, g, n)
    Cc = Cm.astype(f32).reshape(b, c, SSD_CHUNK, g, n)
    a_cs = jnp.cumsum(a, axis=2)
    causal = jnp.tril(jnp.ones((SSD_CHUNK, SSD_CHUNK), dtype=bool))[None, None, :, :, None, None]
    seg = a_cs[:, :, :, None] - a_cs[:, :, None, :]
    lmat = jnp.exp(jnp.where(causal, seg, -jnp.inf))
    cb = jnp.einsum('bclgn,bcsgn->bclsg', Cc, Bc)
    y_diag = jnp.einsum('bclsg,bclsgj,bcsgjp->bclgjp', cb, lmat, x)
    decay_states = jnp.exp(a_cs[:, :, -1:] - a_cs)
    states = jnp.einsum('bclgn,bclgj,bclgjp->bcgjpn', Bc, decay_states, x)
    chunk_decay = jnp.exp(a_cs[:, :, -1])

    def step(h, inp):
        s_c, d_c = inp
        return h * d_c[..., None, None] + s_c, h

    h0 = jnp.zeros((b, g, j, p, n), f32)
    _, prev = lax.scan(step, h0, (jnp.swapaxes(states, 0, 1), jnp.swapaxes(chunk_decay, 0, 1)))
    prev = jnp.swapaxes(prev, 0, 1)
    y_off = jnp.einsum('bclgn,bcgjpn,bclgj->bclgjp', Cc, prev, jnp.exp(a_cs))
    return (y_diag + y_off).reshape(b, T, g, j, p).astype(out_dtype)


def _dsa_attention(q, k, v, qi, ki, wi):
    b, T = q.shape[0], q.shape[1]
    topk = min(TOPK_MAX, T // 4)
    nb = T // Q_BLOCK
    att_scale = ATT_HEAD_DIM ** -0.5
    idx_scale = (IDX_HEADS ** -0.5) * (IDX_HEAD_DIM ** -0.5)
    key_pos = jnp.arange(T)
    ki32 = ki.astype(jnp.float32)

    def blockify(a):
        return jnp.swapaxes(a.reshape((b, nb, Q_BLOCK) + a.shape[2:]), 0, 1)

    def one_block(args):
        qb, qib, wib, pos = args
        s = jnp.einsum('bqhd,bsd->bqhs', qib.astype(jnp.float32), ki32)
        score = jnp.einsum('bqhs,bqh->bqs', jax.nn.relu(s), wib.astype(jnp.float32) * idx_scale)
        admissible = key_pos[None, None, :] <= pos[None, :, None]
        score = jnp.where(admissible, score, -jnp.inf)
        _, idx = lax.top_k(score, topk)
        valid = idx <= pos[None, :, None]
        ksel = jax.vmap(lambda kb, ib: kb[ib])(k, idx)
        vsel = jax.vmap(lambda vb, ib: vb[ib])(v, idx)
        logits = jnp.einsum('bqhgd,bqkhd->bqhgk', qb, ksel).astype(jnp.float32) * att_scale
        logits = jnp.where(valid[:, :, None, None, :], logits, -jnp.inf)
        prob = jax.nn.softmax(logits, axis=-1)
        return jnp.einsum('bqhgk,bqkhd->bqhgd', prob.astype(vsel.dtype), vsel)

    pos_blocks = jnp.arange(T).reshape(nb, Q_BLOCK)
    out = lax.map(one_block, (blockify(q), blockify(qi), blockify(wi), pos_blocks))
    return jnp.swapaxes(out, 0, 1).reshape(b, T, ATT_WIDTH)


def _pool_mixer(u, w, scale):
    b, T, _ = u.shape
    ug = u.reshape(b, T, POOL_GROUPS, POOL_GROUP_DIM).astype(jnp.float32)
    cs = jnp.cumsum(ug, axis=1)
    t1 = jnp.arange(1, T + 1, dtype=jnp.float32)
    outs = []
    for gi, win in enumerate(POOL_WINDOWS):
        c = cs[:, :, gi]
        lag = jnp.pad(c, ((0, 0), (win, 0), (0, 0)))[:, :T]
        cnt = jnp.minimum(t1, float(win))[None, :, None]
        outs.append((c - lag) / cnt - ug[:, :, gi])
    pooled = jnp.stack(outs, axis=2).astype(u.dtype)
    mixed = jnp.einsum('btgc,gcd->btgd', pooled, w)
    return mixed.reshape(b, T, POOL_WIDTH) * scale


def _layer(h, norm1_w, w_in, conv_w, conv_b, dt_bias, a_log, d_skip, ssd_norm_w, pool_w, pool_scale, w_up_ssd, w_up_attn, w_up_pool, w_out, norm2_w, w_ffn_in, w_ffn_out):
    b, T, _ = h.shape
    xn = _rmsnorm(h, norm1_w)
    proj = xn @ w_in
    z, xbc, dt_raw, q, k, v, qi, ki, wi, u, gates = _split_proj(proj)
    xbc = jax.nn.silu(_causal_dwconv(xbc, conv_w, conv_b))
    xs, Bm, Cm = jnp.split(xbc, [SSD_INNER, SSD_INNER + SSD_GROUPS * SSD_STATE], axis=-1)
    dt = jax.nn.softplus(dt_raw + dt_bias)
    A = -jnp.exp(a_log)
    hpg = SSD_HEADS // SSD_GROUPS
    xh = xs.reshape(b, T, SSD_GROUPS, hpg, SSD_HEAD_DIM)
    y = _ssd_scan(xh, dt.reshape(b, T, SSD_GROUPS, hpg), A.reshape(SSD_GROUPS, hpg), Bm.reshape(b, T, SSD_GROUPS, SSD_STATE), Cm.reshape(b, T, SSD_GROUPS, SSD_STATE))
    y = y + d_skip.reshape(SSD_GROUPS, hpg)[..., None] * xh
    y_ssd = _rmsnorm(y.reshape(b, T, SSD_INNER) * jax.nn.silu(z), ssd_norm_w)
    qh = q.reshape(b, T, ATT_KV_HEADS, ATT_HEADS // ATT_KV_HEADS, ATT_HEAD_DIM)
    kh = k.reshape(b, T, ATT_KV_HEADS, ATT_HEAD_DIM)
    vh = v.reshape(b, T, ATT_KV_HEADS, ATT_HEAD_DIM)
    y_att = _dsa_attention(qh, kh, vh, qi.reshape(b, T, IDX_HEADS, IDX_HEAD_DIM), ki, wi)
    y_pool = _pool_mixer(u, pool_w, pool_scale)
    g = jax.nn.sigmoid(gates.reshape(b, T, N_BRANCH, D_MODEL))
    merged = g[:, :, 0] * (y_ssd @ w_up_ssd) + g[:, :, 1] * (y_att @ w_up_attn) + g[:, :, 2] * (y_pool @ w_up_pool)
    h = h + merged @ w_out
    hn = _rmsnorm(h, norm2_w)
    a_part, b_part = jnp.split(hn @ w_ffn_in, 2, axis=-1)
    return h + (jax.nn.silu(a_part) * b_part) @ w_ffn_out


def setup_inputs(seed: int = 0) -> dict:
    key = jax.random.key(seed)
    ks = jax.random.split(key, 20)
    f32 = jnp.float32
    L = DEPTH

    def nrm(k, shape, fan_in):
        return jax.random.normal(k, shape, f32) * (fan_in ** -0.5)

    def gain(k, shape):
        return 1.0 + 0.02 * jax.random.normal(k, shape, f32)

    u_dt = jax.random.uniform(ks[5], (L, SSD_HEADS), f32)
    dt0 = jnp.exp(u_dt * (math.log(0.1) - math.log(0.001)) + math.log(0.001))
    dt_bias = dt0 + jnp.log(-jnp.expm1(-dt0))
    return {
        "x": jax.random.normal(ks[0], (BATCH, SEQ, D_MODEL), f32),
        "norm1_w": gain(ks[1], (L, D_MODEL)),
        "w_in": nrm(ks[2], (L, D_MODEL, IN_WIDTH), D_MODEL),
        "conv_w": nrm(ks[3], (L, CONV_WIDTH, SSD_CONV_DIM), CONV_WIDTH),
        "conv_b": 0.02 * jax.random.normal(ks[4], (L, SSD_CONV_DIM), f32),
        "dt_bias": dt_bias,
        "a_log": jnp.log(jax.random.uniform(ks[6], (L, SSD_HEADS), f32, minval=1.0, maxval=16.0)),
        "d_skip": gain(ks[7], (L, SSD_HEADS)),
        "ssd_norm_w": gain(ks[8], (L, SSD_INNER)),
        "pool_w": nrm(ks[9], (L, POOL_GROUPS, POOL_GROUP_DIM, POOL_GROUP_DIM), POOL_GROUP_DIM),
        "pool_scale": gain(ks[10], (L, POOL_WIDTH)),
        "w_up_ssd": nrm(ks[11], (L, SSD_INNER, D_MODEL), SSD_INNER),
        "w_up_attn": nrm(ks[12], (L, ATT_WIDTH, D_MODEL), ATT_WIDTH),
        "w_up_pool": nrm(ks[13], (L, POOL_WIDTH, D_MODEL), POOL_WIDTH),
        "w_out": nrm(ks[14], (L, D_MODEL, D_MODEL), D_MODEL),
        "norm2_w": gain(ks[15], (L, D_MODEL)),
        "w_ffn_in": nrm(ks[16], (L, D_MODEL, 2 * FFN_HIDDEN), D_MODEL),
        "w_ffn_out": nrm(ks[17], (L, FFN_HIDDEN, D_MODEL), FFN_HIDDEN),
        "final_norm_w": gain(ks[18], (D_MODEL,)),
    }


def reference(x, norm1_w, w_in, conv_w, conv_b, dt_bias, a_log, d_skip, ssd_norm_w, pool_w, pool_scale, w_up_ssd, w_up_attn, w_up_pool, w_out, norm2_w, w_ffn_in, w_ffn_out, final_norm_w):
    h = x
    for l in range(DEPTH):
        h = _layer(h, norm1_w[l], w_in[l], conv_w[l], conv_b[l], dt_bias[l], a_log[l], d_skip[l], ssd_norm_w[l], pool_w[l], pool_scale[l], w_up_ssd[l], w_up_attn[l], w_up_pool[l], w_out[l], norm2_w[l], w_ffn_in[l], w_ffn_out[l])
    return _rmsnorm(h, final_norm_w)
```

```python
import numpy as np
from contextlib import ExitStack
import concourse.bass as bass
import concourse.mybir as mybir
from concourse.bass_utils import run_bass_kernel_spmd

F32 = mybir.dt.float32
BF16 = mybir.dt.bfloat16
ALU = mybir.AluOpType
AF = mybir.ActivationFunctionType
AX = mybir.AxisListType

D = 1024
SEQ = 4096
DEPTH = 4
NCORES = 8
S = 512
NSLOT = 151
NPP = 88
EPS = 1e-6
SEM_LIMIT = 30000
NEG = -1.0e30
POOL_WINDOWS = (2, 4, 8, 16)


class Buf:
    __slots__ = ("w", "r")

    def __init__(self):
        self.w = None
        self.r = {}


class Ctr:
    def __init__(self, K, name, step):
        self.K, self.name, self.step = K, name, step
        self.n = 0
        self.h = K.new_sem(f"{name}_0")
        self.v = 0

    def next(self):
        if self.v + self.step > SEM_LIMIT:
            self.n += 1
            self.h = self.K.new_sem(f"{self.name}_{self.n}")
            self.v = 0
        self.v += self.step
        return (self.h, self.v)


class Eng:
    def __init__(self, K, name):
        self.name = name
        self.ops = []
        self.ctr = Ctr(K, "s_" + name, 1)
        self.seen = {}
        self.own = set()


class Kern:
    def __init__(self, nc, stack, n_dma_sems=12):
        self.nc = nc
        self.stack = stack
        self.nsem = 0
        self.E = {n: Eng(self, n) for n in ("pe", "act", "dve", "pool", "sp")}
        self.dma_ctrs = [Ctr(self, f"d{i}", 16) for i in range(n_dma_sems)]
        self.dma_rr = 0
        self.nops = 0

    def new_sem(self, name):
        self.nsem += 1
        return self.stack.enter_context(self.nc.semaphore(name))

    def _deps(self, e, r, w):
        deps = {}
        for b in r:
            if b.w is not None and deps.get(b.w[0], 0) < b.w[1]:
                deps[b.w[0]] = b.w[1]
        for b in w:
            if b.w is not None and deps.get(b.w[0], 0) < b.w[1]:
                deps[b.w[0]] = b.w[1]
            for h, v in b.r.items():
                if deps.get(h, 0) < v:
                    deps[h] = v
        waits = []
        for h, v in deps.items():
            if e.name == "pe" and h in e.own:
                continue
            if e.seen.get(h, 0) < v:
                waits.append((h, v))
                e.seen[h] = v
        return waits

    def _commit(self, tok, r, w):
        h, v = tok
        for b in w:
            b.w = tok
            b.r = {}
        for b in r:
            if b.w is not tok and b.r.get(h, 0) < v:
                b.r[h] = v

    def op(self, en, fn, r=(), w=()):
        e = self.E[en]
        waits = self._deps(e, r, w)
        tok = e.ctr.next()
        e.own.add(tok[0])
        e.ops.append((waits, fn, tok[0], 1))
        self._commit(tok, r, w)
        self.nops += 1

    def dma(self, en, out, in_, r=(), w=()):
        e = self.E[en]
        waits = self._deps(e, r, w)
        c = self.dma_ctrs[self.dma_rr]
        self.dma_rr = (self.dma_rr + 1) % len(self.dma_ctrs)
        if c.v > 0 and e.seen.get(c.h, 0) < c.v:
            waits.append((c.h, c.v))
            e.seen[c.h] = c.v
        tok = c.next()
        e.ops.append((waits, (lambda q: q.dma_start(out=out, in_=in_)), tok[0], 16))
        self._commit(tok, r, w)
        self.nops += 1

    def handoff(self, src, dst):
        u = {}
        for b in src:
            if b.w is not None and u.get(b.w[0], 0) < b.w[1]:
                u[b.w[0]] = b.w[1]
            for h, v in b.r.items():
                if u.get(h, 0) < v:
                    u[h] = v
        for b in dst:
            for h, v in u.items():
                if b.r.get(h, 0) < v:
                    b.r[h] = v

    def final_wait(self, en, bufs):
        e = self.E[en]
        waits = self._deps(e, bufs, ())
        e.ops.append((waits, None, None, 0))

    def emit(self):
        nc = self.nc
        E = self.E

        def run(q, e):
            for waits, fn, sem, inc in e.ops:
                for h, v in waits:
                    q.wait_ge(h, v)
                if fn is not None:
                    fn(q).then_inc(sem, inc)

        with nc.Block() as block:
            @block.tensor
            def _(q):
                run(q, E["pe"])

            @block.scalar
            def _(q):
                run(q, E["act"])

            @block.vector
            def _(q):
                run(q, E["dve"])

            @block.gpsimd
            def _(q):
                run(q, E["pool"])

            @block.sync
            def _(q):
                run(q, E["sp"])


def build(T=SEQ, L=DEPTH, dbg=False):
    NCH = T // 128
    NSC = T // S
    nc = bass.Bass("TRN2", target_bir_lowering=False)

    def din(name, shape):
        return nc.dram_tensor(name, shape, F32, kind="ExternalInput").ap()

    x_d = din("x", [T, D])
    W_d = din("W", [L, NSLOT, 128, 1024])
    pp_d = din("pp", [L, 128, NPP])
    rows_d = din("rows", [L, 128, 48])
    fnw_d = din("fnw", [128, D])
    cst_d = din("cst", [128, 7, 128])
    rc16_d = din("rc16", [128, 4, 16])
    y_d = nc.dram_tensor("y", [T, D], F32, kind="ExternalOutput").ap()
    hb_d = nc.dram_tensor("hbuf", [T, D], F32, kind="Internal").ap()

    with ExitStack() as st:
        K = Kern(nc, st)

        def sb(name, shape, dt=F32):
            return st.enter_context(nc.sbuf_tensor("sb_" + name, shape, dt))

        ps = st.enter_context(nc.psum_tensor("ps", [128, 8, 512], F32))
        PB = [Buf() for _ in range(8)]
        pbs = [0]

        def bank(excl=()):
            while True:
                i = pbs[0]
                pbs[0] = (i + 1) % 8
                if i not in excl:
                    return i

        def bank2(excl=()):
            while True:
                if pbs[0] % 2:
                    pbs[0] = (pbs[0] + 1) % 8
                i = pbs[0]
                pbs[0] = (i + 2) % 8
                if i not in excl and (i + 1) not in excl:
                    return i

        def bank4():
            i = 0 if pbs[0] <= 0 or pbs[0] > 4 else 4
            pbs[0] = (i + 4) % 8
            return i

        def psb(i):
            return ps[:, i, :].bitcast(BF16)

        B_dbg, B_dbgout = Buf(), Buf()
        dbgbuf = sb("dbgbuf", [128, 4096]) if dbg else None

        def dump(name, view, bufs, n):
            if not dbg:
                return
            K.op("pool", lambda q: q.tensor_copy(out=dbgbuf[:view.shape[0], 0:n], in_=view), r=bufs, w=[B_dbg])
            d = nc.dram_tensor("dbg_" + name, [view.shape[0], n], F32, kind="ExternalOutput").ap()
            K.dma("sp", d, dbgbuf[:view.shape[0], 0:n], r=[B_dbg], w=[B_dbgout])

        cst = sb("cst", [128, 7, 128])
        rc16 = sb("rc16", [128, 4, 16])
        identb = sb("identb", [128, 128], BF16)
        fnw = sb("fnw", [128, D])
        LHS = sb("LHS", [64, 16, 128])
        RHS = sb("RHS", [64, 128])
        Bc = Buf()
        identf = cst[:, 0, :]
        tri_le = cst[:, 1, :]
        tri_gt = cst[:, 2, :]
        onesf = cst[:, 3, :]
        maskneg = cst[:, 4, :]
        cmaskneg = cst[:, 5, :]
        oh = cst[:, 6, :]
        K.dma("sp", cst[:], cst_d, w=[Bc])
        K.dma("sp", rc16[:], rc16_d, w=[Bc])
        K.dma("sp", fnw[:], fnw_d, w=[Bc])
        K.op("dve", lambda q: q.tensor_copy(out=identb[:], in_=identf), r=[Bc], w=[Bc])
        B_LHS, B_RHS = Buf(), Buf()
        K.op("pool", lambda q: q.memset(LHS[:], 0.0), w=[B_LHS])
        K.op("pool", lambda q: q.memset(RHS[:], 0.0), w=[B_RHS])
        K.op("dve", lambda q: q.tensor_copy(out=LHS[0:16, :, :], in_=oh[0:16, 0:16].unsqueeze(2).to_broadcast([16, 16, 128])), r=[Bc], w=[B_LHS])
        K.op("pool", lambda q: q.memset(RHS[32:48, :], 1.0), w=[B_RHS])

        pp = sb("pp", [128, NPP])
        rows = sb("rows", [128, 48])
        A_b = sb("A_b", [128, 16])
        B_par = Buf()
        hS = sb("hS", [128, 4, D])
        B_hS = Buf()
        xn_tok = [sb(f"xn_tok{i}", [128, D], BF16) for i in range(2)]
        B_xn = [Buf(), Buf()]
        junk = sb("junk", [128, D], BF16)
        B_junk = Buf()
        ssq = sb("ssq", [128, 8])
        B_ssq = Buf()
        xnT = sb("xnT", [128, 8, S], BF16)
        B_xnT = Buf()
        qT = sb("qT", [128, 4, S], BF16)
        B_qT = Buf()
        kT = sb("kT", [128, T], BF16)
        B_kT = Buf()
        qiT = sb("qiT", [128, 2, S], BF16)
        B_qiT = Buf()
        kiT = sb("kiT", [128, T], BF16)
        B_kiT = Buf()
        Vh = sb("Vh", [128, NCH, 128], BF16)
        B_V = Buf()
        dw = sb("dw", [128, 4, 20])
        B_dw = Buf()
        state = sb("state", [128, 16, 64])
        state_bf = sb("state_bf", [128, 16, 64], BF16)
        B_state, B_statebf = Buf(), Buf()
        chalo = sb("chalo", [128, 12, 3], BF16)
        B_chalo = Buf()
        phalo = sb("phalo", [128, 4, 15])
        B_phalo = Buf()
        ynT = sb("ynT", [128, 8, S], BF16)
        B_ynT = Buf()
        yattT = sb("yattT", [128, 4, S], BF16)
        B_yattT = Buf()
        ypoolT = sb("ypoolT", [128, 4, S], BF16)
        B_ypoolT = Buf()
        NS, NB = 3, 3
        wst = [sb(f"wst{i}", [128, 8, 128]) for i in range(NS)]
        wbf = [sb(f"wbf{i}", [128, 8, 128], BF16) for i in range(NB)]
        B_wst = [Buf() for _ in range(NS)]
        B_wbf = [Buf() for _ in range(NB)]
        upre = [sb(f"upre{i}", [128, 515], BF16) for i in range(2)]
        B_upre = [Buf(), Buf()]
        ubuf = [sb(f"ubuf{i}", [128, 527]) for i in range(2)]
        B_ubuf = [Buf(), Buf()]
        ptmp = [sb(f"ptmp{i}", [128, 527]) for i in range(2)]
        B_ptmp = [Buf(), Buf()]
        pooled = [sb(f"pooled{i}", [128, S], BF16) for i in range(2)]
        B_pooled = [Buf(), Buf()]
        small = sb("small", [128, 256])
        B_sm = {n: Buf() for n in ("dt", "a48", "e3", "dtdec", "m8", "thr", "rs", "rsum", "rinv")}
        rsb = sb("rsb", [128, 8, 8])

        R1N = 15872
        R1 = sb("R1", [128, R1N])

        def carve(off_f32, nelem, dt):
            if dt == F32:
                return R1[:, off_f32:off_f32 + nelem]
            return R1[:, off_f32:off_f32 + nelem // 2].bitcast(BF16)

        o = 0
        xbc_act = carve(o, 12 * S, BF16).rearrange("p (j t) -> p j t", j=12); o += 6 * S
        zs = carve(o, 4 * D, BF16).rearrange("p (c d) -> p c d", c=4); o += 2 * D
        xs_tok = carve(o, 4 * D, BF16).rearrange("p (c d) -> p c d", c=4); o += 2 * D
        B_tok = carve(o, 4 * 256, BF16).rearrange("p (c d) -> p c d", c=4); o += 512
        ctmp = carve(o, S, F32); o += S
        x_dt = carve(o, D, BF16); o += D // 2
        xdec = carve(o, D, BF16); o += D // 2
        xsd = carve(o, D, BF16); o += D // 2
        Lexp = carve(o, D, BF16).rearrange("p (j t) -> p j t", j=8); o += D // 2
        Mt = carve(o, D, BF16).rearrange("p (j t) -> p j t", j=8); o += D // 2
        cb = carve(o, 128, F32); o += 128
        yacc = carve(o, D, F32); o += D
        ynb = carve(o, D, BF16); o += D // 2
        assert o <= R1N, o
        ssd_bufs = {n: Buf() for n in ("xbc_act", "zs", "xs_tok", "B_tok", "ctmp", "x_dt", "xdec", "xsd", "Lexp", "Mt", "cb", "yacc", "ynb")}
        o = 0
        score = carve(o, T, F32); o += T
        work = carve(o, T, F32); o += T
        rtmp = [carve(o + i * S, S, F32) for i in range(2)]; o += 2 * S
        Pb = [carve(o + i * (S // 2), S, BF16) for i in range(2)]; o += S
        Pm = [carve(o + i * (S // 2), S, BF16) for i in range(2)]; o += S
        PmT = [carve(o + i * (S // 2), S, BF16) for i in range(2)]; o += S
        yatt = carve(o, 512, BF16); o += 256
        assert o <= R1N, o
        att_bufs = {n: Buf() for n in ("score", "work", "rtmp0", "rtmp1", "Pb0", "Pb1", "Pm0", "Pm1", "PmT0", "PmT1", "yatt")}
        o = 0
        mergedT = carve(o, 8 * S, BF16).rearrange("p (j t) -> p j t", j=8); o += 4 * S
        sig = [carve(o + i * (S // 2), S, BF16) for i in range(3)]; o += 3 * (S // 2)
        mt = [carve(o + i * S, S, F32) for i in range(3)]; o += 3 * S
        assert o <= R1N, o
        mrg_bufs = {n: Buf() for n in ("mergedT", "sig0", "sig1", "sig2", "mt0", "mt1", "mt2")}
        o = 0
        hidT = carve(o, 22 * S, BF16).rearrange("p (j t) -> p j t", j=22); o += 11 * S
        sa = [carve(o + i * (S // 2), S, BF16) for i in range(2)]; o += S
        assert o <= R1N, o
        ffn_bufs = {n: Buf() for n in ("hidT", "sa0", "sa1")}

        wctr = [0]
        cur = {"l": 0, "slot": 0}

        def wtile():
            i = wctr[0]
            wctr[0] += 1
            l, slot = cur["l"], cur["slot"]
            cur["slot"] += 1
            s_, b_ = i % NS, i % NB
            K.dma("sp", wst[s_][:].rearrange("p k c -> p (k c)"), W_d[l, slot], w=[B_wst[s_]])
            K.op("pool", lambda q: q.tensor_copy(out=wbf[b_][:], in_=wst[s_][:]), r=[B_wst[s_]], w=[B_wbf[b_]])
            return wbf[b_], B_wbf[b_]

        def rms_T(nwoff, dstT, B_dst):
            for c in range(4):
                K.op("act", lambda q, c=c: q.activation(out=junk[:], in_=hS[:, c, :], func=AF.Square, accum_out=ssq[:, c:c + 1]),
                     r=[B_hS], w=[B_junk, B_ssq])
            K.op("act", lambda q: q.activation(out=ssq[:, 4:8], in_=ssq[:, 0:4], func=AF.Sqrt, scale=1.0 / D, bias=EPS), r=[B_ssq], w=[B_ssq])
            K.op("dve", lambda q: q.reciprocal(out=ssq[:, 4:8], in_=ssq[:, 4:8]), r=[B_ssq], w=[B_ssq])
            for c in range(4):
                xt, bx = xn_tok[c % 2], B_xn[c % 2]
                K.op("dve", lambda q, c=c, xt=xt: q.tensor_scalar(out=xt[:], in0=hS[:, c, :], scalar1=ssq[:, 4 + c:5 + c], scalar2=None, op0=ALU.mult),
                     r=[B_hS, B_ssq], w=[bx])
                for half in range(2):
                    b = bank()
                    for j in range(4):
                        kt = half * 4 + j
                        K.op("pe", lambda q, b=b, j=j, kt=kt, xt=xt: q.transpose(out=psb(b)[:, j * 128:(j + 1) * 128], in_=xt[:, kt * 128:(kt + 1) * 128], identity=identb[:]),
                             r=[bx, Bc], w=[PB[b]])
                    K.op("dve", lambda q, b=b, half=half, c=c: q.tensor_tensor(
                        out=dstT[:, half * 4:half * 4 + 4, c * 128:(c + 1) * 128],
                        in0=psb(b)[:, 0:512].rearrange("p (j t) -> p j t", j=4),
                        in1=pp[:, nwoff + half * 4:nwoff + half * 4 + 4].unsqueeze(2).to_broadcast([128, 4, 128]), op=ALU.mult),
                        r=[PB[b], B_par], w=[B_dst])

        def mm_fm(wt, bw, KT, rhsT, B_rhs, b, kt0=0, first=True, last=True, wkt0=0):
            for kt in range(KT):
                K.op("pe", lambda q, kt=kt: q.matmul(ps[:, b, :], lhsT=wt[:, wkt0 + kt, :], rhs=rhsT[:, kt0 + kt, :],
                                                    start=(first and kt == 0), stop=(last and kt == KT - 1)),
                     r=[bw, B_rhs], w=[PB[b]])

        def mm_tm(wt, bw, KT, lhsT_src, B_l, b, ncols=128, cstride=128, wkt0=0, kt0=0, first=True, last=True):
            for c in range(4):
                for kt in range(KT):
                    K.op("pe", lambda q, c=c, kt=kt: q.matmul(ps[:, b, c * cstride:c * cstride + ncols],
                                                              lhsT=lhsT_src[:, kt0 + kt, c * 128:(c + 1) * 128], rhs=wt[:, wkt0 + kt, 0:ncols],
                                                              start=(first and kt == 0), stop=(last and kt == KT - 1)),
                         r=[bw, B_l], w=[PB[b]])

        for l in range(L):
            src_d = x_d if l == 0 else hb_d
            last_layer = (l == L - 1)
            K.dma("sp", pp[:], pp_d[l], w=[B_par])
            K.dma("sp", rows[:], rows_d[l], w=[B_par])
            K.op("act", lambda q: q.activation(out=A_b[:], in_=rows[:, 16:32], func=AF.Exp), r=[B_par], w=[B_par])
            K.op("dve", lambda q: q.tensor_scalar(out=A_b[:], in0=A_b[:], scalar1=-1.0, scalar2=None, op0=ALU.mult), r=[B_par], w=[B_par])
            K.op("pool", lambda q: q.memset(state[:], 0.0), w=[B_state])
            K.op("pool", lambda q: q.memset(state_bf[:], 0.0), w=[B_statebf])
            K.op("pool", lambda q: q.memset(chalo[:], 0.0), w=[B_chalo])
            K.op("pool", lambda q: q.memset(phalo[:], 0.0), w=[B_phalo])

            for sc in range(NSC):
                t0 = sc * S
                cg0 = sc * 4
                cur["l"], cur["slot"] = l, 0
                SB = ssd_bufs
                K.dma("act", hS[:], src_d[t0:t0 + S, :].rearrange("(c p) d -> p c d", p=128), r=([B_hb[sc]] if l > 0 else []), w=[B_hS])
                rms_T(64, xnT, B_xnT)
                K.handoff(list(ffn_bufs.values()) + list(mrg_bufs.values()) + list(att_bufs.values()), list(SB.values()))
                dg = dbg and l == 0 and sc < 2
                if dg:
                    dump(f"xnT{sc}", xnT[:].rearrange("p k t -> p (k t)"), [B_xnT], 4096)

                wpool, bwpool = wtile()
                K.op("pool", lambda q, wpool=wpool: q.tensor_copy(out=junk[:, 0:512].rearrange("p (g c) -> p g c", g=4), in_=wpool[:, 0:4, :]), r=[bwpool], w=[B_junk])
                poolw = junk[:, 0:512].rearrange("p (g c) -> p g c", g=4)

                for j in range(12):
                    wt, bw = wtile()
                    b = bank()
                    mm_fm(wt, bw, 8, xnT, B_xnT, b)
                    up, bu = upre[j % 2], B_upre[j % 2]
                    K.op("act", lambda q, b=b, up=up: q.activation(out=up[:, 3:515], in_=ps[:, b, :], func=AF.Copy), r=[PB[b]], w=[bu])
                    K.op("pool", lambda q, j=j, up=up: q.tensor_copy(out=up[:, 0:3], in_=chalo[:, j, :]), r=[B_chalo], w=[bu])
                    K.op("dve", lambda q, j=j, up=up: q.tensor_scalar(out=ctmp, in0=up[:, 0:512], scalar1=pp[:, j * 4:j * 4 + 1], scalar2=None, op0=ALU.mult),
                         r=[bu, B_par], w=[SB["ctmp"]])
                    for k in range(1, 4):
                        K.op("dve", lambda q, j=j, k=k, up=up: q.scalar_tensor_tensor(out=ctmp, in0=up[:, k:k + 512], scalar=pp[:, j * 4 + k:j * 4 + k + 1], in1=ctmp, op0=ALU.mult, op1=ALU.add),
                             r=[bu, B_par, SB["ctmp"]], w=[SB["ctmp"]])
                    K.op("pool", lambda q, j=j, up=up: q.tensor_copy(out=chalo[:, j, :], in_=up[:, 512:515]), r=[bu], w=[B_chalo])
                    K.op("act", lambda q, j=j: q.activation(out=xbc_act[:, j, :], in_=ctmp, func=AF.Silu, bias=pp[:, 48 + j:49 + j]),
                         r=[SB["ctmp"], B_par], w=[SB["xbc_act"]])
                for c in range(4):
                    for half in range(2):
                        b = bank()
                        for j in range(4):
                            K.op("pe", lambda q, b=b, j=j, c=c, half=half: q.transpose(out=psb(b)[:, j * 128:(j + 1) * 128], in_=xbc_act[:, half * 4 + j, c * 128:(c + 1) * 128], identity=identb[:]),
                                 r=[SB["xbc_act"], Bc], w=[PB[b]])
                        K.op("act", lambda q, b=b, c=c, half=half: q.activation(out=xs_tok[:, c, half * 512:(half + 1) * 512], in_=psb(b)[:, 0:512], func=AF.Copy),
                             r=[PB[b]], w=[SB["xs_tok"]])
                    b = bank()
                    for g in range(2):
                        K.op("pe", lambda q, b=b, g=g, c=c: q.transpose(out=psb(b)[:, g * 128:(g + 1) * 128], in_=xbc_act[:, 8 + g, c * 128:(c + 1) * 128], identity=identb[:]),
                             r=[SB["xbc_act"], Bc], w=[PB[b]])
                    K.op("act", lambda q, b=b, c=c: q.activation(out=B_tok[:, c, :], in_=psb(b)[:, 0:256], func=AF.Copy), r=[PB[b]], w=[SB["B_tok"]])

                if dg:
                    dump(f"xbc{sc}a", xbc_act[:, 0:8, :].rearrange("p k t -> p (k t)"), [SB["xbc_act"]], 4096)
                    dump(f"xbc{sc}b", xbc_act[:, 8:12, :].rearrange("p k t -> p (k t)"), [SB["xbc_act"]], 2048)
                    dump(f"xstok{sc}", xs_tok.rearrange("p c d -> p (c d)"), [SB["xs_tok"]], 4096)
                for f in range(8):
                    wt, bw = wtile()
                    b = bank()
                    mm_tm(wt, bw, 8, xnT, B_xnT, b)
                    K.op("act", lambda q, b=b, f=f: q.activation(out=zs[:, :, f * 128:(f + 1) * 128], in_=ps[:, b, :].rearrange("p (c t) -> p c t", c=4), func=AF.Silu),
                         r=[PB[b]], w=[SB["zs"]])
                wt, bw = wtile()
                b = bank()
                mm_tm(wt, bw, 8, xnT, B_xnT, b, ncols=20, cstride=32)
                K.op("act", lambda q, b=b: q.activation(out=dw[:], in_=ps[:, b, 0:128].rearrange("p (c t) -> p c t", c=4)[:, :, 0:20], func=AF.Copy), r=[PB[b]], w=[B_dw])
                wt, bw = wtile()
                b = bank()
                mm_tm(wt, bw, 8, xnT, B_xnT, b)
                K.op("act", lambda q, b=b, cg0=cg0: q.activation(out=Vh[:, cg0:cg0 + 4, :], in_=ps[:, b, :].rearrange("p (c t) -> p c t", c=4), func=AF.Copy), r=[PB[b]], w=[B_V])
                for i in range(4):
                    wt, bw = wtile()
                    b = bank()
                    mm_fm(wt, bw, 8, xnT, B_xnT, b)
                    K.op("act", lambda q, b=b, i=i: q.activation(out=qT[:, i, :], in_=ps[:, b, :], func=AF.Copy, scale=0.125), r=[PB[b]], w=[B_qT])
                wt, bw = wtile()
                b = bank()
                mm_fm(wt, bw, 8, xnT, B_xnT, b)
                K.op("act", lambda q, b=b, t0=t0: q.activation(out=kT[:, t0:t0 + S], in_=ps[:, b, :], func=AF.Copy), r=[PB[b]], w=[B_kT])
                for i in range(2):
                    wt, bw = wtile()
                    b = bank()
                    mm_fm(wt, bw, 8, xnT, B_xnT, b)
                    K.op("act", lambda q, b=b, i=i: q.activation(out=qiT[:, i, :], in_=ps[:, b, :], func=AF.Copy, scale=0.0625), r=[PB[b]], w=[B_qiT])
                wt, bw = wtile()
                b = bank()
                mm_fm(wt, bw, 8, xnT, B_xnT, b)
                K.op("act", lambda q, b=b, t0=t0: q.activation(out=kiT[:, t0:t0 + S], in_=ps[:, b, :], func=AF.Copy), r=[PB[b]], w=[B_kiT])

                for g in range(4):
                    win = POOL_WINDOWS[g]
                    wt, bw = wtile()
                    b = bank()
                    mm_fm(wt, bw, 8, xnT, B_xnT, b)
                    ub, bub = ubuf[g % 2], B_ubuf[g % 2]
                    K.op("act", lambda q, b=b, ub=ub: q.activation(out=ub[:, 15:527], in_=ps[:, b, :], func=AF.Copy), r=[PB[b]], w=[bub])
                    K.op("pool", lambda q, g=g, ub=ub: q.tensor_copy(out=ub[:, 0:15], in_=phalo[:, g, :]), r=[B_phalo], w=[bub])
                    srcb, bsrc = ub, bub
                    lvl = 1
                    pi = 0
                    while lvl < win:
                        dst, bdst = ptmp[pi], B_ptmp[pi]
                        lo = 2 * lvl - 1
                        K.op("dve", lambda q, srcb=srcb, dst=dst, lo=lo, lvl=lvl: q.tensor_tensor(out=dst[:, lo:527], in0=srcb[:, lo:527], in1=srcb[:, lo - lvl:527 - lvl], op=ALU.add),
                             r=[bsrc], w=[bdst])
                        srcb, bsrc = dst, bdst
                        pi ^= 1
                        lvl *= 2
                    pl, bpl = pooled[g % 2], B_pooled[g % 2]
                    if sc == 0:
                        K.op("dve", lambda q, srcb=srcb, g=g: q.tensor_tensor(out=srcb[:, 15:31], in0=srcb[:, 15:31], in1=rc16[:, g, :], op=ALU.mult), r=[bsrc, Bc], w=[bsrc])
                        K.op("dve", lambda q, srcb=srcb, ub=ub, pl=pl: q.tensor_tensor(out=pl[:, 0:16], in0=srcb[:, 15:31], in1=ub[:, 15:31], op=ALU.subtract), r=[bsrc, bub], w=[bpl])
                        K.op("dve", lambda q, srcb=srcb, ub=ub, pl=pl, win=win: q.scalar_tensor_tensor(out=pl[:, 16:512], in0=srcb[:, 31:527], scalar=1.0 / win, in1=ub[:, 31:527], op0=ALU.mult, op1=ALU.subtract),
                             r=[bsrc, bub], w=[bpl])
                    else:
                        K.op("dve", lambda q, srcb=srcb, ub=ub, pl=pl, win=win: q.scalar_tensor_tensor(out=pl[:, 0:512], in0=srcb[:, 15:527], scalar=1.0 / win, in1=ub[:, 15:527], op0=ALU.mult, op1=ALU.subtract),
                             r=[bsrc, bub], w=[bpl])
                    K.op("pool", lambda q, g=g, ub=ub: q.tensor_copy(out=phalo[:, g, :], in_=ub[:, 512:527]), r=[bub], w=[B_phalo])
                    b2 = bank()
                    K.op("pe", lambda q, b2=b2, g=g, pl=pl: q.matmul(ps[:, b2, :], lhsT=poolw[:, g, :], rhs=pl[:], start=True, stop=True), r=[B_junk, bpl], w=[PB[b2]])
                    K.op("act", lambda q, b2=b2, g=g: q.activation(out=ypoolT[:, g, :], in_=ps[:, b2, :], func=AF.Copy, scale=pp[:, 60 + g:61 + g]), r=[PB[b2], B_par], w=[B_ypoolT])

                if dg:
                    dump(f"zs{sc}", zs.rearrange("p c d -> p (c d)"), [SB["zs"]], 4096)
                    dump(f"dw{sc}", dw[:].rearrange("p c d -> p (c d)"), [B_dw], 80)
                    dump(f"ypoolT{sc}", ypoolT[:].rearrange("p k t -> p (k t)"), [B_ypoolT], 2048)
                    dump(f"qT{sc}", qT[:].rearrange("p k t -> p (k t)"), [B_qT], 2048)
                sm = small
                for c in range(4):
                    cc = slice(c * 128, (c + 1) * 128)
                    K.op("dve", lambda q, c=c: q.tensor_tensor(out=sm[:, 0:16], in0=dw[:, c, 0:16], in1=rows[:, 0:16], op=ALU.add), r=[B_dw, B_par], w=[B_sm["dt"]])
                    K.op("act", lambda q: q.activation(out=sm[:, 0:16], in_=sm[:, 0:16], func=AF.Exp), r=[B_sm["dt"]], w=[B_sm["dt"]])
                    K.op("act", lambda q: q.activation(out=sm[:, 0:16], in_=sm[:, 0:16], func=AF.Ln, bias=1.0), r=[B_sm["dt"]], w=[B_sm["dt"]])
                    K.op("pool", lambda q: q.memset(sm[:, 32:80], 0.0), w=[B_sm["a48"]])
                    K.op("dve", lambda q: q.tensor_tensor(out=sm[:, 32:48], in0=sm[:, 0:16], in1=A_b[:], op=ALU.mult), r=[B_sm["dt"], B_par], w=[B_sm["a48"]])
                    K.op("dve", lambda q: q.tensor_tensor(out=sm[:, 64:80], in0=sm[:, 0:16], in1=A_b[:], op=ALU.mult), r=[B_sm["dt"], B_par], w=[B_sm["a48"]])
                    bs = bank()
                    a16 = sm[:, 32:48]
                    K.op("pe", lambda q, bs=bs: q.matmul(ps[:, bs, 0:16], lhsT=tri_le, rhs=a16, start=True, stop=True), r=[Bc, B_sm["a48"]], w=[PB[bs]])
                    K.op("pe", lambda q, bs=bs: q.matmul(ps[:, bs, 16:32], lhsT=tri_gt, rhs=a16, start=True, stop=True), r=[Bc, B_sm["a48"]], w=[PB[bs]])
                    K.op("pe", lambda q, bs=bs: q.matmul(ps[:, bs, 32:48], lhsT=onesf, rhs=a16, start=True, stop=True), r=[Bc, B_sm["a48"]], w=[PB[bs]])
                    K.op("act", lambda q, bs=bs: q.activation(out=sm[:, 80:128], in_=ps[:, bs, 0:48], func=AF.Exp), r=[PB[bs]], w=[B_sm["e3"]])
                    bt = bank()
                    K.op("pe", lambda q, bt=bt: q.matmul(ps[0:48, bt, 0:128], lhsT=sm[:, 32:80], rhs=tri_le, start=True, stop=True), r=[Bc, B_sm["a48"]], w=[PB[bt]])
                    K.op("act", lambda q, bt=bt: q.activation(out=RHS[0:16, :], in_=ps[0:16, bt, 0:128], func=AF.Copy), r=[PB[bt]], w=[B_RHS])
                    K.op("dve", lambda q, bt=bt: q.tensor_tensor(out=LHS[32:48, :, :], in0=ps[32:48, bt, 0:128].unsqueeze(1).to_broadcast([16, 16, 128]),
                                                                  in1=oh[32:48, 16:32].unsqueeze(2).to_broadcast([16, 16, 128]), op=ALU.mult),
                         r=[PB[bt], Bc], w=[B_LHS])
                    xs3 = xs_tok[:, c, :].rearrange("p (h d) -> p h d", h=16)
                    K.op("dve", lambda q: q.tensor_tensor(out=sm[:, 128:144], in0=sm[:, 0:16], in1=sm[:, 96:112], op=ALU.mult), r=[B_sm["dt"], B_sm["e3"]], w=[B_sm["dtdec"]])
                    K.op("dve", lambda q, xs3=xs3: q.tensor_tensor(out=x_dt.rearrange("p (h d) -> p h d", h=16), in0=xs3, in1=sm[:, 0:16].unsqueeze(2).to_broadcast([128, 16, 64]), op=ALU.mult),
                         r=[SB["xs_tok"], B_sm["dt"]], w=[SB["x_dt"]])
                    K.op("dve", lambda q, xs3=xs3: q.tensor_tensor(out=xdec.rearrange("p (h d) -> p h d", h=16), in0=xs3, in1=sm[:, 128:144].unsqueeze(2).to_broadcast([128, 16, 64]), op=ALU.mult),
                         r=[SB["xs_tok"], B_sm["dtdec"]], w=[SB["xdec"]])
                    K.op("dve", lambda q, xs3=xs3: q.tensor_tensor(out=xsd.rearrange("p (h d) -> p h d", h=16), in0=xs3, in1=rows[:, 32:48].unsqueeze(2).to_broadcast([128, 16, 64]), op=ALU.mult),
                         r=[SB["xs_tok"], B_par], w=[SB["xsd"]])
                    by = bank2()
                    live = (by, by + 1)
                    for g in range(2):
                        bcb = bank(live)
                        K.op("pe", lambda q, bcb=bcb, g=g, cc=cc: q.matmul(ps[:, bcb, 0:128], lhsT=xbc_act[:, 8 + g, cc], rhs=xbc_act[:, 10 + g, cc], start=True, stop=True),
                             r=[SB["xbc_act"]], w=[PB[bcb]])
                        K.op("act", lambda q, bcb=bcb: q.activation(out=cb, in_=ps[:, bcb, 0:128], func=AF.Copy), r=[PB[bcb]], w=[SB["cb"]])
                        be = bank2(live)
                        for j in range(8):
                            h = g * 8 + j
                            bb_, off = be + j // 4, (j % 4) * 128
                            K.op("pe", lambda q, bb_=bb_, off=off, h=h: q.matmul(ps[:, bb_, off:off + 128], lhsT=LHS[:, h, :], rhs=RHS[:], start=True, stop=False),
                                 r=[B_LHS, B_RHS], w=[PB[bb_]])
                            K.op("pe", lambda q, bb_=bb_, off=off: q.matmul(ps[:, bb_, off:off + 128], lhsT=identf, rhs=maskneg, start=False, stop=True),
                                 r=[Bc], w=[PB[bb_]])
                        K.op("act", lambda q, be=be: q.activation(out=Lexp, in_=ps[:, be:be + 2, :].rearrange("p b (j t) -> p (b j) t", j=4), func=AF.Exp),
                             r=[PB[be], PB[be + 1]], w=[SB["Lexp"]])
                        K.op("dve", lambda q: q.tensor_tensor(out=Mt, in0=Lexp, in1=cb.unsqueeze(1).to_broadcast([128, 8, 128]), op=ALU.mult),
                             r=[SB["Lexp"], SB["cb"]], w=[SB["Mt"]])
                        for j in range(8):
                            h = g * 8 + j
                            hb_, hoff = h // 8, (h % 8) * 64
                            K.op("pe", lambda q, j=j, hb_=hb_, hoff=hoff, h=h, by=by: q.matmul(ps[:, by + hb_, hoff:hoff + 64], lhsT=Mt[:, j, :], rhs=x_dt[:, h * 64:(h + 1) * 64], start=True, stop=False),
                                 r=[SB["Mt"], SB["x_dt"]], w=[PB[by + hb_]])
                            K.op("pe", lambda q, hb_=hb_, hoff=hoff, h=h, by=by: q.matmul(ps[:, by + hb_, hoff:hoff + 64], lhsT=identb[:], rhs=xsd[:, h * 64:(h + 1) * 64], start=False, stop=True),
                                 r=[Bc, SB["xsd"]], w=[PB[by + hb_]])
                    bo = bank2(live)
                    for h in range(16):
                        g = h // 8
                        hb_, hoff = h // 8, (h % 8) * 64
                        K.op("pe", lambda q, hb_=hb_, hoff=hoff, h=h, g=g, cc=cc, bo=bo: q.matmul(ps[:, bo + hb_, hoff:hoff + 64], lhsT=xbc_act[:, 10 + g, cc], rhs=state_bf[:, h, :], start=True, stop=True),
                             r=[SB["xbc_act"], B_statebf], w=[PB[bo + hb_]])
                    bsn = bank2(live + (bo, bo + 1))
                    for h in range(16):
                        g = h // 8
                        hb_, hoff = h // 8, (h % 8) * 64
                        K.op("pe", lambda q, hb_=hb_, hoff=hoff, h=h, g=g, c=c, bsn=bsn: q.matmul(ps[:, bsn + hb_, hoff:hoff + 64], lhsT=B_tok[:, c, g * 128:(g + 1) * 128], rhs=xdec[:, h * 64:(h + 1) * 64], start=True, stop=True),
                             r=[SB["B_tok"], SB["xdec"]], w=[PB[bsn + hb_]])
                    cd_b = sm[:, 112:128].unsqueeze(2).to_broadcast([128, 16, 64])
                    K.op("dve", lambda q, cd_b=cd_b: q.tensor_tensor(out=state[:], in0=state[:], in1=cd_b, op=ALU.mult), r=[B_state, B_sm["e3"]], w=[B_state])
                    K.op("dve", lambda q, bsn=bsn: q.tensor_tensor(out=state[:].rearrange("p (b h) d -> p b (h d)", b=2), in0=ps[:, bsn:bsn + 2, :], in1=state[:].rearrange("p (b h) d -> p b (h d)", b=2), op=ALU.add),
                         r=[PB[bsn], PB[bsn + 1], B_state], w=[B_state])
                    K.op("pool", lambda q: q.tensor_copy(out=state_bf[:], in_=state[:]), r=[B_state], w=[B_statebf])
                    ea_b = sm[:, 80:96].unsqueeze(2).to_broadcast([128, 16, 64])
                    K.op("dve", lambda q, bo=bo, ea_b=ea_b: q.tensor_tensor(out=yacc.rearrange("p (h d) -> p h d", h=16), in0=ps[:, bo:bo + 2, :].rearrange("p b (h d) -> p (b h) d", h=8), in1=ea_b, op=ALU.mult),
                         r=[PB[bo], PB[bo + 1], B_sm["e3"]], w=[SB["yacc"]])
                    K.op("dve", lambda q, by=by: q.tensor_tensor(out=yacc.rearrange("p (b f) -> p b f", b=2), in0=ps[:, by:by + 2, :], in1=yacc.rearrange("p (b f) -> p b f", b=2), op=ALU.add),
                         r=[PB[by], PB[by + 1], SB["yacc"]], w=[SB["yacc"]])
                    if dg:
                        dump(f"y{sc}_{c}", yacc, [SB["yacc"]], 1024)
                        dump(f"sm{sc}_{c}", sm[:, 0:128], [B_sm["dt"], B_sm["e3"], B_sm["a48"]], 128)
                    K.op("dve", lambda q, c=c: q.tensor_tensor(out=yacc, in0=yacc, in1=zs[:, c, :], op=ALU.mult), r=[SB["yacc"], SB["zs"]], w=[SB["yacc"]])
                    K.op("act", lambda q: q.activation(out=junk[:], in_=yacc, func=AF.Square, accum_out=sm[:, 169:170]), r=[SB["yacc"]], w=[B_junk, B_sm["rs"]])
                    K.op("act", lambda q: q.activation(out=sm[:, 170:171], in_=sm[:, 169:170], func=AF.Sqrt, scale=1.0 / D, bias=EPS), r=[B_sm["rs"]], w=[B_sm["rs"]])
                    K.op("dve", lambda q: q.reciprocal(out=sm[:, 170:171], in_=sm[:, 170:171]), r=[B_sm["rs"]], w=[B_sm["rs"]])
                    K.op("dve", lambda q: q.tensor_scalar(out=ynb, in0=yacc, scalar1=sm[:, 170:171], scalar2=None, op0=ALU.mult), r=[SB["yacc"], B_sm["rs"]], w=[SB["ynb"]])
                    for half in range(2):
                        b = bank()
                        for j in range(4):
                            kt = half * 4 + j
                            K.op("pe", lambda q, b=b, j=j, kt=kt: q.transpose(out=psb(b)[:, j * 128:(j + 1) * 128], in_=ynb[:, kt * 128:(kt + 1) * 128], identity=identb[:]),
                                 r=[SB["ynb"], Bc], w=[PB[b]])
                        K.op("dve", lambda q, b=b, half=half, cc=cc: q.tensor_tensor(
                            out=ynT[:, half * 4:half * 4 + 4, cc], in0=psb(b)[:, 0:512].rearrange("p (j t) -> p j t", j=4),
                            in1=pp[:, 72 + half * 4:72 + half * 4 + 4].unsqueeze(2).to_broadcast([128, 4, 128]), op=ALU.mult),
                            r=[PB[b], B_par], w=[B_ynT])
                AB = att_bufs
                K.handoff(list(SB.values()), list(AB.values()))
                for c in range(4):
                    cg = cg0 + c
                    nk = (cg + 1) * 128
                    nb = (nk + S - 1) // S
                    cc = slice(c * 128, (c + 1) * 128)
                    for kb in range(nb):
                        w_ = min(S, nk - kb * S)
                        ks = slice(kb * S, kb * S + w_)
                        for h in range(4):
                            b = bank()
                            half, ti = h // 2, h % 2
                            prt = slice(half * 64, half * 64 + 64)
                            K.op("pe", lambda q, b=b, prt=prt, ti=ti, ks=ks, w_=w_, cc=cc: q.matmul(ps[:, b, 0:w_], lhsT=qiT[prt, ti, cc], rhs=kiT[prt, ks], start=True, stop=True),
                                 r=[B_qiT, B_kiT], w=[PB[b]])
                            rt, brt = rtmp[h % 2], AB[f"rtmp{h % 2}"]
                            K.op("act", lambda q, b=b, rt=rt, w_=w_: q.activation(out=rt[:, 0:w_], in_=ps[:, b, 0:w_], func=AF.Relu), r=[PB[b]], w=[brt])
                            if h == 0:
                                K.op("dve", lambda q, rt=rt, ks=ks, w_=w_, c=c: q.tensor_scalar(out=score[:, ks], in0=rt[:, 0:w_], scalar1=dw[:, c, 16:17], scalar2=None, op0=ALU.mult),
                                     r=[brt, B_dw], w=[AB["score"]])
                            else:
                                K.op("dve", lambda q, rt=rt, ks=ks, w_=w_, c=c, h=h: q.scalar_tensor_tensor(out=score[:, ks], in0=rt[:, 0:w_], scalar=dw[:, c, 16 + h:17 + h], in1=score[:, ks], op0=ALU.mult, op1=ALU.add),
                                     r=[brt, B_dw, AB["score"]], w=[AB["score"]])
                    K.op("dve", lambda q, nk=nk: q.tensor_tensor(out=score[:, nk - 128:nk], in0=score[:, nk - 128:nk], in1=cmaskneg, op=ALU.add), r=[AB["score"], Bc], w=[AB["score"]])
                    if cg >= 2:
                        for it in range(32):
                            srcv = score if it == 0 else work
                            bsrc = AB["score"] if it == 0 else AB["work"]
                            K.op("dve", lambda q, srcv=srcv, nk=nk: q.max(out=sm[:, 144:152], in_=srcv[:, 0:nk]), r=[bsrc], w=[B_sm["m8"]])
                            if it < 31:
                                K.op("dve", lambda q, srcv=srcv, nk=nk: q.match_replace(out=work[:, 0:nk], in_to_replace=sm[:, 144:152], in_values=srcv[:, 0:nk], imm_value=-3.0e38),
                                     r=[bsrc, B_sm["m8"]], w=[AB["work"]])
                        K.op("dve", lambda q: q.tensor_copy(out=sm[:, 152:153], in_=sm[:, 151:152]), r=[B_sm["m8"]], w=[B_sm["thr"]])
                    else:
                        K.op("dve", lambda q: q.memset(sm[:, 152:153], -1.0e29), w=[B_sm["thr"]])
                    bO = bank()
                    K.op("dve", lambda q: q.memset(rsb[:], 0.0), w=[B_sm["rsum"]])
                    for h in range(8):
                        g = h // 4
                        prt = slice(g * 64, g * 64 + 64)
                        ti = h % 4
                        for kb in range(nb):
                            w_ = min(S, nk - kb * S)
                            ks = slice(kb * S, kb * S + w_)
                            nsub = w_ // 128
                            b = bank((bO,))
                            i2 = (h * nb + kb) % 2
                            K.op("pe", lambda q, b=b, prt=prt, ti=ti, ks=ks, w_=w_, cc=cc: q.matmul(ps[:, b, 0:w_], lhsT=qT[prt, ti, cc], rhs=kT[prt, ks], start=True, stop=True),
                                 r=[B_qT, B_kT], w=[PB[b]])
                            K.op("act", lambda q, b=b, i2=i2, w_=w_: q.activation(out=Pb[i2][:, 0:w_], in_=ps[:, b, 0:w_], func=AF.Exp), r=[PB[b]], w=[AB[f"Pb{i2}"]])
                            K.op("dve", lambda q, i2=i2, ks=ks, w_=w_, h=h, kb=kb: q.scalar_tensor_tensor(out=Pm[i2][:, 0:w_], in0=score[:, ks], scalar=sm[:, 152:153], in1=Pb[i2][:, 0:w_],
                                                                                                  op0=ALU.is_ge, op1=ALU.mult, accum_out=rsb[:, h, kb:kb + 1]),
                                 r=[AB["score"], B_sm["thr"], AB[f"Pb{i2}"]], w=[AB[f"Pm{i2}"], B_sm["rsum"]])
                            b2 = bank((bO,))
                            for i in range(nsub):
                                K.op("pe", lambda q, b2=b2, i=i, i2=i2: q.transpose(out=psb(b2)[:, i * 128:(i + 1) * 128], in_=Pm[i2][:, i * 128:(i + 1) * 128], identity=identb[:]),
                                     r=[AB[f"Pm{i2}"], Bc], w=[PB[b2]])
                            K.op("act", lambda q, b2=b2, i2=i2, w_=w_: q.activation(out=PmT[i2][:, 0:w_], in_=psb(b2)[:, 0:w_], func=AF.Copy), r=[PB[b2]], w=[AB[f"PmT{i2}"]])
                            for i in range(nsub):
                                kc = kb * 4 + i
                                K.op("pe", lambda q, i=i, i2=i2, kc=kc, h=h, g=g, bO=bO, first=(kb == 0 and i == 0), last=(kb == nb - 1 and i == nsub - 1):
                                     q.matmul(ps[:, bO, h * 64:(h + 1) * 64], lhsT=PmT[i2][:, i * 128:(i + 1) * 128], rhs=Vh[:, kc, g * 64:(g + 1) * 64], start=first, stop=last),
                                     r=[AB[f"PmT{i2}"], B_V], w=[PB[bO]])
                    K.op("dve", lambda q: q.tensor_reduce(out=sm[:, 153:161], in_=rsb[:], axis=AX.X, op=ALU.add), r=[B_sm["rsum"]], w=[B_sm["rinv"]])
                    K.op("dve", lambda q: q.reciprocal(out=sm[:, 161:169], in_=sm[:, 153:161]), r=[B_sm["rinv"]], w=[B_sm["rinv"]])
                    K.op("dve", lambda q, bO=bO: q.tensor_tensor(out=yatt.rearrange("p (h d) -> p h d", h=8), in0=ps[:, bO, :].rearrange("p (h d) -> p h d", h=8),
                                                                  in1=sm[:, 161:169].unsqueeze(2).to_broadcast([128, 8, 64]), op=ALU.mult),
                         r=[PB[bO], B_sm["rinv"]], w=[AB["yatt"]])
                    if dg and sc == 0 and c in (1, 2):
                        dump(f"score{c}", score[:, 0:512], [AB["score"]], 512)
                        dump(f"smatt{c}", sm[:, 128:256], [B_sm["m8"], B_sm["thr"], B_sm["rinv"]], 128)
                        dump(f"rsb{c}", rsb[:].rearrange("p h k -> p (h k)"), [B_sm["rsum"]], 64)
                        dump(f"yatt{c}", yatt, [AB["yatt"]], 512)
                        dump(f"Pm{c}", Pm[(7 * nb + nb - 1) % 2], [AB["Pm0"], AB["Pm1"]], 512)
                        dump(f"PmT{c}", PmT[(7 * nb + nb - 1) % 2], [AB["PmT0"], AB["PmT1"]], 512)
                        dump(f"Pb{c}", Pb[(7 * nb + nb - 1) % 2], [AB["Pb0"], AB["Pb1"]], 512)
                    b = bank()
                    for j in range(4):
                        K.op("pe", lambda q, b=b, j=j: q.transpose(out=psb(b)[:, j * 128:(j + 1) * 128], in_=yatt[:, j * 128:(j + 1) * 128], identity=identb[:]), r=[AB["yatt"], Bc], w=[PB[b]])
                    K.op("act", lambda q, b=b, cc=cc: q.activation(out=yattT[:, :, cc], in_=psb(b)[:, 0:512].rearrange("p (j t) -> p j t", j=4), func=AF.Copy), r=[PB[b]], w=[B_yattT])

                if dg:
                    dump(f"ynT{sc}", ynT[:].rearrange("p k t -> p (k t)"), [B_ynT], 4096)
                    dump(f"yattT{sc}", yattT[:].rearrange("p k t -> p (k t)"), [B_yattT], 2048)
                MB = mrg_bufs
                K.handoff(list(AB.values()), list(MB.values()))
                for f in range(8):
                    w1, bw1 = wtile()
                    b1 = bank()
                    mm_fm(w1, bw1, 8, ynT, B_ynT, b1)
                    w2, bw2 = wtile()
                    b2 = bank()
                    mm_fm(w2, bw2, 4, yattT, B_yattT, b2)
                    b3 = bank()
                    mm_fm(w2, bw2, 4, ypoolT, B_ypoolT, b3, wkt0=4)
                    ups = [b1, b2, b3]
                    for br in range(3):
                        wg, bwg = wtile()
                        bg = bank()
                        mm_fm(wg, bwg, 8, xnT, B_xnT, bg)
                        K.op("act", lambda q, bg=bg, br=br: q.activation(out=sig[br], in_=ps[:, bg, :], func=AF.Sigmoid), r=[PB[bg]], w=[MB[f"sig{br}"]])
                        K.op("dve", lambda q, br=br, bu_=ups[br]: q.tensor_tensor(out=mt[br], in0=ps[:, bu_, :], in1=sig[br], op=ALU.mult), r=[PB[ups[br]], MB[f"sig{br}"]], w=[MB[f"mt{br}"]])
                    K.op("pool", lambda q: q.tensor_tensor(out=mt[0], in0=mt[0], in1=mt[1], op=ALU.add), r=[MB["mt0"], MB["mt1"]], w=[MB["mt0"]])
                    K.op("pool", lambda q, f=f: q.tensor_tensor(out=mergedT[:, f, :], in0=mt[0], in1=mt[2], op=ALU.add), r=[MB["mt0"], MB["mt2"]], w=[MB["mergedT"]])
                for f in range(8):
                    wt, bw = wtile()
                    b = bank()
                    mm_tm(wt, bw, 8, mergedT, MB["mergedT"], b)
                    K.op("dve", lambda q, b=b, f=f: q.tensor_tensor(out=hS[:, :, f * 128:(f + 1) * 128], in0=ps[:, b, :].rearrange("p (c t) -> p c t", c=4), in1=hS[:, :, f * 128:(f + 1) * 128], op=ALU.add),
                         r=[PB[b], B_hS], w=[B_hS])

                if dg:
                    dump(f"mergedT{sc}", mergedT.rearrange("p k t -> p (k t)"), [MB["mergedT"]], 4096)
                    dump(f"h1_{sc}", hS[:].rearrange("p c d -> p (c d)"), [B_hS], 4096)
                FB = ffn_bufs
                rms_T(80, xnT, B_xnT)
                K.handoff(list(MB.values()), list(FB.values()))
                for j in range(22):
                    wa, bwa = wtile()
                    ba = bank()
                    mm_fm(wa, bwa, 8, xnT, B_xnT, ba)
                    K.op("act", lambda q, ba=ba, j=j: q.activation(out=sa[j % 2], in_=ps[:, ba, :], func=AF.Silu), r=[PB[ba]], w=[FB[f"sa{j % 2}"]])
                    wb_, bwb = wtile()
                    bb = bank()
                    mm_fm(wb_, bwb, 8, xnT, B_xnT, bb)
                    K.op("dve", lambda q, bb=bb, j=j: q.tensor_tensor(out=hidT[:, j, :], in0=ps[:, bb, :], in1=sa[j % 2], op=ALU.mult), r=[PB[bb], FB[f"sa{j % 2}"]], w=[FB["hidT"]])
                for f in range(8):
                    b4 = bank4()
                    for part in range(3):
                        wt, bw = wtile()
                        nkt = 8 if part < 2 else 6
                        for c in range(4):
                            for kt in range(nkt):
                                K.op("pe", lambda q, c=c, kt=kt, part=part, wt=wt, b4=b4, nkt=nkt: q.matmul(ps[:, b4 + c, 0:128], lhsT=hidT[:, part * 8 + kt, c * 128:(c + 1) * 128], rhs=wt[:, kt, :],
                                                                                                 start=(part == 0 and kt == 0), stop=(part == 2 and kt == nkt - 1)),
                                     r=[bw, FB["hidT"]], w=[PB[b4 + c]])
                    K.op("dve", lambda q, b4=b4, f=f: q.tensor_tensor(out=hS[:, :, f * 128:(f + 1) * 128], in0=ps[:, b4:b4 + 4, 0:128], in1=hS[:, :, f * 128:(f + 1) * 128], op=ALU.add),
                         r=[PB[b4], PB[b4 + 1], PB[b4 + 2], PB[b4 + 3], B_hS], w=[B_hS])
                assert cur["slot"] == NSLOT, cur["slot"]

                if last_layer:
                    for c in range(4):
                        K.op("act", lambda q, c=c: q.activation(out=junk[:], in_=hS[:, c, :], func=AF.Square, accum_out=ssq[:, c:c + 1]), r=[B_hS], w=[B_junk, B_ssq])
                    K.op("act", lambda q: q.activation(out=ssq[:, 4:8], in_=ssq[:, 0:4], func=AF.Sqrt, scale=1.0 / D, bias=EPS), r=[B_ssq], w=[B_ssq])
                    K.op("dve", lambda q: q.reciprocal(out=ssq[:, 4:8], in_=ssq[:, 4:8]), r=[B_ssq], w=[B_ssq])
                    for c in range(4):
                        K.op("dve", lambda q, c=c: q.scalar_tensor_tensor(out=hS[:, c, :], in0=hS[:, c, :], scalar=ssq[:, 4 + c:5 + c], in1=fnw[:], op0=ALU.mult, op1=ALU.mult),
                             r=[B_hS, B_ssq, Bc], w=[B_hS])
                    K.dma("act", y_d[t0:t0 + S, :].rearrange("(c p) d -> p c d", p=128), hS[:], r=[B_hS], w=[B_out])
                else:
                    K.dma("act", hb_d[t0:t0 + S, :].rearrange("(c p) d -> p c d", p=128), hS[:], r=[B_hS], w=[B_hb[sc]])
                    pass
        K.final_wait("act", [B_out])
        K.final_wait("sp", [B_out])
        K.emit()
    return nc, K


B_out = Buf()
B_hb = [Buf() for _ in range(64)]


def _tile_k(wcols, kt_total=8):
    Kd, ncol = wcols.shape
    nkt = Kd // 128
    out = np.zeros((128, kt_total, 128), np.float32)
    out[:, :nkt, :ncol] = wcols.reshape(nkt, 128, ncol).transpose(1, 0, 2)
    return out


def pack_layer(inp, l):
    w_in = inp["w_in"][l]
    slots = []
    pw = np.zeros((128, 8, 128), np.float32)
    for g in range(4):
        pw[:, g, :] = inp["pool_w"][l, g]
    slots.append(pw)
    for j in range(12):
        slots.append(_tile_k(w_in[:, 1024 + 128 * j:1024 + 128 * (j + 1)]))
    for f in range(8):
        slots.append(_tile_k(w_in[:, 128 * f:128 * (f + 1)]))
    slots.append(_tile_k(np.concatenate([w_in[:, 2560:2576], w_in[:, 3664:3668]], axis=1)))
    slots.append(_tile_k(w_in[:, 3216:3344]))
    for i in range(4):
        slots.append(_tile_k(np.concatenate([w_in[:, 2576 + 64 * i:2576 + 64 * (i + 1)], w_in[:, 2576 + 64 * (4 + i):2576 + 64 * (5 + i)]], axis=1)))
    slots.append(_tile_k(w_in[:, 3088:3216]))
    for i in range(2):
        slots.append(_tile_k(np.concatenate([w_in[:, 3344 + 64 * i:3344 + 64 * (i + 1)], w_in[:, 3344 + 64 * (2 + i):3344 + 64 * (3 + i)]], axis=1)))
    slots.append(_tile_k(np.concatenate([w_in[:, 3600:3664], w_in[:, 3600:3664]], axis=1)))
    for g in range(4):
        slots.append(_tile_k(w_in[:, 3668 + 128 * g:3668 + 128 * (g + 1)]))
    for f in range(8):
        cs = slice(128 * f, 128 * (f + 1))
        slots.append(_tile_k(inp["w_up_ssd"][l][:, cs]))
        ap = np.zeros((128, 8, 128), np.float32)
        ap[:, 0:4, :] = inp["w_up_attn"][l][:, cs].reshape(4, 128, 128).transpose(1, 0, 2)
        ap[:, 4:8, :] = inp["w_up_pool"][l][:, cs].reshape(4, 128, 128).transpose(1, 0, 2)
        slots.append(ap)
        for br in range(3):
            c0 = 4180 + (br * 8 + f) * 128
            slots.append(_tile_k(w_in[:, c0:c0 + 128]))
    for f in range(8):
        slots.append(_tile_k(inp["w_out"][l][:, 128 * f:128 * (f + 1)]))
    wfi = inp["w_ffn_in"][l]
    for j in range(22):
        slots.append(_tile_k(wfi[:, 128 * j:128 * (j + 1)]))
        slots.append(_tile_k(wfi[:, 2816 + 128 * j:2816 + 128 * (j + 1)]))
    wfo = inp["w_ffn_out"][l]
    for f in range(8):
        t = wfo[:, 128 * f:128 * (f + 1)].reshape(22, 128, 128).transpose(1, 0, 2)
        for part in range(3):
            s_ = np.zeros((128, 8, 128), np.float32)
            n = 8 if part < 2 else 6
            s_[:, 0:n, :] = t[:, part * 8:part * 8 + n, :]
            slots.append(s_)
    assert len(slots) == NSLOT, len(slots)
    W = np.stack(slots).reshape(NSLOT, 128, 1024)
    pp = np.zeros((128, NPP), np.float32)
    cw = inp["conv_w"][l]
    pp[:, 0:48] = cw.reshape(4, 12, 128).transpose(2, 1, 0).reshape(128, 48)
    pp[:, 48:60] = inp["conv_b"][l].reshape(12, 128).T
    pp[:, 60:64] = inp["pool_scale"][l].reshape(4, 128).T
    pp[:, 64:72] = inp["norm1_w"][l].reshape(8, 128).T
    pp[:, 72:80] = inp["ssd_norm_w"][l].reshape(8, 128).T
    pp[:, 80:88] = inp["norm2_w"][l].reshape(8, 128).T
    rows = np.concatenate([inp["dt_bias"][l], inp["a_log"][l], inp["d_skip"][l]])[None, :].repeat(128, 0).astype(np.float32)
    return W, pp, rows


def make_consts():
    k = np.arange(128)
    cst = np.zeros((128, 7, 128), np.float32)
    cst[:, 0, :] = np.eye(128)
    cst[:, 1, :] = (k[:, None] <= k[None, :])
    cst[:, 2, :] = (k[:, None] > k[None, :])
    cst[:, 3, :] = 1.0
    cst[:, 4, :] = np.where(k[None, :] >= k[:, None], 0.0, -30000.0)
    cst[:, 5, :] = np.where(k[None, :] <= k[:, None], 0.0, NEG)
    oh = np.zeros((128, 128), np.float32)
    oh[0:16, 0:16] = np.eye(16)
    oh[32:48, 16:32] = -np.eye(16)
    cst[:, 6, :] = oh
    rc16 = np.zeros((128, 4, 16), np.float32)
    for g, w in enumerate(POOL_WINDOWS):
        rc16[:, g, :] = 1.0 / np.minimum(np.arange(1, 17), w)
    return cst, rc16


_CACHE = {}


def run(inputs, T=SEQ, L=DEPTH, ncores=NCORES, dbg=False):
    inp = {k: np.asarray(v, np.float32) for k, v in inputs.items()}
    key = (T, L, dbg)
    if key not in _CACHE:
        _CACHE[key] = build(T, L, dbg)
    nc, _ = _CACHE[key]
    Ws, pps, rws = [], [], []
    for l in range(L):
        W, pp, rows = pack_layer(inp, l)
        Ws.append(W)
        pps.append(pp)
        rws.append(rows)
    W = np.stack(Ws)
    pp = np.stack(pps)
    rows = np.stack(rws)
    cst, rc16 = make_consts()
    fnw = np.ascontiguousarray(np.broadcast_to(inp["final_norm_w"][None, :], (128, D))).astype(np.float32)
    in_maps = []
    for b in range(ncores):
        in_maps.append({"x": np.ascontiguousarray(inp["x"][b, :T]), "W": W, "pp": pp, "rows": rows, "fnw": fnw, "cst": cst, "rc16": rc16})
    res = run_bass_kernel_spmd(nc, in_maps, core_ids=list(range(ncores)))
    if dbg:
        return res.results
    return np.stack([np.asarray(r["y"], np.float32) for r in res.results])


def kernel(**inputs):
    return run(inputs)
```

```python
import numpy as np
from contextlib import ExitStack
import concourse.bass as bass
import concourse.mybir as mybir
from concourse.bass_utils import run_bass_kernel_spmd

F32 = mybir.dt.float32
BF16 = mybir.dt.bfloat16
ALU = mybir.AluOpType
AF = mybir.ActivationFunctionType
AX = mybir.AxisListType

D = 1024
SEQ = 4096
DEPTH = 4
NCORES = 8
S = 512
NSLOT = 151
NPP = 88
EPS = 1e-6
SEM_LIMIT = 30000
NEG = -1.0e30
POOL_WINDOWS = (2, 4, 8, 16)


class Buf:
    __slots__ = ("w", "r")

    def __init__(self):
        self.w = None
        self.r = {}


class Ctr:
    def __init__(self, K, name, step):
        self.K, self.name, self.step = K, name, step
        self.n = 0
        self.h = K.new_sem(f"{name}_0")
        self.v = 0

    def next(self):
        if self.v + self.step > SEM_LIMIT:
            self.n += 1
            self.h = self.K.new_sem(f"{self.name}_{self.n}")
            self.v = 0
        self.v += self.step
        return (self.h, self.v)


class Eng:
    def __init__(self, K, name):
        self.name = name
        self.ops = []
        self.ctr = Ctr(K, "s_" + name, 1)
        self.seen = {}
        self.own = set()


class Kern:
    def __init__(self, nc, stack, n_dma_sems=12):
        self.nc = nc
        self.stack = stack
        self.nsem = 0
        self.E = {n: Eng(self, n) for n in ("pe", "act", "dve", "pool", "sp")}
        self.dma_ctrs = [Ctr(self, f"d{i}", 16) for i in range(n_dma_sems)]
        self.dma_rr = 0
        self.nops = 0

    def new_sem(self, name):
        self.nsem += 1
        return self.stack.enter_context(self.nc.semaphore(name))

    def _deps(self, e, r, w):
        deps = {}
        for b in r:
            if b.w is not None and deps.get(b.w[0], 0) < b.w[1]:
                deps[b.w[0]] = b.w[1]
        for b in w:
            if b.w is not None and deps.get(b.w[0], 0) < b.w[1]:
                deps[b.w[0]] = b.w[1]
            for h, v in b.r.items():
                if deps.get(h, 0) < v:
                    deps[h] = v
        waits = []
        for h, v in deps.items():
            if e.name == "pe" and h in e.own:
                continue
            if e.seen.get(h, 0) < v:
                waits.append((h, v))
                e.seen[h] = v
        return waits

    def _commit(self, tok, r, w):
        h, v = tok
        for b in w:
            b.w = tok
            b.r = {}
        for b in r:
            if b.w is not tok and b.r.get(h, 0) < v:
                b.r[h] = v

    def op(self, en, fn, r=(), w=()):
        e = self.E[en]
        waits = self._deps(e, r, w)
        tok = e.ctr.next()
        e.own.add(tok[0])
        e.ops.append((waits, fn, tok[0], 1))
        self._commit(tok, r, w)
        self.nops += 1

    def dma(self, en, out, in_, r=(), w=()):
        e = self.E[en]
        waits = self._deps(e, r, w)
        c = self.dma_ctrs[self.dma_rr]
        self.dma_rr = (self.dma_rr + 1) % len(self.dma_ctrs)
        if c.v > 0 and e.seen.get(c.h, 0) < c.v:
            waits.append((c.h, c.v))
            e.seen[c.h] = c.v
        tok = c.next()
        e.ops.append((waits, (lambda q: q.dma_start(out=out, in_=in_)), tok[0], 16))
        self._commit(tok, r, w)
        self.nops += 1

    def handoff(self, src, dst):
        u = {}
        for b in src:
            if b.w is not None and u.get(b.w[0], 0) < b.w[1]:
                u[b.w[0]] = b.w[1]
            for h, v in b.r.items():
                if u.get(h, 0) < v:
                    u[h] = v
        for b in dst:
            for h, v in u.items():
                if b.r.get(h, 0) < v:
                    b.r[h] = v

    def final_wait(self, en, bufs):
        e = self.E[en]
        waits = self._deps(e, bufs, ())
        e.ops.append((waits, None, None, 0))

    def emit(self):
        nc = self.nc
        E = self.E

        def run(q, e):
            for waits, fn, sem, inc in e.ops:
                for h, v in waits:
                    q.wait_ge(h, v)
                if fn is not None:
                    fn(q).then_inc(sem, inc)

        with nc.Block() as block:
            @block.tensor
            def _(q):
                run(q, E["pe"])

            @block.scalar
            def _(q):
                run(q, E["act"])

            @block.vector
            def _(q):
                run(q, E["dve"])

            @block.gpsimd
            def _(q):
                run(q, E["pool"])

            @block.sync
            def _(q):
                run(q, E["sp"])


def build(T=SEQ, L=DEPTH, dbg=False):
    NCH = T // 128
    NSC = T // S
    nc = bass.Bass("TRN2", target_bir_lowering=False)

    def din(name, shape):
        return nc.dram_tensor(name, shape, F32, kind="ExternalInput").ap()

    x_d = din("x", [T, D])
    W_d = din("W", [L, NSLOT, 128, 1024])
    pp_d = din("pp", [L, 128, NPP])
    rows_d = din("rows", [L, 128, 48])
    fnw_d = din("fnw", [128, D])
    cst_d = din("cst", [128, 8, 128])
    rc16_d = din("rc16", [128, 4, 16])
    y_d = nc.dram_tensor("y", [T, D], F32, kind="ExternalOutput").ap()
    hb_d = nc.dram_tensor("hbuf", [T, D], F32, kind="Internal").ap()

    with ExitStack() as st:
        K = Kern(nc, st)

        def sb(name, shape, dt=F32):
            return st.enter_context(nc.sbuf_tensor("sb_" + name, shape, dt))

        ps = st.enter_context(nc.psum_tensor("ps", [128, 8, 512], F32))
        PB = [Buf() for _ in range(8)]
        pbs = [0]

        def bank(excl=()):
            while True:
                i = pbs[0]
                pbs[0] = (i + 1) % 8
                if i not in excl:
                    return i

        def bank2(excl=()):
            while True:
                if pbs[0] % 2:
                    pbs[0] = (pbs[0] + 1) % 8
                i = pbs[0]
                pbs[0] = (i + 2) % 8
                if i not in excl and (i + 1) not in excl:
                    return i

        def bank4():
            i = 0 if pbs[0] <= 0 or pbs[0] > 4 else 4
            pbs[0] = (i + 4) % 8
            return i

        def psb(i):
            return ps[:, i, :].bitcast(BF16)

        B_dbg, B_dbgout = Buf(), Buf()
        dbgbuf = sb("dbgbuf", [128, 4096]) if dbg else None

        def dump(name, view, bufs, n):
            if not dbg:
                return
            K.op("pool", lambda q: q.tensor_copy(out=dbgbuf[:view.shape[0], 0:n], in_=view), r=bufs, w=[B_dbg])
            d = nc.dram_tensor("dbg_" + name, [view.shape[0], n], F32, kind="ExternalOutput").ap()
            K.dma("sp", d, dbgbuf[:view.shape[0], 0:n], r=[B_dbg], w=[B_dbgout])

        cst = sb("cst", [128, 8, 128])
        rc16 = sb("rc16", [128, 4, 16])
        identb = sb("identb", [128, 128], BF16)
        fnw = sb("fnw", [128, D])
        LHS = sb("LHS", [64, 16, 128])
        RHS = sb("RHS", [64, 128])
        Bc = Buf()
        identf = cst[:, 0, :]
        tri_le = cst[:, 1, :]
        tri_gt = cst[:, 2, :]
        onesf = cst[:, 3, :]
        maskneg = cst[:, 4, :]
        cmaskneg = cst[:, 5, :]
        oh = cst[:, 6, :]
        K.dma("sp", cst[:], cst_d, w=[Bc])
        K.dma("sp", rc16[:], rc16_d, w=[Bc])
        K.dma("sp", fnw[:], fnw_d, w=[Bc])
        K.op("dve", lambda q: q.tensor_copy(out=identb[:], in_=identf), r=[Bc], w=[Bc])
        identBIG = sb("identBIG", [128, 128], BF16)
        K.op("dve", lambda q: q.tensor_scalar(out=identBIG[:], in0=identf, scalar1=30000.0, scalar2=None, op0=ALU.mult), r=[Bc], w=[Bc])
        B_LHS, B_RHS = Buf(), Buf()
        K.op("pool", lambda q: q.memset(LHS[:], 0.0), w=[B_LHS])
        K.op("pool", lambda q: q.memset(RHS[:], 0.0), w=[B_RHS])
        K.op("dve", lambda q: q.tensor_copy(out=LHS[0:16, :, :], in_=oh[0:16, 0:16].unsqueeze(2).to_broadcast([16, 16, 128])), r=[Bc], w=[B_LHS])
        K.op("pool", lambda q: q.memset(RHS[32:48, :], 1.0), w=[B_RHS])

        pp = sb("pp", [128, NPP])
        rows = sb("rows", [128, 48])
        A_b = sb("A_b", [128, 16])
        B_par = Buf()
        hS = sb("hS", [128, 4, D])
        B_hS = Buf()
        xn_tok = [sb(f"xn_tok{i}", [128, D], BF16) for i in range(2)]
        B_xn = [Buf(), Buf()]
        junk = sb("junk", [128, D], BF16)
        B_junk = Buf()
        ssq = sb("ssq", [128, 8])
        B_ssq = Buf()
        xnT = sb("xnT", [128, 8, S], BF16)
        B_xnT = Buf()
        qT = sb("qT", [128, 4, S], BF16)
        B_qT = Buf()
        kT = sb("kT", [128, T], BF16)
        B_kT = Buf()
        qiT = sb("qiT", [128, 2, S], BF16)
        B_qiT = Buf()
        kiT = sb("kiT", [128, T], BF16)
        B_kiT = Buf()
        Vh = sb("Vh", [128, NCH, 128], BF16)
        B_V = Buf()
        dw = sb("dw", [128, 4, 20])
        B_dw = Buf()
        state = sb("state", [128, 16, 64])
        state_bf = sb("state_bf", [128, 16, 64], BF16)
        B_state, B_statebf = Buf(), Buf()
        chalo = sb("chalo", [128, 12, 3], BF16)
        B_chalo = Buf()
        phalo = sb("phalo", [128, 4, 15])
        B_phalo = Buf()
        ynT = sb("ynT", [128, 8, S], BF16)
        B_ynT = Buf()
        yattT = sb("yattT", [128, 4, S], BF16)
        B_yattT = Buf()
        ypoolT = sb("ypoolT", [128, 4, S], BF16)
        B_ypoolT = Buf()
        NS, NB = 3, 8
        wbf = [sb(f"wbf{i}", [128, 8, 128], BF16) for i in range(NB)]
        B_wst = [Buf() for _ in range(NS)]
        B_wbf = [Buf() for _ in range(NB)]
        upre = [sb(f"upre{i}", [128, 515], BF16) for i in range(2)]
        B_upre = [Buf(), Buf()]
        ubuf = [sb(f"ubuf{i}", [128, 527]) for i in range(2)]
        B_ubuf = [Buf(), Buf()]
        ptmp = [sb(f"ptmp{i}", [128, 527]) for i in range(2)]
        B_ptmp = [Buf(), Buf()]
        pooled = [sb(f"pooled{i}", [128, S], BF16) for i in range(2)]
        B_pooled = [Buf(), Buf()]
        small = sb("small", [128, 256])
        B_sm = {n: Buf() for n in ("dt", "a48", "e3", "dtdec", "m8", "thr", "rs", "rsum", "rinv")}
        bis = sb("bis", [128, 64])
        B_bis = {n: Buf() for n in ("D", "cnt", "u", "mid", "lo", "d0")}
        rsb = sb("rsb", [128, 8, 8])
        B_rsb = [Buf() for _ in range(8)]

        R1N = 15872
        R1 = sb("R1", [128, R1N])

        def carve(off_f32, nelem, dt):
            if dt == F32:
                return R1[:, off_f32:off_f32 + nelem]
            return R1[:, off_f32:off_f32 + nelem // 2].bitcast(BF16)

        o = 0
        xbc_act = carve(o, 12 * S, BF16).rearrange("p (j t) -> p j t", j=12); o += 6 * S
        zs = carve(o, 4 * D, BF16).rearrange("p (c d) -> p c d", c=4); o += 2 * D
        xs_tok = carve(o, 4 * D, BF16).rearrange("p (c d) -> p c d", c=4); o += 2 * D
        B_tok = carve(o, 4 * 256, BF16).rearrange("p (c d) -> p c d", c=4); o += 512
        ctmp = carve(o, S, F32); o += S
        x_dt = carve(o, D, BF16); o += D // 2
        xdec = carve(o, D, BF16); o += D // 2
        xsd = carve(o, D, BF16); o += D // 2
        Lexp = carve(o, D, BF16).rearrange("p (j t) -> p j t", j=8); o += D // 2
        Mt = carve(o, D, BF16).rearrange("p (j t) -> p j t", j=8); o += D // 2
        cb = carve(o, 128, F32); o += 128
        yacc = carve(o, D, F32); o += D
        ynb = carve(o, D, BF16); o += D // 2
        assert o <= R1N, o
        ssd_bufs = {n: Buf() for n in ("xbc_act", "zs", "xs_tok", "B_tok", "ctmp", "x_dt", "xdec", "xsd", "Lexp", "Mt", "cb", "yacc", "ynb")}
        o = 0
        score = carve(o, T, F32); o += T
        work = carve(o, T, F32); o += T
        rtmp = [carve(o + i * S, S, F32) for i in range(2)]; o += 2 * S
        Pm = [carve(o + i * (S // 2), S, BF16) for i in range(3)]; o += 3 * (S // 2)
        PmT = [carve(o + i * (S // 2), S, BF16) for i in range(3)]; o += 3 * (S // 2)
        yatt = carve(o, 512, BF16); o += 256
        mbias = [carve(o + i * (T // 2), T, BF16) for i in range(2)]; o += T
        assert o <= R1N, o
        att_bufs = {n: Buf() for n in ("score", "work", "rtmp0", "rtmp1", "Pm0", "Pm1", "Pm2", "PmT0", "PmT1", "PmT2", "yatt", "mb0", "mb1")}
        o = 0
        mergedT = carve(o, 8 * S, BF16).rearrange("p (j t) -> p j t", j=8); o += 4 * S
        sig = [carve(o + i * (S // 2), S, BF16) for i in range(3)]; o += 3 * (S // 2)
        mt = [carve(o + i * S, S, F32) for i in range(3)]; o += 3 * S
        assert o <= R1N, o
        mrg_bufs = {n: Buf() for n in ("mergedT", "sig0", "sig1", "sig2", "mt0", "mt1", "mt2")}
        o = 0
        hidT = carve(o, 22 * S, BF16).rearrange("p (j t) -> p j t", j=22); o += 11 * S
        sa = [carve(o + i * (S // 2), S, BF16) for i in range(2)]; o += S
        assert o <= R1N, o
        ffn_bufs = {n: Buf() for n in ("hidT", "sa0", "sa1")}

        Wb_d = nc.dram_tensor("Wb", [L, NSLOT, 128, 1024], BF16, kind="Internal").ap()
        B_wb = [[Buf() for _ in range(NSLOT)] for _ in range(L)]
        wst = [carve(i * 1024, 1024, F32).rearrange("p (k c) -> p k c", k=8) for i in range(NS)]
        cvt = [carve(NS * 1024 + i * 512, 1024, BF16) for i in range(4)]
        B_cvt = [Buf() for _ in range(4)]
        n_ = 0
        for l_ in range(L):
            for slot_ in range(NSLOT):
                s_, c_ = n_ % NS, n_ % 4
                K.dma("sp", wst[s_].rearrange("p k c -> p (k c)"), W_d[l_, slot_], w=[B_wst[s_]])
                if n_ % 2 == 0:
                    K.op("dve", lambda q, s_=s_, c_=c_: q.tensor_copy(out=cvt[c_], in_=wst[s_].rearrange("p k c -> p (k c)")), r=[B_wst[s_]], w=[B_cvt[c_]])
                else:
                    K.op("act", lambda q, s_=s_, c_=c_: q.activation(out=cvt[c_], in_=wst[s_].rearrange("p k c -> p (k c)"), func=AF.Copy), r=[B_wst[s_]], w=[B_cvt[c_]])
                K.dma("pool", Wb_d[l_, slot_], cvt[c_], r=[B_cvt[c_]], w=[B_wb[l_][slot_]])
                n_ += 1
        wctr = [0]
        cur = {"l": 0, "slot": 0}

        def wtile():
            i = wctr[0]
            wctr[0] += 1
            l, slot = cur["l"], cur["slot"]
            cur["slot"] += 1
            b_ = i % NB
            K.dma("sp", wbf[b_][:].rearrange("p k c -> p (k c)"), Wb_d[l, slot], r=[B_wb[l][slot]], w=[B_wbf[b_]])
            return wbf[b_], B_wbf[b_]

        def rms_T(nwoff, dstT, B_dst):
            for c in range(4):
                K.op("act", lambda q, c=c: q.activation(out=junk[:], in_=hS[:, c, :], func=AF.Square, accum_out=ssq[:, c:c + 1]),
                     r=[B_hS], w=[B_junk, B_ssq])
            K.op("act", lambda q: q.activation(out=ssq[:, 4:8], in_=ssq[:, 0:4], func=AF.Sqrt, scale=1.0 / D, bias=EPS), r=[B_ssq], w=[B_ssq])
            K.op("dve", lambda q: q.reciprocal(out=ssq[:, 4:8], in_=ssq[:, 4:8]), r=[B_ssq], w=[B_ssq])
            for c in range(4):
                xt, bx = xn_tok[c % 2], B_xn[c % 2]
                K.op("dve", lambda q, c=c, xt=xt: q.tensor_scalar(out=xt[:], in0=hS[:, c, :], scalar1=ssq[:, 4 + c:5 + c], scalar2=None, op0=ALU.mult),
                     r=[B_hS, B_ssq], w=[bx])
                for half in range(2):
                    b = bank()
                    for j in range(4):
                        kt = half * 4 + j
                        K.op("pe", lambda q, b=b, j=j, kt=kt, xt=xt: q.transpose(out=psb(b)[:, j * 128:(j + 1) * 128], in_=xt[:, kt * 128:(kt + 1) * 128], identity=identb[:]),
                             r=[bx, Bc], w=[PB[b]])
                    K.op("dve", lambda q, b=b, half=half, c=c: q.tensor_tensor(
                        out=dstT[:, half * 4:half * 4 + 4, c * 128:(c + 1) * 128],
                        in0=psb(b)[:, 0:512].rearrange("p (j t) -> p j t", j=4),
                        in1=pp[:, nwoff + half * 4:nwoff + half * 4 + 4].unsqueeze(2).to_broadcast([128, 4, 128]), op=ALU.mult),
                        r=[PB[b], B_par], w=[B_dst])

        def mm_fm(wt, bw, KT, rhsT, B_rhs, b, kt0=0, first=True, last=True, wkt0=0):
            for kt in range(KT):
                K.op("pe", lambda q, kt=kt: q.matmul(ps[:, b, :], lhsT=wt[:, wkt0 + kt, :], rhs=rhsT[:, kt0 + kt, :],
                                                    start=(first and kt == 0), stop=(last and kt == KT - 1)),
                     r=[bw, B_rhs], w=[PB[b]])

        def mm_tm(wt, bw, KT, lhsT_src, B_l, b, ncols=128, cstride=128, wkt0=0, kt0=0, first=True, last=True):
            for c in range(4):
                for kt in range(KT):
                    K.op("pe", lambda q, c=c, kt=kt: q.matmul(ps[:, b, c * cstride:c * cstride + ncols],
                                                              lhsT=lhsT_src[:, kt0 + kt, c * 128:(c + 1) * 128], rhs=wt[:, wkt0 + kt, 0:ncols],
                                                              start=(first and kt == 0), stop=(last and kt == KT - 1)),
                         r=[bw, B_l], w=[PB[b]])

        for l in range(L):
            src_d = x_d if l == 0 else hb_d
            last_layer = (l == L - 1)
            K.dma("sp", pp[:], pp_d[l], w=[B_par])
            K.dma("sp", rows[:], rows_d[l], w=[B_par])
            K.op("act", lambda q: q.activation(out=A_b[:], in_=rows[:, 16:32], func=AF.Exp), r=[B_par], w=[B_par])
            K.op("dve", lambda q: q.tensor_scalar(out=A_b[:], in0=A_b[:], scalar1=-1.0, scalar2=None, op0=ALU.mult), r=[B_par], w=[B_par])
            K.op("pool", lambda q: q.memset(state[:], 0.0), w=[B_state])
            K.op("pool", lambda q: q.memset(state_bf[:], 0.0), w=[B_statebf])
            K.op("pool", lambda q: q.memset(chalo[:], 0.0), w=[B_chalo])
            K.op("pool", lambda q: q.memset(phalo[:], 0.0), w=[B_phalo])

            for sc in range(NSC):
                t0 = sc * S
                cg0 = sc * 4
                cur["l"], cur["slot"] = l, 0
                SB = ssd_bufs
                K.dma("act", hS[:], src_d[t0:t0 + S, :].rearrange("(c p) d -> p c d", p=128), r=([B_hb[sc]] if l > 0 else []), w=[B_hS])
                rms_T(64, xnT, B_xnT)
                K.handoff(list(ffn_bufs.values()) + list(mrg_bufs.values()) + list(att_bufs.values()) + B_wst + B_cvt, list(SB.values()))
                dg = dbg and l == 0 and sc < 2
                if dg:
                    dump(f"xnT{sc}", xnT[:].rearrange("p k t -> p (k t)"), [B_xnT], 4096)

                wpool, bwpool = wtile()
                K.op("pool", lambda q, wpool=wpool: q.tensor_copy(out=junk[:, 0:512].rearrange("p (g c) -> p g c", g=4), in_=wpool[:, 0:4, :]), r=[bwpool], w=[B_junk])
                poolw = junk[:, 0:512].rearrange("p (g c) -> p g c", g=4)

                for j in range(12):
                    wt, bw = wtile()
                    b = bank()
                    mm_fm(wt, bw, 8, xnT, B_xnT, b)
                    up, bu = upre[j % 2], B_upre[j % 2]
                    K.op("act", lambda q, b=b, up=up: q.activation(out=up[:, 3:515], in_=ps[:, b, :], func=AF.Copy), r=[PB[b]], w=[bu])
                    K.op("pool", lambda q, j=j, up=up: q.tensor_copy(out=up[:, 0:3], in_=chalo[:, j, :]), r=[B_chalo], w=[bu])
                    K.op("dve", lambda q, j=j, up=up: q.tensor_scalar(out=ctmp, in0=up[:, 0:512], scalar1=pp[:, j * 4:j * 4 + 1], scalar2=None, op0=ALU.mult),
                         r=[bu, B_par], w=[SB["ctmp"]])
                    for k in range(1, 4):
                        K.op("dve", lambda q, j=j, k=k, up=up: q.scalar_tensor_tensor(out=ctmp, in0=up[:, k:k + 512], scalar=pp[:, j * 4 + k:j * 4 + k + 1], in1=ctmp, op0=ALU.mult, op1=ALU.add),
                             r=[bu, B_par, SB["ctmp"]], w=[SB["ctmp"]])
                    K.op("pool", lambda q, j=j, up=up: q.tensor_copy(out=chalo[:, j, :], in_=up[:, 512:515]), r=[bu], w=[B_chalo])
                    K.op("act", lambda q, j=j: q.activation(out=xbc_act[:, j, :], in_=ctmp, func=AF.Silu, bias=pp[:, 48 + j:49 + j]),
                         r=[SB["ctmp"], B_par], w=[SB["xbc_act"]])
                for c in range(4):
                    for half in range(2):
                        b = bank()
                        for j in range(4):
                            K.op("pe", lambda q, b=b, j=j, c=c, half=half: q.transpose(out=psb(b)[:, j * 128:(j + 1) * 128], in_=xbc_act[:, half * 4 + j, c * 128:(c + 1) * 128], identity=identb[:]),
                                 r=[SB["xbc_act"], Bc], w=[PB[b]])
                        K.op("act", lambda q, b=b, c=c, half=half: q.activation(out=xs_tok[:, c, half * 512:(half + 1) * 512], in_=psb(b)[:, 0:512], func=AF.Copy),
                             r=[PB[b]], w=[SB["xs_tok"]])
                    b = bank()
                    for g in range(2):
                        K.op("pe", lambda q, b=b, g=g, c=c: q.transpose(out=psb(b)[:, g * 128:(g + 1) * 128], in_=xbc_act[:, 8 + g, c * 128:(c + 1) * 128], identity=identb[:]),
                             r=[SB["xbc_act"], Bc], w=[PB[b]])
                    K.op("act", lambda q, b=b, c=c: q.activation(out=B_tok[:, c, :], in_=psb(b)[:, 0:256], func=AF.Copy), r=[PB[b]], w=[SB["B_tok"]])

                if dg:
                    dump(f"xbc{sc}a", xbc_act[:, 0:8, :].rearrange("p k t -> p (k t)"), [SB["xbc_act"]], 4096)
                    dump(f"xbc{sc}b", xbc_act[:, 8:12, :].rearrange("p k t -> p (k t)"), [SB["xbc_act"]], 2048)
                    dump(f"xstok{sc}", xs_tok.rearrange("p c d -> p (c d)"), [SB["xs_tok"]], 4096)
                for f in range(8):
                    wt, bw = wtile()
                    b = bank()
                    mm_tm(wt, bw, 8, xnT, B_xnT, b)
                    K.op("act", lambda q, b=b, f=f: q.activation(out=zs[:, :, f * 128:(f + 1) * 128], in_=ps[:, b, :].rearrange("p (c t) -> p c t", c=4), func=AF.Silu),
                         r=[PB[b]], w=[SB["zs"]])
                wt, bw = wtile()
                b = bank()
                mm_tm(wt, bw, 8, xnT, B_xnT, b, ncols=20, cstride=32)
                K.op("act", lambda q, b=b: q.activation(out=dw[:], in_=ps[:, b, 0:128].rearrange("p (c t) -> p c t", c=4)[:, :, 0:20], func=AF.Copy), r=[PB[b]], w=[B_dw])
                wt, bw = wtile()
                b = bank()
                mm_tm(wt, bw, 8, xnT, B_xnT, b)
                K.op("act", lambda q, b=b, cg0=cg0: q.activation(out=Vh[:, cg0:cg0 + 4, :], in_=ps[:, b, :].rearrange("p (c t) -> p c t", c=4), func=AF.Copy), r=[PB[b]], w=[B_V])
                for i in range(4):
                    wt, bw = wtile()
                    b = bank()
                    mm_fm(wt, bw, 8, xnT, B_xnT, b)
                    K.op("act", lambda q, b=b, i=i: q.activation(out=qT[:, i, :], in_=ps[:, b, :], func=AF.Copy, scale=0.125), r=[PB[b]], w=[B_qT])
                wt, bw = wtile()
                b = bank()
                mm_fm(wt, bw, 8, xnT, B_xnT, b)
                K.op("act", lambda q, b=b, t0=t0: q.activation(out=kT[:, t0:t0 + S], in_=ps[:, b, :], func=AF.Copy), r=[PB[b]], w=[B_kT])
                for i in range(2):
                    wt, bw = wtile()
                    b = bank()
                    mm_fm(wt, bw, 8, xnT, B_xnT, b)
                    K.op("act", lambda q, b=b, i=i: q.activation(out=qiT[:, i, :], in_=ps[:, b, :], func=AF.Copy, scale=0.0625), r=[PB[b]], w=[B_qiT])
                wt, bw = wtile()
                b = bank()
                mm_fm(wt, bw, 8, xnT, B_xnT, b)
                K.op("act", lambda q, b=b, t0=t0: q.activation(out=kiT[:, t0:t0 + S], in_=ps[:, b, :], func=AF.Copy), r=[PB[b]], w=[B_kiT])

                for g in range(4):
                    win = POOL_WINDOWS[g]
                    wt, bw = wtile()
                    b = bank()
                    mm_fm(wt, bw, 8, xnT, B_xnT, b)
                    ub, bub = ubuf[g % 2], B_ubuf[g % 2]
                    K.op("act", lambda q, b=b, ub=ub: q.activation(out=ub[:, 15:527], in_=ps[:, b, :], func=AF.Copy), r=[PB[b]], w=[bub])
                    K.op("pool", lambda q, g=g, ub=ub: q.tensor_copy(out=ub[:, 0:15], in_=phalo[:, g, :]), r=[B_phalo], w=[bub])
                    srcb, bsrc = ub, bub
                    lvl = 1
                    pi = 0
                    while lvl < win:
                        dst, bdst = ptmp[pi], B_ptmp[pi]
                        lo = 2 * lvl - 1
                        K.op("dve", lambda q, srcb=srcb, dst=dst, lo=lo, lvl=lvl: q.tensor_tensor(out=dst[:, lo:527], in0=srcb[:, lo:527], in1=srcb[:, lo - lvl:527 - lvl], op=ALU.add),
                             r=[bsrc], w=[bdst])
                        srcb, bsrc = dst, bdst
                        pi ^= 1
                        lvl *= 2
                    pl, bpl = pooled[g % 2], B_pooled[g % 2]
                    if sc == 0:
                        K.op("dve", lambda q, srcb=srcb, g=g: q.tensor_tensor(out=srcb[:, 15:31], in0=srcb[:, 15:31], in1=rc16[:, g, :], op=ALU.mult), r=[bsrc, Bc], w=[bsrc])
                        K.op("dve", lambda q, srcb=srcb, ub=ub, pl=pl: q.tensor_tensor(out=pl[:, 0:16], in0=srcb[:, 15:31], in1=ub[:, 15:31], op=ALU.subtract), r=[bsrc, bub], w=[bpl])
                        K.op("dve", lambda q, srcb=srcb, ub=ub, pl=pl, win=win: q.scalar_tensor_tensor(out=pl[:, 16:512], in0=srcb[:, 31:527], scalar=1.0 / win, in1=ub[:, 31:527], op0=ALU.mult, op1=ALU.subtract),
                             r=[bsrc, bub], w=[bpl])
                    else:
                        K.op("dve", lambda q, srcb=srcb, ub=ub, pl=pl, win=win: q.scalar_tensor_tensor(out=pl[:, 0:512], in0=srcb[:, 15:527], scalar=1.0 / win, in1=ub[:, 15:527], op0=ALU.mult, op1=ALU.subtract),
                             r=[bsrc, bub], w=[bpl])
                    K.op("pool", lambda q, g=g, ub=ub: q.tensor_copy(out=phalo[:, g, :], in_=ub[:, 512:527]), r=[bub], w=[B_phalo])
                    b2 = bank()
                    K.op("pe", lambda q, b2=b2, g=g, pl=pl: q.matmul(ps[:, b2, :], lhsT=poolw[:, g, :], rhs=pl[:], start=True, stop=True), r=[B_junk, bpl], w=[PB[b2]])
                    K.op("act", lambda q, b2=b2, g=g: q.activation(out=ypoolT[:, g, :], in_=ps[:, b2, :], func=AF.Copy, scale=pp[:, 60 + g:61 + g]), r=[PB[b2], B_par], w=[B_ypoolT])

                if dg:
                    dump(f"zs{sc}", zs.rearrange("p c d -> p (c d)"), [SB["zs"]], 4096)
                    dump(f"dw{sc}", dw[:].rearrange("p c d -> p (c d)"), [B_dw], 80)
                    dump(f"ypoolT{sc}", ypoolT[:].rearrange("p k t -> p (k t)"), [B_ypoolT], 2048)
                    dump(f"qT{sc}", qT[:].rearrange("p k t -> p (k t)"), [B_qT], 2048)
                sm = small
                for c in range(4):
                    cc = slice(c * 128, (c + 1) * 128)
                    K.op("dve", lambda q, c=c: q.tensor_tensor(out=sm[:, 0:16], in0=dw[:, c, 0:16], in1=rows[:, 0:16], op=ALU.add), r=[B_dw, B_par], w=[B_sm["dt"]])
                    K.op("act", lambda q: q.activation(out=sm[:, 0:16], in_=sm[:, 0:16], func=AF.Exp), r=[B_sm["dt"]], w=[B_sm["dt"]])
                    K.op("act", lambda q: q.activation(out=sm[:, 0:16], in_=sm[:, 0:16], func=AF.Ln, bias=1.0), r=[B_sm["dt"]], w=[B_sm["dt"]])
                    K.op("pool", lambda q: q.memset(sm[:, 32:80], 0.0), w=[B_sm["a48"]])
                    K.op("dve", lambda q: q.tensor_tensor(out=sm[:, 32:48], in0=sm[:, 0:16], in1=A_b[:], op=ALU.mult), r=[B_sm["dt"], B_par], w=[B_sm["a48"]])
                    K.op("dve", lambda q: q.tensor_tensor(out=sm[:, 64:80], in0=sm[:, 0:16], in1=A_b[:], op=ALU.mult), r=[B_sm["dt"], B_par], w=[B_sm["a48"]])
                    bs = bank()
                    a16 = sm[:, 32:48]
                    K.op("pe", lambda q, bs=bs: q.matmul(ps[:, bs, 0:16], lhsT=tri_le, rhs=a16, start=True, stop=True), r=[Bc, B_sm["a48"]], w=[PB[bs]])
                    K.op("pe", lambda q, bs=bs: q.matmul(ps[:, bs, 16:32], lhsT=tri_gt, rhs=a16, start=True, stop=True), r=[Bc, B_sm["a48"]], w=[PB[bs]])
                    K.op("pe", lambda q, bs=bs: q.matmul(ps[:, bs, 32:48], lhsT=onesf, rhs=a16, start=True, stop=True), r=[Bc, B_sm["a48"]], w=[PB[bs]])
                    K.op("act", lambda q, bs=bs: q.activation(out=sm[:, 80:128], in_=ps[:, bs, 0:48], func=AF.Exp), r=[PB[bs]], w=[B_sm["e3"]])
                    bt = bank()
                    K.op("pe", lambda q, bt=bt: q.matmul(ps[0:48, bt, 0:128], lhsT=sm[:, 32:80], rhs=tri_le, start=True, stop=True), r=[Bc, B_sm["a48"]], w=[PB[bt]])
                    K.op("act", lambda q, bt=bt: q.activation(out=RHS[0:16, :], in_=ps[0:16, bt, 0:128], func=AF.Copy), r=[PB[bt]], w=[B_RHS])
                    K.op("dve", lambda q, bt=bt: q.tensor_tensor(out=LHS[32:48, :, :], in0=ps[32:48, bt, 0:128].unsqueeze(1).to_broadcast([16, 16, 128]),
                                                                  in1=oh[32:48, 16:32].unsqueeze(2).to_broadcast([16, 16, 128]), op=ALU.mult),
                         r=[PB[bt], Bc], w=[B_LHS])
                    xs3 = xs_tok[:, c, :].rearrange("p (h d) -> p h d", h=16)
                    K.op("dve", lambda q: q.tensor_tensor(out=sm[:, 128:144], in0=sm[:, 0:16], in1=sm[:, 96:112], op=ALU.mult), r=[B_sm["dt"], B_sm["e3"]], w=[B_sm["dtdec"]])
                    K.op("dve", lambda q, xs3=xs3: q.tensor_tensor(out=x_dt.rearrange("p (h d) -> p h d", h=16), in0=xs3, in1=sm[:, 0:16].unsqueeze(2).to_broadcast([128, 16, 64]), op=ALU.mult),
                         r=[SB["xs_tok"], B_sm["dt"]], w=[SB["x_dt"]])
                    K.op("dve", lambda q, xs3=xs3: q.tensor_tensor(out=xdec.rearrange("p (h d) -> p h d", h=16), in0=xs3, in1=sm[:, 128:144].unsqueeze(2).to_broadcast([128, 16, 64]), op=ALU.mult),
                         r=[SB["xs_tok"], B_sm["dtdec"]], w=[SB["xdec"]])
                    K.op("dve", lambda q, xs3=xs3: q.tensor_tensor(out=xsd.rearrange("p (h d) -> p h d", h=16), in0=xs3, in1=rows[:, 32:48].unsqueeze(2).to_broadcast([128, 16, 64]), op=ALU.mult),
                         r=[SB["xs_tok"], B_par], w=[SB["xsd"]])
                    by = bank2()
                    live = (by, by + 1)
                    for g in range(2):
                        bcb = bank(live)
                        K.op("pe", lambda q, bcb=bcb, g=g, cc=cc: q.matmul(ps[:, bcb, 0:128], lhsT=xbc_act[:, 8 + g, cc], rhs=xbc_act[:, 10 + g, cc], start=True, stop=True),
                             r=[SB["xbc_act"]], w=[PB[bcb]])
                        K.op("act", lambda q, bcb=bcb: q.activation(out=cb, in_=ps[:, bcb, 0:128], func=AF.Copy), r=[PB[bcb]], w=[SB["cb"]])
                        be = bank2(live)
                        for j in range(8):
                            h = g * 8 + j
                            bb_, off = be + j // 4, (j % 4) * 128
                            K.op("pe", lambda q, bb_=bb_, off=off, h=h: q.matmul(ps[:, bb_, off:off + 128], lhsT=LHS[:, h, :], rhs=RHS[:], start=True, stop=False),
                                 r=[B_LHS, B_RHS], w=[PB[bb_]])
                            K.op("pe", lambda q, bb_=bb_, off=off: q.matmul(ps[:, bb_, off:off + 128], lhsT=identf, rhs=maskneg, start=False, stop=True),
                                 r=[Bc], w=[PB[bb_]])
                        K.op("act", lambda q, be=be: q.activation(out=Lexp, in_=ps[:, be:be + 2, :].rearrange("p b (j t) -> p (b j) t", j=4), func=AF.Exp),
                             r=[PB[be], PB[be + 1]], w=[SB["Lexp"]])
                        K.op("dve", lambda q: q.tensor_tensor(out=Mt, in0=Lexp, in1=cb.unsqueeze(1).to_broadcast([128, 8, 128]), op=ALU.mult),
                             r=[SB["Lexp"], SB["cb"]], w=[SB["Mt"]])
                        for j in range(8):
                            h = g * 8 + j
                            hb_, hoff = h // 8, (h % 8) * 64
                            K.op("pe", lambda q, j=j, hb_=hb_, hoff=hoff, h=h, by=by: q.matmul(ps[:, by + hb_, hoff:hoff + 64], lhsT=Mt[:, j, :], rhs=x_dt[:, h * 64:(h + 1) * 64], start=True, stop=False),
                                 r=[SB["Mt"], SB["x_dt"]], w=[PB[by + hb_]])
                            K.op("pe", lambda q, hb_=hb_, hoff=hoff, h=h, by=by: q.matmul(ps[:, by + hb_, hoff:hoff + 64], lhsT=identb[:], rhs=xsd[:, h * 64:(h + 1) * 64], start=False, stop=True),
                                 r=[Bc, SB["xsd"]], w=[PB[by + hb_]])
                    bo = bank2(live)
                    for h in range(16):
                        g = h // 8
                        hb_, hoff = h // 8, (h % 8) * 64
                        K.op("pe", lambda q, hb_=hb_, hoff=hoff, h=h, g=g, cc=cc, bo=bo: q.matmul(ps[:, bo + hb_, hoff:hoff + 64], lhsT=xbc_act[:, 10 + g, cc], rhs=state_bf[:, h, :], start=True, stop=True),
                             r=[SB["xbc_act"], B_statebf], w=[PB[bo + hb_]])
                    bsn = bank2(live + (bo, bo + 1))
                    for h in range(16):
                        g = h // 8
                        hb_, hoff = h // 8, (h % 8) * 64
                        K.op("pe", lambda q, hb_=hb_, hoff=hoff, h=h, g=g, c=c, bsn=bsn: q.matmul(ps[:, bsn + hb_, hoff:hoff + 64], lhsT=B_tok[:, c, g * 128:(g + 1) * 128], rhs=xdec[:, h * 64:(h + 1) * 64], start=True, stop=True),
                             r=[SB["B_tok"], SB["xdec"]], w=[PB[bsn + hb_]])
                    cd_b = sm[:, 112:128].unsqueeze(2).to_broadcast([128, 16, 64])
                    K.op("dve", lambda q, cd_b=cd_b: q.tensor_tensor(out=state[:], in0=state[:], in1=cd_b, op=ALU.mult), r=[B_state, B_sm["e3"]], w=[B_state])
                    K.op("dve", lambda q, bsn=bsn: q.tensor_tensor(out=state[:].rearrange("p (b h) d -> p b (h d)", b=2), in0=ps[:, bsn:bsn + 2, :], in1=state[:].rearrange("p (b h) d -> p b (h d)", b=2), op=ALU.add),
                         r=[PB[bsn], PB[bsn + 1], B_state], w=[B_state])
                    K.op("pool", lambda q: q.tensor_copy(out=state_bf[:], in_=state[:]), r=[B_state], w=[B_statebf])
                    ea_b = sm[:, 80:96].unsqueeze(2).to_broadcast([128, 16, 64])
                    K.op("dve", lambda q, bo=bo, ea_b=ea_b: q.tensor_tensor(out=yacc.rearrange("p (h d) -> p h d", h=16), in0=ps[:, bo:bo + 2, :].rearrange("p b (h d) -> p (b h) d", h=8), in1=ea_b, op=ALU.mult),
                         r=[PB[bo], PB[bo + 1], B_sm["e3"]], w=[SB["yacc"]])
                    K.op("dve", lambda q, by=by: q.tensor_tensor(out=yacc.rearrange("p (b f) -> p b f", b=2), in0=ps[:, by:by + 2, :], in1=yacc.rearrange("p (b f) -> p b f", b=2), op=ALU.add),
                         r=[PB[by], PB[by + 1], SB["yacc"]], w=[SB["yacc"]])
                    if dg:
                        dump(f"y{sc}_{c}", yacc, [SB["yacc"]], 1024)
                        dump(f"sm{sc}_{c}", sm[:, 0:128], [B_sm["dt"], B_sm["e3"], B_sm["a48"]], 128)
                    K.op("dve", lambda q, c=c: q.tensor_tensor(out=yacc, in0=yacc, in1=zs[:, c, :], op=ALU.mult), r=[SB["yacc"], SB["zs"]], w=[SB["yacc"]])
                    K.op("act", lambda q: q.activation(out=junk[:], in_=yacc, func=AF.Square, accum_out=sm[:, 169:170]), r=[SB["yacc"]], w=[B_junk, B_sm["rs"]])
                    K.op("act", lambda q: q.activation(out=sm[:, 170:171], in_=sm[:, 169:170], func=AF.Sqrt, scale=1.0 / D, bias=EPS), r=[B_sm["rs"]], w=[B_sm["rs"]])
                    K.op("dve", lambda q: q.reciprocal(out=sm[:, 170:171], in_=sm[:, 170:171]), r=[B_sm["rs"]], w=[B_sm["rs"]])
                    K.op("dve", lambda q: q.tensor_scalar(out=ynb, in0=yacc, scalar1=sm[:, 170:171], scalar2=None, op0=ALU.mult), r=[SB["yacc"], B_sm["rs"]], w=[SB["ynb"]])
                    for half in range(2):
                        b = bank()
                        for j in range(4):
                            kt = half * 4 + j
                            K.op("pe", lambda q, b=b, j=j, kt=kt: q.transpose(out=psb(b)[:, j * 128:(j + 1) * 128], in_=ynb[:, kt * 128:(kt + 1) * 128], identity=identb[:]),
                                 r=[SB["ynb"], Bc], w=[PB[b]])
                        K.op("dve", lambda q, b=b, half=half, cc=cc: q.tensor_tensor(
                            out=ynT[:, half * 4:half * 4 + 4, cc], in0=psb(b)[:, 0:512].rearrange("p (j t) -> p j t", j=4),
                            in1=pp[:, 72 + half * 4:72 + half * 4 + 4].unsqueeze(2).to_broadcast([128, 4, 128]), op=ALU.mult),
                            r=[PB[b], B_par], w=[B_ynT])
                AB = att_bufs
                K.handoff(list(SB.values()), list(AB.values()))

                def idx_topk(c):
                    cg = cg0 + c
                    nk = (cg + 1) * 128
                    nb = (nk + S - 1) // S
                    cc = slice(c * 128, (c + 1) * 128)
                    for kb in range(nb):
                        w_ = min(S, nk - kb * S)
                        ks = slice(kb * S, kb * S + w_)
                        for h in range(4):
                            b = bank()
                            half, ti = h // 2, h % 2
                            prt = slice(half * 64, half * 64 + 64)
                            K.op("pe", lambda q, b=b, prt=prt, ti=ti, ks=ks, w_=w_, cc=cc: q.matmul(ps[:, b, 0:w_], lhsT=qiT[prt, ti, cc], rhs=kiT[prt, ks], start=True, stop=True),
                                 r=[B_qiT, B_kiT], w=[PB[b]])
                            rt, brt = rtmp[h % 2], AB[f"rtmp{h % 2}"]
                            K.op("act", lambda q, b=b, rt=rt, w_=w_: q.activation(out=rt[:, 0:w_], in_=ps[:, b, 0:w_], func=AF.Relu), r=[PB[b]], w=[brt])
                            if h == 0:
                                K.op("dve", lambda q, rt=rt, ks=ks, w_=w_, c=c: q.tensor_scalar(out=score[:, ks], in0=rt[:, 0:w_], scalar1=dw[:, c, 16:17], scalar2=None, op0=ALU.mult),
                                     r=[brt, B_dw], w=[AB["score"]])
                            else:
                                K.op("dve", lambda q, rt=rt, ks=ks, w_=w_, c=c, h=h: q.scalar_tensor_tensor(out=score[:, ks], in0=rt[:, 0:w_], scalar=dw[:, c, 16 + h:17 + h], in1=score[:, ks], op0=ALU.mult, op1=ALU.add),
                                     r=[brt, B_dw, AB["score"]], w=[AB["score"]])
                    K.op("dve", lambda q, nk=nk: q.tensor_tensor(out=score[:, nk - 128:nk], in0=score[:, nk - 128:nk], in1=cmaskneg, op=ALU.add), r=[AB["score"], Bc], w=[AB["score"]])
                    if cg >= 2:
                        NIT = 26
                        nlo = cg * 128
                        K.op("dve", lambda q, nk=nk: q.max(out=sm[:, 144:152], in_=score[:, 0:nk]), r=[AB["score"]], w=[B_sm["m8"]])
                        K.op("dve", lambda q, nlo=nlo: q.tensor_reduce(out=bis[:, 59:60], in_=score[:, 0:nlo], axis=AX.X, op=ALU.min), r=[AB["score"]], w=[B_bis["lo"]])
                        K.op("dve", lambda q: q.tensor_tensor(out=bis[:, 60:61], in0=sm[:, 144:145], in1=bis[:, 59:60], op=ALU.subtract), r=[B_sm["m8"], B_bis["lo"]], w=[B_bis["d0"]])
                        K.op("dve", lambda q: q.tensor_scalar(out=bis[:, 0:56], in0=cst[:, 7, 0:56], scalar1=bis[:, 60:61], scalar2=None, op0=ALU.mult), r=[Bc, B_bis["d0"]], w=[B_bis["D"]])
                        K.op("dve", lambda q: q.tensor_tensor(out=bis[:, 58:59], in0=bis[:, 59:60], in1=bis[:, 0:1], op=ALU.add), r=[B_bis["lo"], B_bis["D"]], w=[B_bis["mid"]])
                        for it in range(NIT):
                            K.op("dve", lambda q, nk=nk: q.tensor_scalar(out=work[:, 0:nk], in0=score[:, 0:nk], scalar1=bis[:, 58:59], scalar2=None, op0=ALU.is_ge, op1=ALU.add, accum_out=bis[:, 56:57]),
                                 r=[AB["score"], B_bis["mid"]], w=[AB["work"], B_bis["cnt"]])
                            K.op("dve", lambda q, it=it: q.scalar_tensor_tensor(out=bis[:, 57:58], in0=bis[:, 56:57], scalar=255.5, in1=bis[:, it:it + 1], op0=ALU.is_ge, op1=ALU.mult),
                                 r=[B_bis["cnt"], B_bis["D"]], w=[B_bis["u"]])
                            K.op("dve", lambda q, it=it: q.scalar_tensor_tensor(out=bis[:, 58:59], in0=bis[:, 57:58], scalar=bis[:, 28 + it + 1:28 + it + 2], in1=bis[:, 58:59], op0=ALU.add, op1=ALU.add),
                                 r=[B_bis["u"], B_bis["D"], B_bis["mid"]], w=[B_bis["mid"]])
                        K.op("dve", lambda q: q.tensor_tensor(out=sm[:, 152:153], in0=bis[:, 58:59], in1=bis[:, 28 + NIT:28 + NIT + 1], op=ALU.add), r=[B_bis["mid"], B_bis["D"]], w=[B_sm["thr"]])
                    else:
                        K.op("dve", lambda q: q.memset(sm[:, 152:153], -1.0e29), w=[B_sm["thr"]])
                    mb = mbias[c % 2]
                    K.op("dve", lambda q, mb=mb, nk=nk: q.tensor_scalar(out=mb[:, 0:nk], in0=score[:, 0:nk], scalar1=sm[:, 152:153], scalar2=1.0, op0=ALU.is_ge, op1=ALU.subtract),
                         r=[AB["score"], B_sm["thr"]], w=[AB[f"mb{c % 2}"]])

                def heads(c):
                    cg = cg0 + c
                    nk = (cg + 1) * 128
                    nb = (nk + S - 1) // S
                    cc = slice(c * 128, (c + 1) * 128)
                    mb, bmb = mbias[c % 2], AB[f"mb{c % 2}"]
                    bO = bank()
                    K.op("pool", lambda q: q.memset(rsb[:], 0.0), w=B_rsb)
                    items = [(h, kb) for h in range(8) for kb in range(nb)]

                    def stA(i):
                        h, kb = items[i]
                        g, ti = h // 4, h % 4
                        prt = slice(g * 64, g * 64 + 64)
                        w_ = min(S, nk - kb * S)
                        ks = slice(kb * S, kb * S + w_)
                        b = bank((bO,))
                        i3 = i % 3
                        K.op("pe", lambda q: q.matmul(ps[:, b, 0:w_], lhsT=qT[prt, ti, cc], rhs=kT[prt, ks], start=True, stop=False),
                             r=[B_qT, B_kT], w=[PB[b]])
                        K.op("pe", lambda q: q.matmul(ps[:, b, 0:w_], lhsT=identBIG[:], rhs=mb[:, ks], start=False, stop=True),
                             r=[Bc, bmb], w=[PB[b]])
                        K.op("act", lambda q: q.activation(out=Pm[i3][:, 0:w_], in_=ps[:, b, 0:w_], func=AF.Exp, accum_out=rsb[:, h, kb:kb + 1]),
                             r=[PB[b]], w=[AB[f"Pm{i3}"], B_rsb[i % 8]])

                    def stB(i):
                        h, kb = items[i]
                        g = h // 4
                        w_ = min(S, nk - kb * S)
                        nsub = w_ // 128
                        i3 = i % 3
                        b2 = bank((bO,))
                        for ii in range(nsub):
                            K.op("pe", lambda q, ii=ii: q.transpose(out=psb(b2)[:, ii * 128:(ii + 1) * 128], in_=Pm[i3][:, ii * 128:(ii + 1) * 128], identity=identb[:]),
                                 r=[AB[f"Pm{i3}"], Bc], w=[PB[b2]])
                        K.op("act", lambda q: q.activation(out=PmT[i3][:, 0:w_], in_=psb(b2)[:, 0:w_], func=AF.Copy), r=[PB[b2]], w=[AB[f"PmT{i3}"]])
                        for ii in range(nsub):
                            kc = kb * 4 + ii
                            K.op("pe", lambda q, ii=ii, kc=kc, first=(kb == 0 and ii == 0), last=(kb == nb - 1 and ii == nsub - 1):
                                 q.matmul(ps[:, bO, h * 64:(h + 1) * 64], lhsT=PmT[i3][:, ii * 128:(ii + 1) * 128], rhs=Vh[:, kc, g * 64:(g + 1) * 64], start=first, stop=last),
                                 r=[AB[f"PmT{i3}"], B_V], w=[PB[bO]])

                    stA(0)
                    for i in range(len(items)):
                        if i + 1 < len(items):
                            stA(i + 1)
                        stB(i)
                    K.op("dve", lambda q: q.tensor_reduce(out=sm[:, 153:161], in_=rsb[:], axis=AX.X, op=ALU.add), r=B_rsb, w=[B_sm["rinv"]])
                    K.op("dve", lambda q: q.reciprocal(out=sm[:, 161:169], in_=sm[:, 153:161]), r=[B_sm["rinv"]], w=[B_sm["rinv"]])
                    K.op("dve", lambda q: q.tensor_tensor(out=yatt.rearrange("p (h d) -> p h d", h=8), in0=ps[:, bO, :].rearrange("p (h d) -> p h d", h=8),
                                                          in1=sm[:, 161:169].unsqueeze(2).to_broadcast([128, 8, 64]), op=ALU.mult),
                         r=[PB[bO], B_sm["rinv"]], w=[AB["yatt"]])
                    b = bank()
                    for j in range(4):
                        K.op("pe", lambda q, j=j: q.transpose(out=psb(b)[:, j * 128:(j + 1) * 128], in_=yatt[:, j * 128:(j + 1) * 128], identity=identb[:]), r=[AB["yatt"], Bc], w=[PB[b]])
                    K.op("act", lambda q: q.activation(out=yattT[:, :, cc], in_=psb(b)[:, 0:512].rearrange("p (j t) -> p j t", j=4), func=AF.Copy), r=[PB[b]], w=[B_yattT])

                idx_topk(0)
                for c in range(4):
                    if c + 1 < 4:
                        idx_topk(c + 1)
                    heads(c)

                if dg:
                    dump(f"ynT{sc}", ynT[:].rearrange("p k t -> p (k t)"), [B_ynT], 4096)
                    dump(f"yattT{sc}", yattT[:].rearrange("p k t -> p (k t)"), [B_yattT], 2048)
                MB = mrg_bufs
                K.handoff(list(AB.values()), list(MB.values()))
                for f in range(8):
                    w1, bw1 = wtile()
                    b1 = bank()
                    mm_fm(w1, bw1, 8, ynT, B_ynT, b1)
                    w2, bw2 = wtile()
                    b2 = bank()
                    mm_fm(w2, bw2, 4, yattT, B_yattT, b2)
                    b3 = bank()
                    mm_fm(w2, bw2, 4, ypoolT, B_ypoolT, b3, wkt0=4)
                    ups = [b1, b2, b3]
                    for br in range(3):
                        wg, bwg = wtile()
                        bg = bank()
                        mm_fm(wg, bwg, 8, xnT, B_xnT, bg)
                        K.op("act", lambda q, bg=bg, br=br: q.activation(out=sig[br], in_=ps[:, bg, :], func=AF.Sigmoid), r=[PB[bg]], w=[MB[f"sig{br}"]])
                        K.op("dve", lambda q, br=br, bu_=ups[br]: q.tensor_tensor(out=mt[br], in0=ps[:, bu_, :], in1=sig[br], op=ALU.mult), r=[PB[ups[br]], MB[f"sig{br}"]], w=[MB[f"mt{br}"]])
                    K.op("pool", lambda q: q.tensor_tensor(out=mt[0], in0=mt[0], in1=mt[1], op=ALU.add), r=[MB["mt0"], MB["mt1"]], w=[MB["mt0"]])
                    K.op("pool", lambda q, f=f: q.tensor_tensor(out=mergedT[:, f, :], in0=mt[0], in1=mt[2], op=ALU.add), r=[MB["mt0"], MB["mt2"]], w=[MB["mergedT"]])
                for f in range(8):
                    wt, bw = wtile()
                    b = bank()
                    mm_tm(wt, bw, 8, mergedT, MB["mergedT"], b)
                    K.op("dve", lambda q, b=b, f=f: q.tensor_tensor(out=hS[:, :, f * 128:(f + 1) * 128], in0=ps[:, b, :].rearrange("p (c t) -> p c t", c=4), in1=hS[:, :, f * 128:(f + 1) * 128], op=ALU.add),
                         r=[PB[b], B_hS], w=[B_hS])

                if dg:
                    dump(f"mergedT{sc}", mergedT.rearrange("p k t -> p (k t)"), [MB["mergedT"]], 4096)
                    dump(f"h1_{sc}", hS[:].rearrange("p c d -> p (c d)"), [B_hS], 4096)
                FB = ffn_bufs
                rms_T(80, xnT, B_xnT)
                K.handoff(list(MB.values()), list(FB.values()))
                for j in range(22):
                    wa, bwa = wtile()
                    ba = bank()
                    mm_fm(wa, bwa, 8, xnT, B_xnT, ba)
                    K.op("act", lambda q, ba=ba, j=j: q.activation(out=sa[j % 2], in_=ps[:, ba, :], func=AF.Silu), r=[PB[ba]], w=[FB[f"sa{j % 2}"]])
                    wb_, bwb = wtile()
                    bb = bank()
                    mm_fm(wb_, bwb, 8, xnT, B_xnT, bb)
                    K.op("dve", lambda q, bb=bb, j=j: q.tensor_tensor(out=hidT[:, j, :], in0=ps[:, bb, :], in1=sa[j % 2], op=ALU.mult), r=[PB[bb], FB[f"sa{j % 2}"]], w=[FB["hidT"]])
                for f in range(8):
                    b4 = bank4()
                    for part in range(3):
                        wt, bw = wtile()
                        nkt = 8 if part < 2 else 6
                        for c in range(4):
                            for kt in range(nkt):
                                K.op("pe", lambda q, c=c, kt=kt, part=part, wt=wt, b4=b4, nkt=nkt: q.matmul(ps[:, b4 + c, 0:128], lhsT=hidT[:, part * 8 + kt, c * 128:(c + 1) * 128], rhs=wt[:, kt, :],
                                                                                                 start=(part == 0 and kt == 0), stop=(part == 2 and kt == nkt - 1)),
                                     r=[bw, FB["hidT"]], w=[PB[b4 + c]])
                    K.op("dve", lambda q, b4=b4, f=f: q.tensor_tensor(out=hS[:, :, f * 128:(f + 1) * 128], in0=ps[:, b4:b4 + 4, 0:128], in1=hS[:, :, f * 128:(f + 1) * 128], op=ALU.add),
                         r=[PB[b4], PB[b4 + 1], PB[b4 + 2], PB[b4 + 3], B_hS], w=[B_hS])
                assert cur["slot"] == NSLOT, cur["slot"]

                if last_layer:
                    for c in range(4):
                        K.op("act", lambda q, c=c: q.activation(out=junk[:], in_=hS[:, c, :], func=AF.Square, accum_out=ssq[:, c:c + 1]), r=[B_hS], w=[B_junk, B_ssq])
                    K.op("act", lambda q: q.activation(out=ssq[:, 4:8], in_=ssq[:, 0:4], func=AF.Sqrt, scale=1.0 / D, bias=EPS), r=[B_ssq], w=[B_ssq])
                    K.op("dve", lambda q: q.reciprocal(out=ssq[:, 4:8], in_=ssq[:, 4:8]), r=[B_ssq], w=[B_ssq])
                    for c in range(4):
                        K.op("dve", lambda q, c=c: q.scalar_tensor_tensor(out=hS[:, c, :], in0=hS[:, c, :], scalar=ssq[:, 4 + c:5 + c], in1=fnw[:], op0=ALU.mult, op1=ALU.mult),
                             r=[B_hS, B_ssq, Bc], w=[B_hS])
                    K.dma("act", y_d[t0:t0 + S, :].rearrange("(c p) d -> p c d", p=128), hS[:], r=[B_hS], w=[B_out])
                else:
                    K.dma("act", hb_d[t0:t0 + S, :].rearrange("(c p) d -> p c d", p=128), hS[:], r=[B_hS], w=[B_hb[sc]])
                    pass
        K.final_wait("act", [B_out])
        K.final_wait("sp", [B_out])
        K.emit()
    return nc, K


B_out = Buf()
B_hb = [Buf() for _ in range(64)]


def _tile_k(wcols, kt_total=8):
    Kd, ncol = wcols.shape
    nkt = Kd // 128
    out = np.zeros((128, kt_total, 128), np.float32)
    out[:, :nkt, :ncol] = wcols.reshape(nkt, 128, ncol).transpose(1, 0, 2)
    return out


def pack_layer(inp, l):
    w_in = inp["w_in"][l]
    slots = []
    pw = np.zeros((128, 8, 128), np.float32)
    for g in range(4):
        pw[:, g, :] = inp["pool_w"][l, g]
    slots.append(pw)
    for j in range(12):
        slots.append(_tile_k(w_in[:, 1024 + 128 * j:1024 + 128 * (j + 1)]))
    for f in range(8):
        slots.append(_tile_k(w_in[:, 128 * f:128 * (f + 1)]))
    slots.append(_tile_k(np.concatenate([w_in[:, 2560:2576], w_in[:, 3664:3668]], axis=1)))
    slots.append(_tile_k(w_in[:, 3216:3344]))
    for i in range(4):
        slots.append(_tile_k(np.concatenate([w_in[:, 2576 + 64 * i:2576 + 64 * (i + 1)], w_in[:, 2576 + 64 * (4 + i):2576 + 64 * (5 + i)]], axis=1)))
    slots.append(_tile_k(w_in[:, 3088:3216]))
    for i in range(2):
        slots.append(_tile_k(np.concatenate([w_in[:, 3344 + 64 * i:3344 + 64 * (i + 1)], w_in[:, 3344 + 64 * (2 + i):3344 + 64 * (3 + i)]], axis=1)))
    slots.append(_tile_k(np.concatenate([w_in[:, 3600:3664], w_in[:, 3600:3664]], axis=1)))
    for g in range(4):
        slots.append(_tile_k(w_in[:, 3668 + 128 * g:3668 + 128 * (g + 1)]))
    for f in range(8):
        cs = slice(128 * f, 128 * (f + 1))
        slots.append(_tile_k(inp["w_up_ssd"][l][:, cs]))
        ap = np.zeros((128, 8, 128), np.float32)
        ap[:, 0:4, :] = inp["w_up_attn"][l][:, cs].reshape(4, 128, 128).transpose(1, 0, 2)
        ap[:, 4:8, :] = inp["w_up_pool"][l][:, cs].reshape(4, 128, 128).transpose(1, 0, 2)
        slots.append(ap)
        for br in range(3):
            c0 = 4180 + (br * 8 + f) * 128
            slots.append(_tile_k(w_in[:, c0:c0 + 128]))
    for f in range(8):
        slots.append(_tile_k(inp["w_out"][l][:, 128 * f:128 * (f + 1)]))
    wfi = inp["w_ffn_in"][l]
    for j in range(22):
        slots.append(_tile_k(wfi[:, 128 * j:128 * (j + 1)]))
        slots.append(_tile_k(wfi[:, 2816 + 128 * j:2816 + 128 * (j + 1)]))
    wfo = inp["w_ffn_out"][l]
    for f in range(8):
        t = wfo[:, 128 * f:128 * (f + 1)].reshape(22, 128, 128).transpose(1, 0, 2)
        for part in range(3):
            s_ = np.zeros((128, 8, 128), np.float32)
            n = 8 if part < 2 else 6
            s_[:, 0:n, :] = t[:, part * 8:part * 8 + n, :]
            slots.append(s_)
    assert len(slots) == NSLOT, len(slots)
    W = np.stack(slots).reshape(NSLOT, 128, 1024)
    pp = np.zeros((128, NPP), np.float32)
    cw = inp["conv_w"][l]
    pp[:, 0:48] = cw.reshape(4, 12, 128).transpose(2, 1, 0).reshape(128, 48)
    pp[:, 48:60] = inp["conv_b"][l].reshape(12, 128).T
    pp[:, 60:64] = inp["pool_scale"][l].reshape(4, 128).T
    pp[:, 64:72] = inp["norm1_w"][l].reshape(8, 128).T
    pp[:, 72:80] = inp["ssd_norm_w"][l].reshape(8, 128).T
    pp[:, 80:88] = inp["norm2_w"][l].reshape(8, 128).T
    rows = np.concatenate([inp["dt_bias"][l], inp["a_log"][l], inp["d_skip"][l]])[None, :].repeat(128, 0).astype(np.float32)
    return W, pp, rows


def make_consts():
    k = np.arange(128)
    cst = np.zeros((128, 8, 128), np.float32)
    cst[:, 0, :] = np.eye(128)
    cst[:, 1, :] = (k[:, None] <= k[None, :])
    cst[:, 2, :] = (k[:, None] > k[None, :])
    cst[:, 3, :] = 1.0
    cst[:, 4, :] = np.where(k[None, :] >= k[:, None], 0.0, -30000.0)
    cst[:, 5, :] = np.where(k[None, :] <= k[:, None], 0.0, NEG)
    oh = np.zeros((128, 128), np.float32)
    oh[0:16, 0:16] = np.eye(16)
    oh[32:48, 16:32] = -np.eye(16)
    cst[:, 6, :] = oh
    p2 = 2.0 ** -(np.arange(28) + 1.0)
    cst[:, 7, 0:28] = p2[None, :]
    cst[:, 7, 28:56] = -p2[None, :]
    rc16 = np.zeros((128, 4, 16), np.float32)
    for g, w in enumerate(POOL_WINDOWS):
        rc16[:, g, :] = 1.0 / np.minimum(np.arange(1, 17), w)
    return cst, rc16


_CACHE = {}


def run(inputs, T=SEQ, L=DEPTH, ncores=NCORES, dbg=False):
    inp = {k: np.asarray(v, np.float32) for k, v in inputs.items()}
    key = (T, L, dbg)
    if key not in _CACHE:
        _CACHE[key] = build(T, L, dbg)
    nc, _ = _CACHE[key]
    Ws, pps, rws = [], [], []
    for l in range(L):
        W, pp, rows = pack_layer(inp, l)
        Ws.append(W)
        pps.append(pp)
        rws.append(rows)
    W = np.stack(Ws)
    pp = np.stack(pps)
    rows = np.stack(rws)
    cst, rc16 = make_consts()
    fnw = np.ascontiguousarray(np.broadcast_to(inp["final_norm_w"][None, :], (128, D))).astype(np.float32)
    in_maps = []
    for b in range(ncores):
        in_maps.append({"x": np.ascontiguousarray(inp["x"][b, :T]), "W": W, "pp": pp, "rows": rows, "fnw": fnw, "cst": cst, "rc16": rc16})
    res = run_bass_kernel_spmd(nc, in_maps, core_ids=list(range(ncores)))
    if dbg:
        return res.results
    return np.stack([np.asarray(r["y"], np.float32) for r in res.results])


def kernel(**inputs):
    return run(inputs)
```

```python
import numpy as np
from contextlib import ExitStack
import concourse.bass as bass
import concourse.mybir as mybir
from concourse.bass_utils import run_bass_kernel_spmd

F32 = mybir.dt.float32
BF16 = mybir.dt.bfloat16
ALU = mybir.AluOpType
AF = mybir.ActivationFunctionType
AX = mybir.AxisListType

D = 1024
SEQ = 4096
DEPTH = 4
NCORES = 8
S = 512
NSLOT = 149
NPP = 88
EPS = 1e-6
SEM_LIMIT = 30000
NEG = -1.0e30
POOL_WINDOWS = (2, 4, 8, 16)


class Buf:
    __slots__ = ("w", "r")

    def __init__(self):
        self.w = None
        self.r = {}


class Ctr:
    def __init__(self, K, name, step):
        self.K, self.name, self.step = K, name, step
        self.n = 0
        self.h = K.new_sem(f"{name}_0")
        self.v = 0

    def next(self):
        if self.v + self.step > SEM_LIMIT:
            self.n += 1
            self.h = self.K.new_sem(f"{self.name}_{self.n}")
            self.v = 0
        self.v += self.step
        return (self.h, self.v)


class Eng:
    def __init__(self, K, name):
        self.name = name
        self.ops = []
        self.ctr = Ctr(K, "s_" + name, 1)
        self.seen = {}
        self.own = set()


class Kern:
    def __init__(self, nc, stack, n_dma_sems=12):
        self.nc = nc
        self.stack = stack
        self.nsem = 0
        self.E = {n: Eng(self, n) for n in ("pe", "act", "dve", "pool", "sp")}
        self.dma_ctrs = [Ctr(self, f"d{i}", 16) for i in range(n_dma_sems)]
        self.dma_rr = 0
        self.nops = 0

    def new_sem(self, name):
        self.nsem += 1
        return self.stack.enter_context(self.nc.semaphore(name))

    def _deps(self, e, r, w):
        deps = {}
        for b in r:
            if b.w is not None and deps.get(b.w[0], 0) < b.w[1]:
                deps[b.w[0]] = b.w[1]
        for b in w:
            if b.w is not None and deps.get(b.w[0], 0) < b.w[1]:
                deps[b.w[0]] = b.w[1]
            for h, v in b.r.items():
                if deps.get(h, 0) < v:
                    deps[h] = v
        waits = []
        for h, v in deps.items():
            if e.name == "pe" and h in e.own:
                continue
            if e.seen.get(h, 0) < v:
                waits.append((h, v))
                e.seen[h] = v
        return waits

    def _commit(self, tok, r, w):
        h, v = tok
        for b in w:
            b.w = tok
            b.r = {}
        for b in r:
            if b.w is not tok and b.r.get(h, 0) < v:
                b.r[h] = v

    def op(self, en, fn, r=(), w=()):
        e = self.E[en]
        waits = self._deps(e, r, w)
        tok = e.ctr.next()
        e.own.add(tok[0])
        e.ops.append((waits, fn, tok[0], 1))
        self._commit(tok, r, w)
        self.nops += 1

    def dma(self, en, out, in_, r=(), w=()):
        e = self.E[en]
        waits = self._deps(e, r, w)
        c = self.dma_ctrs[self.dma_rr]
        self.dma_rr = (self.dma_rr + 1) % len(self.dma_ctrs)
        if c.v > 0 and e.seen.get(c.h, 0) < c.v:
            waits.append((c.h, c.v))
            e.seen[c.h] = c.v
        tok = c.next()
        e.ops.append((waits, (lambda q: q.dma_start(out=out, in_=in_)), tok[0], 16))
        self._commit(tok, r, w)
        self.nops += 1

    def handoff(self, src, dst):
        u = {}
        for b in src:
            if b.w is not None and u.get(b.w[0], 0) < b.w[1]:
                u[b.w[0]] = b.w[1]
            for h, v in b.r.items():
                if u.get(h, 0) < v:
                    u[h] = v
        for b in dst:
            for h, v in u.items():
                if b.r.get(h, 0) < v:
                    b.r[h] = v

    def final_wait(self, en, bufs):
        e = self.E[en]
        waits = self._deps(e, bufs, ())
        e.ops.append((waits, None, None, 0))

    def emit(self):
        nc = self.nc
        E = self.E

        def run(q, e):
            for waits, fn, sem, inc in e.ops:
                for h, v in waits:
                    q.wait_ge(h, v)
                if fn is not None:
                    fn(q).then_inc(sem, inc)

        with nc.Block() as block:
            @block.tensor
            def _(q):
                run(q, E["pe"])

            @block.scalar
            def _(q):
                run(q, E["act"])

            @block.vector
            def _(q):
                run(q, E["dve"])

            @block.gpsimd
            def _(q):
                run(q, E["pool"])

            @block.sync
            def _(q):
                run(q, E["sp"])


def build(T=SEQ, L=DEPTH, dbg=False):
    NCH = T // 128
    NSC = T // S
    nc = bass.Bass("TRN2", target_bir_lowering=False)

    def din(name, shape):
        return nc.dram_tensor(name, shape, F32, kind="ExternalInput").ap()

    x_d = din("x", [T, D])
    W_d = din("W", [L, NSLOT, 128, 1024])
    pp_d = din("pp", [L, 128, NPP])
    rows_d = din("rows", [L, 128, 48])
    fnw_d = din("fnw", [128, D])
    cst_d = din("cst", [128, 8, 128])
    rc16_d = din("rc16", [128, 4, 16])
    y_d = nc.dram_tensor("y", [T, D], F32, kind="ExternalOutput").ap()
    hb_d = nc.dram_tensor("hbuf", [T, D], F32, kind="Internal").ap()

    with ExitStack() as st:
        K = Kern(nc, st)

        def sb(name, shape, dt=F32):
            return st.enter_context(nc.sbuf_tensor("sb_" + name, shape, dt))

        ps = st.enter_context(nc.psum_tensor("ps", [128, 8, 512], F32))
        PB = [Buf() for _ in range(8)]
        pbs = [0]

        def bank(excl=()):
            while True:
                i = pbs[0]
                pbs[0] = (i + 1) % 8
                if i not in excl:
                    return i

        def bank2(excl=()):
            while True:
                if pbs[0] % 2:
                    pbs[0] = (pbs[0] + 1) % 8
                i = pbs[0]
                pbs[0] = (i + 2) % 8
                if i not in excl and (i + 1) not in excl:
                    return i

        def bank4():
            i = 0 if pbs[0] <= 0 or pbs[0] > 4 else 4
            pbs[0] = (i + 4) % 8
            return i

        def psb(i):
            return ps[:, i, :].bitcast(BF16)

        B_dbg, B_dbgout = Buf(), Buf()
        dbgbuf = sb("dbgbuf", [128, 4096]) if dbg else None

        def dump(name, view, bufs, n):
            if not dbg:
                return
            K.op("pool", lambda q: q.tensor_copy(out=dbgbuf[:view.shape[0], 0:n], in_=view), r=bufs, w=[B_dbg])
            d = nc.dram_tensor("dbg_" + name, [view.shape[0], n], F32, kind="ExternalOutput").ap()
            K.dma("sp", d, dbgbuf[:view.shape[0], 0:n], r=[B_dbg], w=[B_dbgout])

        cst = sb("cst", [128, 8, 128])
        rc16 = sb("rc16", [128, 4, 16])
        identb = sb("identb", [128, 128], BF16)
        fnw = sb("fnw", [128, D])
        LHS = sb("LHS", [64, 16, 128])
        RHS = sb("RHS", [64, 128])
        Bc = Buf()
        identf = cst[:, 0, :]
        tri_le = cst[:, 1, :]
        tri_gt = cst[:, 2, :]
        onesf = cst[:, 3, :]
        maskneg = cst[:, 4, :]
        cmaskneg = cst[:, 5, :]
        oh = cst[:, 6, :]
        K.dma("sp", cst[:], cst_d, w=[Bc])
        K.dma("sp", rc16[:], rc16_d, w=[Bc])
        K.dma("sp", fnw[:], fnw_d, w=[Bc])
        K.op("dve", lambda q: q.tensor_copy(out=identb[:], in_=identf), r=[Bc], w=[Bc])
        identBIG = sb("identBIG", [128, 128], BF16)
        K.op("dve", lambda q: q.tensor_scalar(out=identBIG[:], in0=identf, scalar1=30000.0, scalar2=None, op0=ALU.mult), r=[Bc], w=[Bc])
        B_LHS, B_RHS = Buf(), Buf()
        K.op("pool", lambda q: q.memset(LHS[:], 0.0), w=[B_LHS])
        K.op("pool", lambda q: q.memset(RHS[:], 0.0), w=[B_RHS])
        K.op("dve", lambda q: q.tensor_copy(out=LHS[0:16, :, :], in_=oh[0:16, 0:16].unsqueeze(2).to_broadcast([16, 16, 128])), r=[Bc], w=[B_LHS])
        K.op("pool", lambda q: q.memset(RHS[32:48, :], 1.0), w=[B_RHS])

        pp = sb("pp", [128, NPP])
        rows = sb("rows", [128, 48])
        A_b = sb("A_b", [128, 16])
        B_par = Buf()
        hS = sb("hS", [128, 4, D])
        B_hS = Buf()
        xn_tok = [sb(f"xn_tok{i}", [128, D], BF16) for i in range(2)]
        B_xn = [Buf(), Buf()]
        junk = sb("junk", [128, D], BF16)
        B_junk = Buf()
        ssq = sb("ssq", [128, 8])
        B_ssq = Buf()
        xnT = sb("xnT", [128, 8, S], BF16)
        B_xnT = Buf()
        qT = sb("qT", [128, 4, S], BF16)
        B_qT = Buf()
        kT = sb("kT", [128, T], BF16)
        B_kT = Buf()
        qiT = sb("qiT", [128, 2, S], BF16)
        B_qiT = Buf()
        kiT = sb("kiT", [128, T], BF16)
        B_kiT = Buf()
        Vh = sb("Vh", [128, NCH, 128], BF16)
        B_V = Buf()
        dw = sb("dw", [128, 4, 20])
        B_dw = Buf()
        state = sb("state", [128, 16, 64])
        state_bf = sb("state_bf", [128, 16, 64], BF16)
        B_state, B_statebf = Buf(), Buf()
        chalo = sb("chalo", [128, 12, 3], BF16)
        B_chalo = Buf()
        phalo = sb("phalo", [128, 4, 15])
        B_phalo = Buf()
        ynT = sb("ynT", [128, 8, S], BF16)
        B_ynT = Buf()
        yattT = sb("yattT", [128, 4, S], BF16)
        B_yattT = Buf()
        ypoolT = sb("ypoolT", [128, 4, S], BF16)
        B_ypoolT = Buf()
        NS, NB = 3, 8
        wbf = [sb(f"wbf{i}", [128, 8, 128], BF16) for i in range(NB)]
        B_wst = [Buf() for _ in range(NS)]
        B_wbf = [Buf() for _ in range(NB)]
        upre = [sb(f"upre{i}", [128, 515], BF16) for i in range(2)]
        B_upre = [Buf(), Buf()]
        ubuf = [sb(f"ubuf{i}", [128, 527]) for i in range(2)]
        B_ubuf = [Buf(), Buf()]
        ptmp = [sb(f"ptmp{i}", [128, 527]) for i in range(2)]
        B_ptmp = [Buf(), Buf()]
        pooled = [sb(f"pooled{i}", [128, S], BF16) for i in range(2)]
        B_pooled = [Buf(), Buf()]
        small = sb("small", [128, 256])
        B_sm = {n: Buf() for n in ("dt", "a48", "e3", "dtdec", "m8", "thr", "rs", "rsum", "rinv")}
        bis = sb("bis", [128, 64])
        B_bis = {n: Buf() for n in ("D", "cnt", "u", "mid", "lo", "d0")}
        rsb = sb("rsb", [128, 8, 8])
        B_rsb = [Buf() for _ in range(8)]

        R1N = 15872
        R1 = sb("R1", [128, R1N])

        def carve(off_f32, nelem, dt):
            if dt == F32:
                return R1[:, off_f32:off_f32 + nelem]
            return R1[:, off_f32:off_f32 + nelem // 2].bitcast(BF16)

        o = 0
        xbc_act = carve(o, 12 * S, BF16).rearrange("p (j t) -> p j t", j=12); o += 6 * S
        zs = carve(o, 4 * D, BF16).rearrange("p (c d) -> p c d", c=4); o += 2 * D
        xs_tok = carve(o, 4 * D, BF16).rearrange("p (c d) -> p c d", c=4); o += 2 * D
        B_tok = carve(o, 4 * 256, BF16).rearrange("p (c d) -> p c d", c=4); o += 512
        ctmp = carve(o, S, F32); o += S
        x_dt = carve(o, D, BF16); o += D // 2
        xdec = carve(o, D, BF16); o += D // 2
        xsd = carve(o, D, BF16); o += D // 2
        Lexp = carve(o, D, BF16).rearrange("p (j t) -> p j t", j=8); o += D // 2
        Mt = carve(o, D, BF16).rearrange("p (j t) -> p j t", j=8); o += D // 2
        cb = carve(o, 128, F32); o += 128
        yacc = carve(o, D, F32); o += D
        ynb = carve(o, D, BF16); o += D // 2
        assert o <= R1N, o
        ssd_bufs = {n: Buf() for n in ("xbc_act", "zs", "xs_tok", "B_tok", "ctmp", "x_dt", "xdec", "xsd", "Lexp", "Mt", "cb", "yacc", "ynb")}
        o = 0
        score = carve(o, T, F32); o += T
        work = carve(o, T, F32); o += T
        rtmp = [carve(o + i * S, S, F32) for i in range(2)]; o += 2 * S
        Pm = [carve(o + i * (S // 2), S, BF16) for i in range(3)]; o += 3 * (S // 2)
        PmT = [carve(o + i * (S // 2), S, BF16) for i in range(3)]; o += 3 * (S // 2)
        yatt = carve(o, 512, BF16); o += 256
        mbias = [carve(o + i * (T // 2), T, BF16) for i in range(2)]; o += T
        assert o <= R1N, o
        att_bufs = {n: Buf() for n in ("score", "work", "rtmp0", "rtmp1", "Pm0", "Pm1", "Pm2", "PmT0", "PmT1", "PmT2", "yatt", "mb0", "mb1")}
        o = 0
        mergedT = carve(o, 8 * S, BF16).rearrange("p (j t) -> p j t", j=8); o += 4 * S
        sig = [carve(o + i * (S // 2), S, BF16) for i in range(3)]; o += 3 * (S // 2)
        mt = [carve(o + i * S, S, F32) for i in range(3)]; o += 3 * S
        assert o <= R1N, o
        mrg_bufs = {n: Buf() for n in ("mergedT", "sig0", "sig1", "sig2", "mt0", "mt1", "mt2")}
        o = 0
        hidT = carve(o, 22 * S, BF16).rearrange("p (j t) -> p j t", j=22); o += 11 * S
        sa = [carve(o + i * (S // 2), S, BF16) for i in range(2)]; o += S
        assert o <= R1N, o
        ffn_bufs = {n: Buf() for n in ("hidT", "sa0", "sa1")}

        Wb_d = nc.dram_tensor("Wb", [L, NSLOT, 128, 1024], BF16, kind="Internal").ap()
        B_wb = [[Buf() for _ in range(NSLOT)] for _ in range(L)]
        wst = [carve(i * 1024, 1024, F32).rearrange("p (k c) -> p k c", k=8) for i in range(NS)]
        cvt = [carve(NS * 1024 + i * 512, 1024, BF16) for i in range(4)]
        B_cvt = [Buf() for _ in range(4)]
        n_ = 0
        for l_ in range(L):
            for slot_ in range(NSLOT):
                s_, c_ = n_ % NS, n_ % 4
                K.dma("sp", wst[s_].rearrange("p k c -> p (k c)"), W_d[l_, slot_], w=[B_wst[s_]])
                if n_ % 2 == 0:
                    K.op("dve", lambda q, s_=s_, c_=c_: q.tensor_copy(out=cvt[c_], in_=wst[s_].rearrange("p k c -> p (k c)")), r=[B_wst[s_]], w=[B_cvt[c_]])
                else:
                    K.op("act", lambda q, s_=s_, c_=c_: q.activation(out=cvt[c_], in_=wst[s_].rearrange("p k c -> p (k c)"), func=AF.Copy), r=[B_wst[s_]], w=[B_cvt[c_]])
                K.dma("pool", Wb_d[l_, slot_], cvt[c_], r=[B_cvt[c_]], w=[B_wb[l_][slot_]])
                n_ += 1
        wctr = [0]
        cur = {"l": 0, "slot": 0}

        def wtile():
            i = wctr[0]
            wctr[0] += 1
            l, slot = cur["l"], cur["slot"]
            cur["slot"] += 1
            b_ = i % NB
            K.dma("sp", wbf[b_][:].rearrange("p k c -> p (k c)"), Wb_d[l, slot], r=[B_wb[l][slot]], w=[B_wbf[b_]])
            return wbf[b_], B_wbf[b_]

        def rms_T(nwoff, dstT, B_dst):
            for c in range(4):
                K.op("act", lambda q, c=c: q.activation(out=junk[:], in_=hS[:, c, :], func=AF.Square, accum_out=ssq[:, c:c + 1]),
                     r=[B_hS], w=[B_junk, B_ssq])
            K.op("act", lambda q: q.activation(out=ssq[:, 4:8], in_=ssq[:, 0:4], func=AF.Sqrt, scale=1.0 / D, bias=EPS), r=[B_ssq], w=[B_ssq])
            K.op("dve", lambda q: q.reciprocal(out=ssq[:, 4:8], in_=ssq[:, 4:8]), r=[B_ssq], w=[B_ssq])
            for c in range(4):
                xt, bx = xn_tok[c % 2], B_xn[c % 2]
                K.op("dve", lambda q, c=c, xt=xt: q.tensor_scalar(out=xt[:], in0=hS[:, c, :], scalar1=ssq[:, 4 + c:5 + c], scalar2=None, op0=ALU.mult),
                     r=[B_hS, B_ssq], w=[bx])
                for half in range(2):
                    b = bank()
                    for j in range(4):
                        kt = half * 4 + j
                        K.op("pe", lambda q, b=b, j=j, kt=kt, xt=xt: q.transpose(out=psb(b)[:, j * 128:(j + 1) * 128], in_=xt[:, kt * 128:(kt + 1) * 128], identity=identb[:]),
                             r=[bx, Bc], w=[PB[b]])
                    K.op("dve", lambda q, b=b, half=half, c=c: q.tensor_tensor(
                        out=dstT[:, half * 4:half * 4 + 4, c * 128:(c + 1) * 128],
                        in0=psb(b)[:, 0:512].rearrange("p (j t) -> p j t", j=4),
                        in1=pp[:, nwoff + half * 4:nwoff + half * 4 + 4].unsqueeze(2).to_broadcast([128, 4, 128]), op=ALU.mult),
                        r=[PB[b], B_par], w=[B_dst])

        def mm_fm(wt, bw, KT, rhsT, B_rhs, b, kt0=0, first=True, last=True, wkt0=0):
            for kt in range(KT):
                K.op("pe", lambda q, kt=kt: q.matmul(ps[:, b, :], lhsT=wt[:, wkt0 + kt, :], rhs=rhsT[:, kt0 + kt, :],
                                                    start=(first and kt == 0), stop=(last and kt == KT - 1)),
                     r=[bw, B_rhs], w=[PB[b]])

        def mm_tm(wt, bw, KT, lhsT_src, B_l, b, ncols=128, cstride=128, wkt0=0, kt0=0, first=True, last=True):
            for c in range(4):
                for kt in range(KT):
                    K.op("pe", lambda q, c=c, kt=kt: q.matmul(ps[:, b, c * cstride:c * cstride + ncols],
                                                              lhsT=lhsT_src[:, kt0 + kt, c * 128:(c + 1) * 128], rhs=wt[:, wkt0 + kt, 0:ncols],
                                                              start=(first and kt == 0), stop=(last and kt == KT - 1)),
                         r=[bw, B_l], w=[PB[b]])

        def mm_tm_wide(nkt_total, lhsT_src, B_l, evac):
            for cgp in range(2):
                b4 = bank4()
                nsl = (nkt_total + 1) // 2
                for si in range(nsl):
                    wt, bw = wtile()
                    ww = wt[:].rearrange("p k c -> p (k c)").rearrange("p (k c) -> p k c", k=2)
                    for c in range(4):
                        for k2 in range(2):
                            kt = si * 2 + k2
                            if kt >= nkt_total:
                                continue
                            K.op("pe", lambda q, c=c, k2=k2, kt=kt, ww=ww, b4=b4: q.matmul(ps[:, b4 + c, :], lhsT=lhsT_src[:, kt, c * 128:(c + 1) * 128], rhs=ww[:, k2, :],
                                                                                       start=(kt == 0), stop=(kt == nkt_total - 1)),
                                 r=[bw, B_l], w=[PB[b4 + c]])
                evac(cgp, b4)

        for l in range(L):
            src_d = x_d if l == 0 else hb_d
            last_layer = (l == L - 1)
            K.dma("sp", pp[:], pp_d[l], w=[B_par])
            K.dma("sp", rows[:], rows_d[l], w=[B_par])
            K.op("act", lambda q: q.activation(out=A_b[:], in_=rows[:, 16:32], func=AF.Exp), r=[B_par], w=[B_par])
            K.op("dve", lambda q: q.tensor_scalar(out=A_b[:], in0=A_b[:], scalar1=-1.0, scalar2=None, op0=ALU.mult), r=[B_par], w=[B_par])
            K.op("pool", lambda q: q.memset(state[:], 0.0), w=[B_state])
            K.op("pool", lambda q: q.memset(state_bf[:], 0.0), w=[B_statebf])
            K.op("pool", lambda q: q.memset(chalo[:], 0.0), w=[B_chalo])
            K.op("pool", lambda q: q.memset(phalo[:], 0.0), w=[B_phalo])

            for sc in range(NSC):
                t0 = sc * S
                cg0 = sc * 4
                cur["l"], cur["slot"] = l, 0
                SB = ssd_bufs
                K.dma("act", hS[:], src_d[t0:t0 + S, :].rearrange("(c p) d -> p c d", p=128), r=([B_hb[sc]] if l > 0 else []), w=[B_hS])
                rms_T(64, xnT, B_xnT)
                K.handoff(list(ffn_bufs.values()) + list(mrg_bufs.values()) + list(att_bufs.values()) + B_wst + B_cvt, list(SB.values()))
                dg = dbg and l == 0 and sc < 2
                if dg:
                    dump(f"xnT{sc}", xnT[:].rearrange("p k t -> p (k t)"), [B_xnT], 4096)

                wpool, bwpool = wtile()
                K.op("pool", lambda q, wpool=wpool: q.tensor_copy(out=junk[:, 0:512].rearrange("p (g c) -> p g c", g=4), in_=wpool[:, 0:4, :]), r=[bwpool], w=[B_junk])
                poolw = junk[:, 0:512].rearrange("p (g c) -> p g c", g=4)

                for j in range(12):
                    wt, bw = wtile()
                    b = bank()
                    mm_fm(wt, bw, 8, xnT, B_xnT, b)
                    up, bu = upre[j % 2], B_upre[j % 2]
                    K.op("act", lambda q, b=b, up=up: q.activation(out=up[:, 3:515], in_=ps[:, b, :], func=AF.Copy), r=[PB[b]], w=[bu])
                    K.op("pool", lambda q, j=j, up=up: q.tensor_copy(out=up[:, 0:3], in_=chalo[:, j, :]), r=[B_chalo], w=[bu])
                    K.op("dve", lambda q, j=j, up=up: q.tensor_scalar(out=ctmp, in0=up[:, 0:512], scalar1=pp[:, j * 4:j * 4 + 1], scalar2=None, op0=ALU.mult),
                         r=[bu, B_par], w=[SB["ctmp"]])
                    for k in range(1, 4):
                        K.op("dve", lambda q, j=j, k=k, up=up: q.scalar_tensor_tensor(out=ctmp, in0=up[:, k:k + 512], scalar=pp[:, j * 4 + k:j * 4 + k + 1], in1=ctmp, op0=ALU.mult, op1=ALU.add),
                             r=[bu, B_par, SB["ctmp"]], w=[SB["ctmp"]])
                    K.op("pool", lambda q, j=j, up=up: q.tensor_copy(out=chalo[:, j, :], in_=up[:, 512:515]), r=[bu], w=[B_chalo])
                    K.op("act", lambda q, j=j: q.activation(out=xbc_act[:, j, :], in_=ctmp, func=AF.Silu, bias=pp[:, 48 + j:49 + j]),
                         r=[SB["ctmp"], B_par], w=[SB["xbc_act"]])
                for c in range(4):
                    for half in range(2):
                        b = bank()
                        for j in range(4):
                            K.op("pe", lambda q, b=b, j=j, c=c, half=half: q.transpose(out=psb(b)[:, j * 128:(j + 1) * 128], in_=xbc_act[:, half * 4 + j, c * 128:(c + 1) * 128], identity=identb[:]),
                                 r=[SB["xbc_act"], Bc], w=[PB[b]])
                        K.op("act", lambda q, b=b, c=c, half=half: q.activation(out=xs_tok[:, c, half * 512:(half + 1) * 512], in_=psb(b)[:, 0:512], func=AF.Copy),
                             r=[PB[b]], w=[SB["xs_tok"]])
                    b = bank()
                    for g in range(2):
                        K.op("pe", lambda q, b=b, g=g, c=c: q.transpose(out=psb(b)[:, g * 128:(g + 1) * 128], in_=xbc_act[:, 8 + g, c * 128:(c + 1) * 128], identity=identb[:]),
                             r=[SB["xbc_act"], Bc], w=[PB[b]])
                    K.op("act", lambda q, b=b, c=c: q.activation(out=B_tok[:, c, :], in_=psb(b)[:, 0:256], func=AF.Copy), r=[PB[b]], w=[SB["B_tok"]])

                if dg:
                    dump(f"xbc{sc}a", xbc_act[:, 0:8, :].rearrange("p k t -> p (k t)"), [SB["xbc_act"]], 4096)
                    dump(f"xbc{sc}b", xbc_act[:, 8:12, :].rearrange("p k t -> p (k t)"), [SB["xbc_act"]], 2048)
                    dump(f"xstok{sc}", xs_tok.rearrange("p c d -> p (c d)"), [SB["xs_tok"]], 4096)
                def evac_z(cgp, b4):
                    K.op("act", lambda q: q.activation(out=zs[:, :, cgp * 512:(cgp + 1) * 512], in_=ps[:, b4:b4 + 4, :], func=AF.Silu),
                         r=[PB[b4], PB[b4 + 1], PB[b4 + 2], PB[b4 + 3]], w=[SB["zs"]])
                mm_tm_wide(8, xnT, B_xnT, evac_z)
                wt, bw = wtile()
                b = bank()
                mm_tm(wt, bw, 8, xnT, B_xnT, b, ncols=20, cstride=32)
                K.op("act", lambda q, b=b: q.activation(out=dw[:], in_=ps[:, b, 0:128].rearrange("p (c t) -> p c t", c=4)[:, :, 0:20], func=AF.Copy), r=[PB[b]], w=[B_dw])
                wt, bw = wtile()
                b = bank()
                mm_tm(wt, bw, 8, xnT, B_xnT, b)
                K.op("act", lambda q, b=b, cg0=cg0: q.activation(out=Vh[:, cg0:cg0 + 4, :], in_=ps[:, b, :].rearrange("p (c t) -> p c t", c=4), func=AF.Copy), r=[PB[b]], w=[B_V])
                for i in range(4):
                    wt, bw = wtile()
                    b = bank()
                    mm_fm(wt, bw, 8, xnT, B_xnT, b)
                    K.op("act", lambda q, b=b, i=i: q.activation(out=qT[:, i, :], in_=ps[:, b, :], func=AF.Copy, scale=0.125), r=[PB[b]], w=[B_qT])
                wt, bw = wtile()
                b = bank()
                mm_fm(wt, bw, 8, xnT, B_xnT, b)
                K.op("act", lambda q, b=b, t0=t0: q.activation(out=kT[:, t0:t0 + S], in_=ps[:, b, :], func=AF.Copy), r=[PB[b]], w=[B_kT])
                for i in range(2):
                    wt, bw = wtile()
                    b = bank()
                    mm_fm(wt, bw, 8, xnT, B_xnT, b)
                    K.op("act", lambda q, b=b, i=i: q.activation(out=qiT[:, i, :], in_=ps[:, b, :], func=AF.Copy, scale=0.0625), r=[PB[b]], w=[B_qiT])
                wt, bw = wtile()
                b = bank()
                mm_fm(wt, bw, 8, xnT, B_xnT, b)
                K.op("act", lambda q, b=b, t0=t0: q.activation(out=kiT[:, t0:t0 + S], in_=ps[:, b, :], func=AF.Copy), r=[PB[b]], w=[B_kiT])

                for g in range(4):
                    win = POOL_WINDOWS[g]
                    wt, bw = wtile()
                    b = bank()
                    mm_fm(wt, bw, 8, xnT, B_xnT, b)
                    ub, bub = ubuf[g % 2], B_ubuf[g % 2]
                    K.op("act", lambda q, b=b, ub=ub: q.activation(out=ub[:, 15:527], in_=ps[:, b, :], func=AF.Copy), r=[PB[b]], w=[bub])
                    K.op("pool", lambda q, g=g, ub=ub: q.tensor_copy(out=ub[:, 0:15], in_=phalo[:, g, :]), r=[B_phalo], w=[bub])
                    srcb, bsrc = ub, bub
                    lvl = 1
                    pi = 0
                    while lvl < win:
                        dst, bdst = ptmp[pi], B_ptmp[pi]
                        lo = 2 * lvl - 1
                        K.op("dve", lambda q, srcb=srcb, dst=dst, lo=lo, lvl=lvl: q.tensor_tensor(out=dst[:, lo:527], in0=srcb[:, lo:527], in1=srcb[:, lo - lvl:527 - lvl], op=ALU.add),
                             r=[bsrc], w=[bdst])
                        srcb, bsrc = dst, bdst
                        pi ^= 1
                        lvl *= 2
                    pl, bpl = pooled[g % 2], B_pooled[g % 2]
                    if sc == 0:
                        K.op("dve", lambda q, srcb=srcb, g=g: q.tensor_tensor(out=srcb[:, 15:31], in0=srcb[:, 15:31], in1=rc16[:, g, :], op=ALU.mult), r=[bsrc, Bc], w=[bsrc])
                        K.op("dve", lambda q, srcb=srcb, ub=ub, pl=pl: q.tensor_tensor(out=pl[:, 0:16], in0=srcb[:, 15:31], in1=ub[:, 15:31], op=ALU.subtract), r=[bsrc, bub], w=[bpl])
                        K.op("dve", lambda q, srcb=srcb, ub=ub, pl=pl, win=win: q.scalar_tensor_tensor(out=pl[:, 16:512], in0=srcb[:, 31:527], scalar=1.0 / win, in1=ub[:, 31:527], op0=ALU.mult, op1=ALU.subtract),
                             r=[bsrc, bub], w=[bpl])
                    else:
                        K.op("dve", lambda q, srcb=srcb, ub=ub, pl=pl, win=win: q.scalar_tensor_tensor(out=pl[:, 0:512], in0=srcb[:, 15:527], scalar=1.0 / win, in1=ub[:, 15:527], op0=ALU.mult, op1=ALU.subtract),
                             r=[bsrc, bub], w=[bpl])
                    K.op("pool", lambda q, g=g, ub=ub: q.tensor_copy(out=phalo[:, g, :], in_=ub[:, 512:527]), r=[bub], w=[B_phalo])
                    b2 = bank()
                    K.op("pe", lambda q, b2=b2, g=g, pl=pl: q.matmul(ps[:, b2, :], lhsT=poolw[:, g, :], rhs=pl[:], start=True, stop=True), r=[B_junk, bpl], w=[PB[b2]])
                    K.op("act", lambda q, b2=b2, g=g: q.activation(out=ypoolT[:, g, :], in_=ps[:, b2, :], func=AF.Copy, scale=pp[:, 60 + g:61 + g]), r=[PB[b2], B_par], w=[B_ypoolT])

                if dg:
                    dump(f"zs{sc}", zs.rearrange("p c d -> p (c d)"), [SB["zs"]], 4096)
                    dump(f"dw{sc}", dw[:].rearrange("p c d -> p (c d)"), [B_dw], 80)
                    dump(f"ypoolT{sc}", ypoolT[:].rearrange("p k t -> p (k t)"), [B_ypoolT], 2048)
                    dump(f"qT{sc}", qT[:].rearrange("p k t -> p (k t)"), [B_qT], 2048)
                sm = small
                for c in range(4):
                    cc = slice(c * 128, (c + 1) * 128)
                    K.op("dve", lambda q, c=c: q.tensor_tensor(out=sm[:, 0:16], in0=dw[:, c, 0:16], in1=rows[:, 0:16], op=ALU.add), r=[B_dw, B_par], w=[B_sm["dt"]])
                    K.op("act", lambda q: q.activation(out=sm[:, 0:16], in_=sm[:, 0:16], func=AF.Exp), r=[B_sm["dt"]], w=[B_sm["dt"]])
                    K.op("act", lambda q: q.activation(out=sm[:, 0:16], in_=sm[:, 0:16], func=AF.Ln, bias=1.0), r=[B_sm["dt"]], w=[B_sm["dt"]])
                    K.op("pool", lambda q: q.memset(sm[:, 32:80], 0.0), w=[B_sm["a48"]])
                    K.op("dve", lambda q: q.tensor_tensor(out=sm[:, 32:48], in0=sm[:, 0:16], in1=A_b[:], op=ALU.mult), r=[B_sm["dt"], B_par], w=[B_sm["a48"]])
                    K.op("dve", lambda q: q.tensor_tensor(out=sm[:, 64:80], in0=sm[:, 0:16], in1=A_b[:], op=ALU.mult), r=[B_sm["dt"], B_par], w=[B_sm["a48"]])
                    bs = bank()
                    a16 = sm[:, 32:48]
                    K.op("pe", lambda q, bs=bs: q.matmul(ps[:, bs, 0:16], lhsT=tri_le, rhs=a16, start=True, stop=True), r=[Bc, B_sm["a48"]], w=[PB[bs]])
                    K.op("pe", lambda q, bs=bs: q.matmul(ps[:, bs, 16:32], lhsT=tri_gt, rhs=a16, start=True, stop=True), r=[Bc, B_sm["a48"]], w=[PB[bs]])
                    K.op("pe", lambda q, bs=bs: q.matmul(ps[:, bs, 32:48], lhsT=onesf, rhs=a16, start=True, stop=True), r=[Bc, B_sm["a48"]], w=[PB[bs]])
                    K.op("act", lambda q, bs=bs: q.activation(out=sm[:, 80:128], in_=ps[:, bs, 0:48], func=AF.Exp), r=[PB[bs]], w=[B_sm["e3"]])
                    bt = bank()
                    K.op("pe", lambda q, bt=bt: q.matmul(ps[0:48, bt, 0:128], lhsT=sm[:, 32:80], rhs=tri_le, start=True, stop=True), r=[Bc, B_sm["a48"]], w=[PB[bt]])
                    K.op("act", lambda q, bt=bt: q.activation(out=RHS[0:16, :], in_=ps[0:16, bt, 0:128], func=AF.Copy), r=[PB[bt]], w=[B_RHS])
                    K.op("dve", lambda q, bt=bt: q.tensor_tensor(out=LHS[32:48, :, :], in0=ps[32:48, bt, 0:128].unsqueeze(1).to_broadcast([16, 16, 128]),
                                                                  in1=oh[32:48, 16:32].unsqueeze(2).to_broadcast([16, 16, 128]), op=ALU.mult),
                         r=[PB[bt], Bc], w=[B_LHS])
                    xs3 = xs_tok[:, c, :].rearrange("p (h d) -> p h d", h=16)
                    K.op("dve", lambda q: q.tensor_tensor(out=sm[:, 128:144], in0=sm[:, 0:16], in1=sm[:, 96:112], op=ALU.mult), r=[B_sm["dt"], B_sm["e3"]], w=[B_sm["dtdec"]])
                    K.op("dve", lambda q, xs3=xs3: q.tensor_tensor(out=x_dt.rearrange("p (h d) -> p h d", h=16), in0=xs3, in1=sm[:, 0:16].unsqueeze(2).to_broadcast([128, 16, 64]), op=ALU.mult),
                         r=[SB["xs_tok"], B_sm["dt"]], w=[SB["x_dt"]])
                    K.op("dve", lambda q, xs3=xs3: q.tensor_tensor(out=xdec.rearrange("p (h d) -> p h d", h=16), in0=xs3, in1=sm[:, 128:144].unsqueeze(2).to_broadcast([128, 16, 64]), op=ALU.mult),
                         r=[SB["xs_tok"], B_sm["dtdec"]], w=[SB["xdec"]])
                    K.op("dve", lambda q, xs3=xs3: q.tensor_tensor(out=xsd.rearrange("p (h d) -> p h d", h=16), in0=xs3, in1=rows[:, 32:48].unsqueeze(2).to_broadcast([128, 16, 64]), op=ALU.mult),
                         r=[SB["xs_tok"], B_par], w=[SB["xsd"]])
                    by = bank2()
                    live = (by, by + 1)
                    for g in range(2):
                        bcb = bank(live)
                        K.op("pe", lambda q, bcb=bcb, g=g, cc=cc: q.matmul(ps[:, bcb, 0:128], lhsT=xbc_act[:, 8 + g, cc], rhs=xbc_act[:, 10 + g, cc], start=True, stop=True),
                             r=[SB["xbc_act"]], w=[PB[bcb]])
                        K.op("act", lambda q, bcb=bcb: q.activation(out=cb, in_=ps[:, bcb, 0:128], func=AF.Copy), r=[PB[bcb]], w=[SB["cb"]])
                        be = bank2(live)
                        for j in range(8):
                            h = g * 8 + j
                            bb_, off = be + j // 4, (j % 4) * 128
                            K.op("pe", lambda q, bb_=bb_, off=off, h=h: q.matmul(ps[:, bb_, off:off + 128], lhsT=LHS[:, h, :], rhs=RHS[:], start=True, stop=False),
                                 r=[B_LHS, B_RHS], w=[PB[bb_]])
                            K.op("pe", lambda q, bb_=bb_, off=off: q.matmul(ps[:, bb_, off:off + 128], lhsT=identf, rhs=maskneg, start=False, stop=True),
                                 r=[Bc], w=[PB[bb_]])
                        K.op("act", lambda q, be=be: q.activation(out=Lexp, in_=ps[:, be:be + 2, :].rearrange("p b (j t) -> p (b j) t", j=4), func=AF.Exp),
                             r=[PB[be], PB[be + 1]], w=[SB["Lexp"]])
                        K.op("dve", lambda q: q.tensor_tensor(out=Mt, in0=Lexp, in1=cb.unsqueeze(1).to_broadcast([128, 8, 128]), op=ALU.mult),
                             r=[SB["Lexp"], SB["cb"]], w=[SB["Mt"]])
                        for j in range(8):
                            h = g * 8 + j
                            hb_, hoff = h // 8, (h % 8) * 64
                            K.op("pe", lambda q, j=j, hb_=hb_, hoff=hoff, h=h, by=by: q.matmul(ps[:, by + hb_, hoff:hoff + 64], lhsT=Mt[:, j, :], rhs=x_dt[:, h * 64:(h + 1) * 64], start=True, stop=False),
                                 r=[SB["Mt"], SB["x_dt"]], w=[PB[by + hb_]])
                            K.op("pe", lambda q, hb_=hb_, hoff=hoff, h=h, by=by: q.matmul(ps[:, by + hb_, hoff:hoff + 64], lhsT=identb[:], rhs=xsd[:, h * 64:(h + 1) * 64], start=False, stop=True),
                                 r=[Bc, SB["xsd"]], w=[PB[by + hb_]])
                    bo = bank2(live)
                    for h in range(16):
                        g = h // 8
                        hb_, hoff = h // 8, (h % 8) * 64
                        K.op("pe", lambda q, hb_=hb_, hoff=hoff, h=h, g=g, cc=cc, bo=bo: q.matmul(ps[:, bo + hb_, hoff:hoff + 64], lhsT=xbc_act[:, 10 + g, cc], rhs=state_bf[:, h, :], start=True, stop=True),
                             r=[SB["xbc_act"], B_statebf], w=[PB[bo + hb_]])
                    bsn = bank2(live + (bo, bo + 1))
                    for h in range(16):
                        g = h // 8
                        hb_, hoff = h // 8, (h % 8) * 64
                        K.op("pe", lambda q, hb_=hb_, hoff=hoff, h=h, g=g, c=c, bsn=bsn: q.matmul(ps[:, bsn + hb_, hoff:hoff + 64], lhsT=B_tok[:, c, g * 128:(g + 1) * 128], rhs=xdec[:, h * 64:(h + 1) * 64], start=True, stop=True),
                             r=[SB["B_tok"], SB["xdec"]], w=[PB[bsn + hb_]])
                    cd_b = sm[:, 112:128].unsqueeze(2).to_broadcast([128, 16, 64])
                    K.op("dve", lambda q, cd_b=cd_b: q.tensor_tensor(out=state[:], in0=state[:], in1=cd_b, op=ALU.mult), r=[B_state, B_sm["e3"]], w=[B_state])
                    K.op("dve", lambda q, bsn=bsn: q.tensor_tensor(out=state[:].rearrange("p (b h) d -> p b (h d)", b=2), in0=ps[:, bsn:bsn + 2, :], in1=state[:].rearrange("p (b h) d -> p b (h d)", b=2), op=ALU.add),
                         r=[PB[bsn], PB[bsn + 1], B_state], w=[B_state])
                    K.op("pool", lambda q: q.tensor_copy(out=state_bf[:], in_=state[:]), r=[B_state], w=[B_statebf])
                    ea_b = sm[:, 80:96].unsqueeze(2).to_broadcast([128, 16, 64])
                    K.op("dve", lambda q, bo=bo, ea_b=ea_b: q.tensor_tensor(out=yacc.rearrange("p (h d) -> p h d", h=16), in0=ps[:, bo:bo + 2, :].rearrange("p b (h d) -> p (b h) d", h=8), in1=ea_b, op=ALU.mult),
                         r=[PB[bo], PB[bo + 1], B_sm["e3"]], w=[SB["yacc"]])
                    K.op("dve", lambda q, by=by: q.tensor_tensor(out=yacc.rearrange("p (b f) -> p b f", b=2), in0=ps[:, by:by + 2, :], in1=yacc.rearrange("p (b f) -> p b f", b=2), op=ALU.add),
                         r=[PB[by], PB[by + 1], SB["yacc"]], w=[SB["yacc"]])
                    if dg:
                        dump(f"y{sc}_{c}", yacc, [SB["yacc"]], 1024)
                        dump(f"sm{sc}_{c}", sm[:, 0:128], [B_sm["dt"], B_sm["e3"], B_sm["a48"]], 128)
                    K.op("dve", lambda q, c=c: q.tensor_tensor(out=yacc, in0=yacc, in1=zs[:, c, :], op=ALU.mult), r=[SB["yacc"], SB["zs"]], w=[SB["yacc"]])
                    K.op("act", lambda q: q.activation(out=junk[:], in_=yacc, func=AF.Square, accum_out=sm[:, 169:170]), r=[SB["yacc"]], w=[B_junk, B_sm["rs"]])
                    K.op("act", lambda q: q.activation(out=sm[:, 170:171], in_=sm[:, 169:170], func=AF.Sqrt, scale=1.0 / D, bias=EPS), r=[B_sm["rs"]], w=[B_sm["rs"]])
                    K.op("dve", lambda q: q.reciprocal(out=sm[:, 170:171], in_=sm[:, 170:171]), r=[B_sm["rs"]], w=[B_sm["rs"]])
                    K.op("dve", lambda q: q.tensor_scalar(out=ynb, in0=yacc, scalar1=sm[:, 170:171], scalar2=None, op0=ALU.mult), r=[SB["yacc"], B_sm["rs"]], w=[SB["ynb"]])
                    for half in range(2):
                        b = bank()
                        for j in range(4):
                            kt = half * 4 + j
                            K.op("pe", lambda q, b=b, j=j, kt=kt: q.transpose(out=psb(b)[:, j * 128:(j + 1) * 128], in_=ynb[:, kt * 128:(kt + 1) * 128], identity=identb[:]),
                                 r=[SB["ynb"], Bc], w=[PB[b]])
                        K.op("dve", lambda q, b=b, half=half, cc=cc: q.tensor_tensor(
                            out=ynT[:, half * 4:half * 4 + 4, cc], in0=psb(b)[:, 0:512].rearrange("p (j t) -> p j t", j=4),
                            in1=pp[:, 72 + half * 4:72 + half * 4 + 4].unsqueeze(2).to_broadcast([128, 4, 128]), op=ALU.mult),
                            r=[PB[b], B_par], w=[B_ynT])
                AB = att_bufs
                K.handoff(list(SB.values()), list(AB.values()))

                def idx_topk(c):
                    cg = cg0 + c
                    nk = (cg + 1) * 128
                    nb = (nk + S - 1) // S
                    cc = slice(c * 128, (c + 1) * 128)
                    for kb in range(nb):
                        w_ = min(S, nk - kb * S)
                        ks = slice(kb * S, kb * S + w_)
                        for h in range(4):
                            b = bank()
                            half, ti = h // 2, h % 2
                            prt = slice(half * 64, half * 64 + 64)
                            K.op("pe", lambda q, b=b, prt=prt, ti=ti, ks=ks, w_=w_, cc=cc: q.matmul(ps[:, b, 0:w_], lhsT=qiT[prt, ti, cc], rhs=kiT[prt, ks], start=True, stop=True),
                                 r=[B_qiT, B_kiT], w=[PB[b]])
                            rt, brt = rtmp[h % 2], AB[f"rtmp{h % 2}"]
                            K.op("act", lambda q, b=b, rt=rt, w_=w_: q.activation(out=rt[:, 0:w_], in_=ps[:, b, 0:w_], func=AF.Relu), r=[PB[b]], w=[brt])
                            if h == 0:
                                K.op("dve", lambda q, rt=rt, ks=ks, w_=w_, c=c: q.tensor_scalar(out=score[:, ks], in0=rt[:, 0:w_], scalar1=dw[:, c, 16:17], scalar2=None, op0=ALU.mult),
                                     r=[brt, B_dw], w=[AB["score"]])
                            else:
                                K.op("dve", lambda q, rt=rt, ks=ks, w_=w_, c=c, h=h: q.scalar_tensor_tensor(out=score[:, ks], in0=rt[:, 0:w_], scalar=dw[:, c, 16 + h:17 + h], in1=score[:, ks], op0=ALU.mult, op1=ALU.add),
                                     r=[brt, B_dw, AB["score"]], w=[AB["score"]])
                    K.op("dve", lambda q, nk=nk: q.tensor_tensor(out=score[:, nk - 128:nk], in0=score[:, nk - 128:nk], in1=cmaskneg, op=ALU.add), r=[AB["score"], Bc], w=[AB["score"]])
                    if cg >= 2:
                        NIT = 26
                        nlo = cg * 128
                        K.op("dve", lambda q, nk=nk: q.max(out=sm[:, 144:152], in_=score[:, 0:nk]), r=[AB["score"]], w=[B_sm["m8"]])
                        K.op("dve", lambda q, nlo=nlo: q.tensor_reduce(out=bis[:, 59:60], in_=score[:, 0:nlo], axis=AX.X, op=ALU.min), r=[AB["score"]], w=[B_bis["lo"]])
                        K.op("dve", lambda q: q.tensor_tensor(out=bis[:, 60:61], in0=sm[:, 144:145], in1=bis[:, 59:60], op=ALU.subtract), r=[B_sm["m8"], B_bis["lo"]], w=[B_bis["d0"]])
                        K.op("dve", lambda q: q.tensor_scalar(out=bis[:, 0:56], in0=cst[:, 7, 0:56], scalar1=bis[:, 60:61], scalar2=None, op0=ALU.mult), r=[Bc, B_bis["d0"]], w=[B_bis["D"]])
                        K.op("dve", lambda q: q.tensor_tensor(out=bis[:, 58:59], in0=bis[:, 59:60], in1=bis[:, 0:1], op=ALU.add), r=[B_bis["lo"], B_bis["D"]], w=[B_bis["mid"]])
                        for it in range(NIT):
                            K.op("dve", lambda q, nk=nk: q.tensor_scalar(out=work[:, 0:nk], in0=score[:, 0:nk], scalar1=bis[:, 58:59], scalar2=None, op0=ALU.is_ge, op1=ALU.add, accum_out=bis[:, 56:57]),
                                 r=[AB["score"], B_bis["mid"]], w=[AB["work"], B_bis["cnt"]])
                            K.op("dve", lambda q, it=it: q.scalar_tensor_tensor(out=bis[:, 57:58], in0=bis[:, 56:57], scalar=255.5, in1=bis[:, it:it + 1], op0=ALU.is_ge, op1=ALU.mult),
                                 r=[B_bis["cnt"], B_bis["D"]], w=[B_bis["u"]])
                            K.op("dve", lambda q, it=it: q.scalar_tensor_tensor(out=bis[:, 58:59], in0=bis[:, 57:58], scalar=bis[:, 28 + it + 1:28 + it + 2], in1=bis[:, 58:59], op0=ALU.add, op1=ALU.add),
                                 r=[B_bis["u"], B_bis["D"], B_bis["mid"]], w=[B_bis["mid"]])
                        K.op("dve", lambda q: q.tensor_tensor(out=sm[:, 152:153], in0=bis[:, 58:59], in1=bis[:, 28 + NIT:28 + NIT + 1], op=ALU.add), r=[B_bis["mid"], B_bis["D"]], w=[B_sm["thr"]])
                    else:
                        K.op("dve", lambda q: q.memset(sm[:, 152:153], -1.0e29), w=[B_sm["thr"]])
                    mb = mbias[c % 2]
                    K.op("dve", lambda q, mb=mb, nk=nk: q.tensor_scalar(out=mb[:, 0:nk], in0=score[:, 0:nk], scalar1=sm[:, 152:153], scalar2=1.0, op0=ALU.is_ge, op1=ALU.subtract),
                         r=[AB["score"], B_sm["thr"]], w=[AB[f"mb{c % 2}"]])

                def heads(c):
                    cg = cg0 + c
                    nk = (cg + 1) * 128
                    nb = (nk + S - 1) // S
                    cc = slice(c * 128, (c + 1) * 128)
                    mb, bmb = mbias[c % 2], AB[f"mb{c % 2}"]
                    bO = bank()
                    K.op("pool", lambda q: q.memset(rsb[:], 0.0), w=B_rsb)
                    items = [(h, kb) for h in range(8) for kb in range(nb)]

                    def stA(i):
                        h, kb = items[i]
                        g, ti = h // 4, h % 4
                        prt = slice(g * 64, g * 64 + 64)
                        w_ = min(S, nk - kb * S)
                        ks = slice(kb * S, kb * S + w_)
                        b = bank((bO,))
                        i3 = i % 3
                        K.op("pe", lambda q: q.matmul(ps[:, b, 0:w_], lhsT=qT[prt, ti, cc], rhs=kT[prt, ks], start=True, stop=False),
                             r=[B_qT, B_kT], w=[PB[b]])
                        K.op("pe", lambda q: q.matmul(ps[:, b, 0:w_], lhsT=identBIG[:], rhs=mb[:, ks], start=False, stop=True),
                             r=[Bc, bmb], w=[PB[b]])
                        K.op("act", lambda q: q.activation(out=Pm[i3][:, 0:w_], in_=ps[:, b, 0:w_], func=AF.Exp, accum_out=rsb[:, h, kb:kb + 1]),
                             r=[PB[b]], w=[AB[f"Pm{i3}"], B_rsb[i % 8]])

                    def stB(i):
                        h, kb = items[i]
                        g = h // 4
                        w_ = min(S, nk - kb * S)
                        nsub = w_ // 128
                        i3 = i % 3
                        b2 = bank((bO,))
                        for ii in range(nsub):
                            K.op("pe", lambda q, ii=ii: q.transpose(out=psb(b2)[:, ii * 128:(ii + 1) * 128], in_=Pm[i3][:, ii * 128:(ii + 1) * 128], identity=identb[:]),
                                 r=[AB[f"Pm{i3}"], Bc], w=[PB[b2]])
                        K.op("act", lambda q: q.activation(out=PmT[i3][:, 0:w_], in_=psb(b2)[:, 0:w_], func=AF.Copy), r=[PB[b2]], w=[AB[f"PmT{i3}"]])
                        for ii in range(nsub):
                            kc = kb * 4 + ii
                            K.op("pe", lambda q, ii=ii, kc=kc, first=(kb == 0 and ii == 0), last=(kb == nb - 1 and ii == nsub - 1):
                                 q.matmul(ps[:, bO, h * 64:(h + 1) * 64], lhsT=PmT[i3][:, ii * 128:(ii + 1) * 128], rhs=Vh[:, kc, g * 64:(g + 1) * 64], start=first, stop=last),
                                 r=[AB[f"PmT{i3}"], B_V], w=[PB[bO]])

                    stA(0)
                    for i in range(len(items)):
                        if i + 1 < len(items):
                            stA(i + 1)
                        stB(i)
                    K.op("dve", lambda q: q.tensor_reduce(out=sm[:, 153:161], in_=rsb[:], axis=AX.X, op=ALU.add), r=B_rsb, w=[B_sm["rinv"]])
                    K.op("dve", lambda q: q.reciprocal(out=sm[:, 161:169], in_=sm[:, 153:161]), r=[B_sm["rinv"]], w=[B_sm["rinv"]])
                    K.op("dve", lambda q: q.tensor_tensor(out=yatt.rearrange("p (h d) -> p h d", h=8), in0=ps[:, bO, :].rearrange("p (h d) -> p h d", h=8),
                                                          in1=sm[:, 161:169].unsqueeze(2).to_broadcast([128, 8, 64]), op=ALU.mult),
                         r=[PB[bO], B_sm["rinv"]], w=[AB["yatt"]])
                    b = bank()
                    for j in range(4):
                        K.op("pe", lambda q, j=j: q.transpose(out=psb(b)[:, j * 128:(j + 1) * 128], in_=yatt[:, j * 128:(j + 1) * 128], identity=identb[:]), r=[AB["yatt"], Bc], w=[PB[b]])
                    K.op("act", lambda q: q.activation(out=yattT[:, :, cc], in_=psb(b)[:, 0:512].rearrange("p (j t) -> p j t", j=4), func=AF.Copy), r=[PB[b]], w=[B_yattT])

                idx_topk(0)
                for c in range(4):
                    if c + 1 < 4:
                        idx_topk(c + 1)
                    heads(c)

                if dg:
                    dump(f"ynT{sc}", ynT[:].rearrange("p k t -> p (k t)"), [B_ynT], 4096)
                    dump(f"yattT{sc}", yattT[:].rearrange("p k t -> p (k t)"), [B_yattT], 2048)
                MB = mrg_bufs
                K.handoff(list(AB.values()), list(MB.values()))
                for f in range(8):
                    w1, bw1 = wtile()
                    b1 = bank()
                    mm_fm(w1, bw1, 8, ynT, B_ynT, b1)
                    w2, bw2 = wtile()
                    b2 = bank()
                    mm_fm(w2, bw2, 4, yattT, B_yattT, b2)
                    b3 = bank()
                    mm_fm(w2, bw2, 4, ypoolT, B_ypoolT, b3, wkt0=4)
                    ups = [b1, b2, b3]
                    for br in range(3):
                        wg, bwg = wtile()
                        bg = bank()
                        mm_fm(wg, bwg, 8, xnT, B_xnT, bg)
                        K.op("act", lambda q, bg=bg, br=br: q.activation(out=sig[br], in_=ps[:, bg, :], func=AF.Sigmoid), r=[PB[bg]], w=[MB[f"sig{br}"]])
                        K.op("dve", lambda q, br=br, bu_=ups[br]: q.tensor_tensor(out=mt[br], in0=ps[:, bu_, :], in1=sig[br], op=ALU.mult), r=[PB[ups[br]], MB[f"sig{br}"]], w=[MB[f"mt{br}"]])
                    K.op("pool", lambda q: q.tensor_tensor(out=mt[0], in0=mt[0], in1=mt[1], op=ALU.add), r=[MB["mt0"], MB["mt1"]], w=[MB["mt0"]])
                    K.op("pool", lambda q, f=f: q.tensor_tensor(out=mergedT[:, f, :], in0=mt[0], in1=mt[2], op=ALU.add), r=[MB["mt0"], MB["mt2"]], w=[MB["mergedT"]])
                def evac_res(cgp, b4):
                    K.op("dve", lambda q: q.tensor_tensor(out=hS[:, :, cgp * 512:(cgp + 1) * 512], in0=ps[:, b4:b4 + 4, :], in1=hS[:, :, cgp * 512:(cgp + 1) * 512], op=ALU.add),
                         r=[PB[b4], PB[b4 + 1], PB[b4 + 2], PB[b4 + 3], B_hS], w=[B_hS])
                mm_tm_wide(8, mergedT, MB["mergedT"], evac_res)

                if dg:
                    dump(f"mergedT{sc}", mergedT.rearrange("p k t -> p (k t)"), [MB["mergedT"]], 4096)
                    dump(f"h1_{sc}", hS[:].rearrange("p c d -> p (c d)"), [B_hS], 4096)
                FB = ffn_bufs
                rms_T(80, xnT, B_xnT)
                K.handoff(list(MB.values()), list(FB.values()))
                for j in range(22):
                    wa, bwa = wtile()
                    ba = bank()
                    mm_fm(wa, bwa, 8, xnT, B_xnT, ba)
                    K.op("act", lambda q, ba=ba, j=j: q.activation(out=sa[j % 2], in_=ps[:, ba, :], func=AF.Silu), r=[PB[ba]], w=[FB[f"sa{j % 2}"]])
                    wb_, bwb = wtile()
                    bb = bank()
                    mm_fm(wb_, bwb, 8, xnT, B_xnT, bb)
                    K.op("dve", lambda q, bb=bb, j=j: q.tensor_tensor(out=hidT[:, j, :], in0=ps[:, bb, :], in1=sa[j % 2], op=ALU.mult), r=[PB[bb], FB[f"sa{j % 2}"]], w=[FB["hidT"]])
                mm_tm_wide(22, hidT, FB["hidT"], evac_res)
                assert cur["slot"] == NSLOT, cur["slot"]

                if last_layer:
                    for c in range(4):
                        K.op("act", lambda q, c=c: q.activation(out=junk[:], in_=hS[:, c, :], func=AF.Square, accum_out=ssq[:, c:c + 1]), r=[B_hS], w=[B_junk, B_ssq])
                    K.op("act", lambda q: q.activation(out=ssq[:, 4:8], in_=ssq[:, 0:4], func=AF.Sqrt, scale=1.0 / D, bias=EPS), r=[B_ssq], w=[B_ssq])
                    K.op("dve", lambda q: q.reciprocal(out=ssq[:, 4:8], in_=ssq[:, 4:8]), r=[B_ssq], w=[B_ssq])
                    for c in range(4):
                        K.op("dve", lambda q, c=c: q.scalar_tensor_tensor(out=hS[:, c, :], in0=hS[:, c, :], scalar=ssq[:, 4 + c:5 + c], in1=fnw[:], op0=ALU.mult, op1=ALU.mult),
                             r=[B_hS, B_ssq, Bc], w=[B_hS])
                    K.dma("act", y_d[t0:t0 + S, :].rearrange("(c p) d -> p c d", p=128), hS[:], r=[B_hS], w=[B_out])
                else:
                    K.dma("act", hb_d[t0:t0 + S, :].rearrange("(c p) d -> p c d", p=128), hS[:], r=[B_hS], w=[B_hb[sc]])
                    pass
        K.final_wait("act", [B_out])
        K.final_wait("sp", [B_out])
        K.emit()
    return nc, K


B_out = Buf()
B_hb = [Buf() for _ in range(64)]


def _tile_k(wcols, kt_total=8):
    Kd, ncol = wcols.shape
    nkt = Kd // 128
    out = np.zeros((128, kt_total, 128), np.float32)
    out[:, :nkt, :ncol] = wcols.reshape(nkt, 128, ncol).transpose(1, 0, 2)
    return out


def _tile_wide(w):
    Kd = w.shape[0]
    nkt = Kd // 128
    out = []
    for cgp in range(2):
        t = w[:, cgp * 512:(cgp + 1) * 512].reshape(nkt, 128, 512).transpose(1, 0, 2)
        for si in range((nkt + 1) // 2):
            s_ = np.zeros((128, 2, 512), np.float32)
            n = min(2, nkt - si * 2)
            s_[:, 0:n, :] = t[:, si * 2:si * 2 + n, :]
            out.append(s_.reshape(128, 8, 128))
    return out


def pack_layer(inp, l):
    w_in = inp["w_in"][l]
    slots = []
    pw = np.zeros((128, 8, 128), np.float32)
    for g in range(4):
        pw[:, g, :] = inp["pool_w"][l, g]
    slots.append(pw)
    for j in range(12):
        slots.append(_tile_k(w_in[:, 1024 + 128 * j:1024 + 128 * (j + 1)]))
    slots.extend(_tile_wide(w_in[:, 0:1024]))
    slots.append(_tile_k(np.concatenate([w_in[:, 2560:2576], w_in[:, 3664:3668]], axis=1)))
    slots.append(_tile_k(w_in[:, 3216:3344]))
    for i in range(4):
        slots.append(_tile_k(np.concatenate([w_in[:, 2576 + 64 * i:2576 + 64 * (i + 1)], w_in[:, 2576 + 64 * (4 + i):2576 + 64 * (5 + i)]], axis=1)))
    slots.append(_tile_k(w_in[:, 3088:3216]))
    for i in range(2):
        slots.append(_tile_k(np.concatenate([w_in[:, 3344 + 64 * i:3344 + 64 * (i + 1)], w_in[:, 3344 + 64 * (2 + i):3344 + 64 * (3 + i)]], axis=1)))
    slots.append(_tile_k(np.concatenate([w_in[:, 3600:3664], w_in[:, 3600:3664]], axis=1)))
    for g in range(4):
        slots.append(_tile_k(w_in[:, 3668 + 128 * g:3668 + 128 * (g + 1)]))
    for f in range(8):
        cs = slice(128 * f, 128 * (f + 1))
        slots.append(_tile_k(inp["w_up_ssd"][l][:, cs]))
        ap = np.zeros((128, 8, 128), np.float32)
        ap[:, 0:4, :] = inp["w_up_attn"][l][:, cs].reshape(4, 128, 128).transpose(1, 0, 2)
        ap[:, 4:8, :] = inp["w_up_pool"][l][:, cs].reshape(4, 128, 128).transpose(1, 0, 2)
        slots.append(ap)
        for br in range(3):
            c0 = 4180 + (br * 8 + f) * 128
            slots.append(_tile_k(w_in[:, c0:c0 + 128]))
    slots.extend(_tile_wide(inp["w_out"][l]))
    wfi = inp["w_ffn_in"][l]
    for j in range(22):
        slots.append(_tile_k(wfi[:, 128 * j:128 * (j + 1)]))
        slots.append(_tile_k(wfi[:, 2816 + 128 * j:2816 + 128 * (j + 1)]))
    slots.extend(_tile_wide(inp["w_ffn_out"][l]))
    assert len(slots) == NSLOT, len(slots)
    W = np.stack(slots).reshape(NSLOT, 128, 1024)
    pp = np.zeros((128, NPP), np.float32)
    cw = inp["conv_w"][l]
    pp[:, 0:48] = cw.reshape(4, 12, 128).transpose(2, 1, 0).reshape(128, 48)
    pp[:, 48:60] = inp["conv_b"][l].reshape(12, 128).T
    pp[:, 60:64] = inp["pool_scale"][l].reshape(4, 128).T
    pp[:, 64:72] = inp["norm1_w"][l].reshape(8, 128).T
    pp[:, 72:80] = inp["ssd_norm_w"][l].reshape(8, 128).T
    pp[:, 80:88] = inp["norm2_w"][l].reshape(8, 128).T
    rows = np.concatenate([inp["dt_bias"][l], inp["a_log"][l], inp["d_skip"][l]])[None, :].repeat(128, 0).astype(np.float32)
    return W, pp, rows


def make_consts():
    k = np.arange(128)
    cst = np.zeros((128, 8, 128), np.float32)
    cst[:, 0, :] = np.eye(128)
    cst[:, 1, :] = (k[:, None] <= k[None, :])
    cst[:, 2, :] = (k[:, None] > k[None, :])
    cst[:, 3, :] = 1.0
    cst[:, 4, :] = np.where(k[None, :] >= k[:, None], 0.0, -30000.0)
    cst[:, 5, :] = np.where(k[None, :] <= k[:, None], 0.0, NEG)
    oh = np.zeros((128, 128), np.float32)
    oh[0:16, 0:16] = np.eye(16)
    oh[32:48, 16:32] = -np.eye(16)
    cst[:, 6, :] = oh
    p2 = 2.0 ** -(np.arange(28) + 1.0)
    cst[:, 7, 0:28] = p2[None, :]
    cst[:, 7, 28:56] = -p2[None, :]
    rc16 = np.zeros((128, 4, 16), np.float32)
    for g, w in enumerate(POOL_WINDOWS):
        rc16[:, g, :] = 1.0 / np.minimum(np.arange(1, 17), w)
    return cst, rc16


_CACHE = {}


def run(inputs, T=SEQ, L=DEPTH, ncores=NCORES, dbg=False):
    inp = {k: np.asarray(v, np.float32) for k, v in inputs.items()}
    key = (T, L, dbg)
    if key not in _CACHE:
        _CACHE[key] = build(T, L, dbg)
    nc, _ = _CACHE[key]
    Ws, pps, rws = [], [], []
    for l in range(L):
        W, pp, rows = pack_layer(inp, l)
        Ws.append(W)
        pps.append(pp)
        rws.append(rows)
    W = np.stack(Ws)
    pp = np.stack(pps)
    rows = np.stack(rws)
    cst, rc16 = make_consts()
    fnw = np.ascontiguousarray(np.broadcast_to(inp["final_norm_w"][None, :], (128, D))).astype(np.float32)
    in_maps = []
    for b in range(ncores):
        in_maps.append({"x": np.ascontiguousarray(inp["x"][b, :T]), "W": W, "pp": pp, "rows": rows, "fnw": fnw, "cst": cst, "rc16": rc16})
    res = run_bass_kernel_spmd(nc, in_maps, core_ids=list(range(ncores)))
    if dbg:
        return res.results
    return np.stack([np.asarray(r["y"], np.float32) for r in res.results])


def kernel(**inputs):
    return run(inputs)
```

```python
import numpy as np
from contextlib import ExitStack
import concourse.bass as bass
import concourse.mybir as mybir
from concourse.bass_utils import run_bass_kernel_spmd

F32 = mybir.dt.float32
BF16 = mybir.dt.bfloat16
ALU = mybir.AluOpType
AF = mybir.ActivationFunctionType
AX = mybir.AxisListType

D = 1024
SEQ = 4096
DEPTH = 4
NCORES = 8
S = 512
NSLOT = 149
NPP = 88
EPS = 1e-6
SEM_LIMIT = 30000
NEG = -1.0e30
POOL_WINDOWS = (2, 4, 8, 16)


class Buf:
    __slots__ = ("w", "r")

    def __init__(self):
        self.w = None
        self.r = {}


class Ctr:
    def __init__(self, K, name, step):
        self.K, self.name, self.step = K, name, step
        self.n = 0
        self.h = K.new_sem(f"{name}_0")
        self.v = 0

    def next(self):
        if self.v + self.step > SEM_LIMIT:
            self.n += 1
            self.h = self.K.new_sem(f"{self.name}_{self.n}")
            self.v = 0
        self.v += self.step
        return (self.h, self.v)


class Eng:
    def __init__(self, K, name):
        self.name = name
        self.ops = []
        self.ctr = Ctr(K, "s_" + name, 1)
        self.seen = {}
        self.own = set()


class Kern:
    def __init__(self, nc, stack, n_dma_sems=12):
        self.nc = nc
        self.stack = stack
        self.nsem = 0
        self.E = {n: Eng(self, n) for n in ("pe", "act", "dve", "pool", "sp")}
        self.dma_ctrs = [Ctr(self, f"d{i}", 16) for i in range(n_dma_sems)]
        self.dma_rr = 0
        self.nops = 0

    def new_sem(self, name):
        self.nsem += 1
        return self.stack.enter_context(self.nc.semaphore(name))

    def _deps(self, e, r, w):
        deps = {}
        for b in r:
            if b.w is not None and deps.get(b.w[0], 0) < b.w[1]:
                deps[b.w[0]] = b.w[1]
        for b in w:
            if b.w is not None and deps.get(b.w[0], 0) < b.w[1]:
                deps[b.w[0]] = b.w[1]
            for h, v in b.r.items():
                if deps.get(h, 0) < v:
                    deps[h] = v
        waits = []
        for h, v in deps.items():
            if e.name == "pe" and h in e.own:
                continue
            if e.seen.get(h, 0) < v:
                waits.append((h, v))
                e.seen[h] = v
        return waits

    def _commit(self, tok, r, w):
        h, v = tok
        for b in w:
            b.w = tok
            b.r = {}
        for b in r:
            if b.w is not tok and b.r.get(h, 0) < v:
                b.r[h] = v

    def op(self, en, fn, r=(), w=()):
        e = self.E[en]
        waits = self._deps(e, r, w)
        tok = e.ctr.next()
        e.own.add(tok[0])
        e.ops.append((waits, fn, tok[0], 1))
        self._commit(tok, r, w)
        self.nops += 1

    def dma(self, en, out, in_, r=(), w=()):
        e = self.E[en]
        waits = self._deps(e, r, w)
        c = self.dma_ctrs[self.dma_rr]
        self.dma_rr = (self.dma_rr + 1) % len(self.dma_ctrs)
        if c.v > 0 and e.seen.get(c.h, 0) < c.v:
            waits.append((c.h, c.v))
            e.seen[c.h] = c.v
        tok = c.next()
        e.ops.append((waits, (lambda q: q.dma_start(out=out, in_=in_)), tok[0], 16))
        self._commit(tok, r, w)
        self.nops += 1

    def handoff(self, src, dst):
        u = {}
        for b in src:
            if b.w is not None and u.get(b.w[0], 0) < b.w[1]:
                u[b.w[0]] = b.w[1]
            for h, v in b.r.items():
                if u.get(h, 0) < v:
                    u[h] = v
        for b in dst:
            for h, v in u.items():
                if b.r.get(h, 0) < v:
                    b.r[h] = v

    def final_wait(self, en, bufs):
        e = self.E[en]
        waits = self._deps(e, bufs, ())
        e.ops.append((waits, None, None, 0))

    def emit(self):
        nc = self.nc
        E = self.E

        def run(q, e):
            for waits, fn, sem, inc in e.ops:
                for h, v in waits:
                    q.wait_ge(h, v)
                if fn is not None:
                    fn(q).then_inc(sem, inc)

        with nc.Block() as block:
            @block.tensor
            def _(q):
                run(q, E["pe"])

            @block.scalar
            def _(q):
                run(q, E["act"])

            @block.vector
            def _(q):
                run(q, E["dve"])

            @block.gpsimd
            def _(q):
                run(q, E["pool"])

            @block.sync
            def _(q):
                run(q, E["sp"])


def build(T=SEQ, L=DEPTH, dbg=False):
    NCH = T // 128
    NSC = T // S
    nc = bass.Bass("TRN2", target_bir_lowering=False)

    def din(name, shape):
        return nc.dram_tensor(name, shape, F32, kind="ExternalInput").ap()

    x_d = din("x", [T, D])
    W_d = din("W", [L, NSLOT, 128, 1024])
    pp_d = din("pp", [L, 128, NPP])
    rows_d = din("rows", [L, 128, 48])
    fnw_d = din("fnw", [128, D])
    cst_d = din("cst", [128, 8, 128])
    rc16_d = din("rc16", [128, 4, 16])
    y_d = nc.dram_tensor("y", [T, D], F32, kind="ExternalOutput").ap()
    hb_d = nc.dram_tensor("hbuf", [T, D], F32, kind="Internal").ap()

    with ExitStack() as st:
        K = Kern(nc, st)

        def sb(name, shape, dt=F32):
            return st.enter_context(nc.sbuf_tensor("sb_" + name, shape, dt))

        ps = st.enter_context(nc.psum_tensor("ps", [128, 8, 512], F32))
        PB = [Buf() for _ in range(8)]
        pbs = [0]

        def bank(excl=()):
            while True:
                i = pbs[0]
                pbs[0] = (i + 1) % 8
                if i not in excl:
                    return i

        def bank2(excl=()):
            while True:
                if pbs[0] % 2:
                    pbs[0] = (pbs[0] + 1) % 8
                i = pbs[0]
                pbs[0] = (i + 2) % 8
                if i not in excl and (i + 1) not in excl:
                    return i

        def bank4():
            i = 0 if pbs[0] <= 0 or pbs[0] > 4 else 4
            pbs[0] = (i + 4) % 8
            return i

        def psb(i):
            return ps[:, i, :].bitcast(BF16)

        B_dbg, B_dbgout = Buf(), Buf()
        dbgbuf = sb("dbgbuf", [128, 4096]) if dbg else None

        def dump(name, view, bufs, n):
            if not dbg:
                return
            K.op("pool", lambda q: q.tensor_copy(out=dbgbuf[:view.shape[0], 0:n], in_=view), r=bufs, w=[B_dbg])
            d = nc.dram_tensor("dbg_" + name, [view.shape[0], n], F32, kind="ExternalOutput").ap()
            K.dma("sp", d, dbgbuf[:view.shape[0], 0:n], r=[B_dbg], w=[B_dbgout])

        cst = sb("cst", [128, 8, 128])
        rc16 = sb("rc16", [128, 4, 16])
        identb = sb("identb", [128, 128], BF16)
        fnw = sb("fnw", [128, D])
        LHS = sb("LHS", [64, 16, 128])
        RHS = sb("RHS", [64, 128])
        Bc = Buf()
        identf = cst[:, 0, :]
        tri_le = cst[:, 1, :]
        tri_gt = cst[:, 2, :]
        onesf = cst[:, 3, :]
        maskneg = cst[:, 4, :]
        cmaskneg = cst[:, 5, :]
        oh = cst[:, 6, :]
        K.dma("sp", cst[:], cst_d, w=[Bc])
        K.dma("sp", rc16[:], rc16_d, w=[Bc])
        K.dma("sp", fnw[:], fnw_d, w=[Bc])
        K.op("dve", lambda q: q.tensor_copy(out=identb[:], in_=identf), r=[Bc], w=[Bc])
        identBIG = sb("identBIG", [128, 128], BF16)
        K.op("dve", lambda q: q.tensor_scalar(out=identBIG[:], in0=identf, scalar1=30000.0, scalar2=None, op0=ALU.mult), r=[Bc], w=[Bc])
        B_LHS, B_RHS = Buf(), Buf()
        K.op("pool", lambda q: q.memset(LHS[:], 0.0), w=[B_LHS])
        K.op("pool", lambda q: q.memset(RHS[:], 0.0), w=[B_RHS])
        K.op("dve", lambda q: q.tensor_copy(out=LHS[0:16, :, :], in_=oh[0:16, 0:16].unsqueeze(2).to_broadcast([16, 16, 128])), r=[Bc], w=[B_LHS])
        K.op("pool", lambda q: q.memset(RHS[32:48, :], 1.0), w=[B_RHS])

        pp = sb("pp", [128, NPP])
        rows = sb("rows", [128, 48])
        A_b = sb("A_b", [128, 16])
        B_par = Buf()
        hSb = [sb(f"hS{i}", [128, 4, D]) for i in range(2)]
        B_hSb = [Buf(), Buf()]
        xn_tok = [sb(f"xn_tok{i}", [128, D], BF16) for i in range(2)]
        B_xn = [Buf(), Buf()]
        junk = sb("junk", [128, D], BF16)
        B_junk = Buf()
        ssq = sb("ssq", [128, 8])
        B_ssq = Buf()
        xnT = sb("xnT", [128, 8, S], BF16)
        B_xnT = Buf()
        qT = sb("qT", [128, 4, S], BF16)
        B_qT = Buf()
        kT = sb("kT", [128, T], BF16)
        B_kT = Buf()
        qiT = sb("qiT", [128, 2, S], BF16)
        B_qiT = Buf()
        kiT = sb("kiT", [128, T], BF16)
        B_kiT = Buf()
        Vh = sb("Vh", [128, NCH, 128], BF16)
        B_V = Buf()
        dw = sb("dw", [128, 4, 20])
        B_dw = Buf()
        state = sb("state", [128, 16, 64])
        state_bf = sb("state_bf", [128, 16, 64], BF16)
        B_state, B_statebf = Buf(), Buf()
        chalo = sb("chalo", [128, 12, 3], BF16)
        B_chalo = Buf()
        phalo = sb("phalo", [128, 4, 15])
        B_phalo = Buf()
        ynT = sb("ynT", [128, 8, S], BF16)
        B_ynT = Buf()
        yattT = sb("yattT", [128, 4, S], BF16)
        B_yattT = Buf()
        ypoolT = sb("ypoolT", [128, 4, S], BF16)
        B_ypoolT = Buf()
        NS, NB = 3, 6
        wbf = [sb(f"wbf{i}", [128, 8, 128], BF16) for i in range(NB)]
        B_wst = [Buf() for _ in range(NS)]
        B_wbf = [Buf() for _ in range(NB)]
        upre = [sb(f"upre{i}", [128, 515], BF16) for i in range(2)]
        B_upre = [Buf(), Buf()]
        ubuf = [sb(f"ubuf{i}", [128, 527]) for i in range(2)]
        B_ubuf = [Buf(), Buf()]
        ptmp = [sb(f"ptmp{i}", [128, 527]) for i in range(2)]
        B_ptmp = [Buf(), Buf()]
        pooled = [sb(f"pooled{i}", [128, S], BF16) for i in range(2)]
        B_pooled = [Buf(), Buf()]
        small = sb("small", [128, 256])
        B_sm = {n: Buf() for n in ("dt", "a48", "e3", "dtdec", "m8", "thr", "rs", "rsum", "rinv")}
        bis = sb("bis", [128, 64])
        B_bis = {n: Buf() for n in ("D", "cnt", "u", "mid", "lo", "d0")}
        rsb = sb("rsb", [128, 8, 8])
        B_rsb = [Buf() for _ in range(8)]

        R1N = 15872
        R1 = sb("R1", [128, R1N])

        def carve(off_f32, nelem, dt):
            if dt == F32:
                return R1[:, off_f32:off_f32 + nelem]
            return R1[:, off_f32:off_f32 + nelem // 2].bitcast(BF16)

        o = 0
        xbc_act = carve(o, 12 * S, BF16).rearrange("p (j t) -> p j t", j=12); o += 6 * S
        zs = carve(o, 4 * D, BF16).rearrange("p (c d) -> p c d", c=4); o += 2 * D
        xs_tok = carve(o, 4 * D, BF16).rearrange("p (c d) -> p c d", c=4); o += 2 * D
        B_tok = carve(o, 4 * 256, BF16).rearrange("p (c d) -> p c d", c=4); o += 512
        ctmp = carve(o, S, F32); o += S
        x_dt = carve(o, D, BF16); o += D // 2
        xdec = carve(o, D, BF16); o += D // 2
        xsd = carve(o, D, BF16); o += D // 2
        LexpG = [carve(o + i * (D // 2), D, BF16).rearrange("p (j t) -> p j t", j=8) for i in range(2)]; o += D
        MtG = [carve(o + i * (D // 2), D, BF16).rearrange("p (j t) -> p j t", j=8) for i in range(2)]; o += D
        cbG = [carve(o + i * 128, 128, F32) for i in range(2)]; o += 256
        yacc = carve(o, D, F32); o += D
        ynb = carve(o, D, BF16); o += D // 2
        assert o <= R1N, o
        ssd_bufs = {n: Buf() for n in ("xbc_act", "zs", "xs_tok", "B_tok", "ctmp", "x_dt", "xdec", "xsd", "Lexp0", "Lexp1", "Mt0", "Mt1", "cb0", "cb1", "yacc", "ynb")}
        o = 0
        score = carve(o, T, F32); o += T
        work = carve(o, T, F32); o += T
        rtmp = [carve(o + i * S, S, F32) for i in range(2)]; o += 2 * S
        Pm = [carve(o + i * (S // 2), S, BF16) for i in range(3)]; o += 3 * (S // 2)
        PmT = [carve(o + i * (S // 2), S, BF16) for i in range(3)]; o += 3 * (S // 2)
        yatt = carve(o, 512, BF16); o += 256
        mbias = [carve(o + i * (T // 2), T, BF16) for i in range(2)]; o += T
        assert o <= R1N, o
        att_bufs = {n: Buf() for n in ("score", "work", "rtmp0", "rtmp1", "Pm0", "Pm1", "Pm2", "PmT0", "PmT1", "PmT2", "yatt", "mb0", "mb1")}
        o = 0
        mergedT = carve(o, 8 * S, BF16).rearrange("p (j t) -> p j t", j=8); o += 4 * S
        sig = [carve(o + i * (S // 2), S, BF16) for i in range(3)]; o += 3 * (S // 2)
        mt = [carve(o + i * S, S, F32) for i in range(3)]; o += 3 * S
        assert o <= R1N, o
        mrg_bufs = {n: Buf() for n in ("mergedT", "sig0", "sig1", "sig2", "mt0", "mt1", "mt2")}
        o = 0
        hidT = carve(o, 22 * S, BF16).rearrange("p (j t) -> p j t", j=22); o += 11 * S
        sa = [carve(o + i * (S // 2), S, BF16) for i in range(2)]; o += S
        assert o <= R1N, o
        ffn_bufs = {n: Buf() for n in ("hidT", "sa0", "sa1")}

        Wb_d = nc.dram_tensor("Wb", [L, NSLOT, 128, 1024], BF16, kind="Internal").ap()
        B_wb = [[Buf() for _ in range(NSLOT)] for _ in range(L)]
        wst = [carve(i * 1024, 1024, F32).rearrange("p (k c) -> p k c", k=8) for i in range(NS)]
        cvt = [carve(NS * 1024 + i * 512, 1024, BF16) for i in range(4)]
        B_cvt = [Buf() for _ in range(4)]
        n_ = 0
        for l_ in range(L):
            for slot_ in range(NSLOT):
                s_, c_ = n_ % NS, n_ % 4
                K.dma("sp", wst[s_].rearrange("p k c -> p (k c)"), W_d[l_, slot_], w=[B_wst[s_]])
                if n_ % 2 == 0:
                    K.op("dve", lambda q, s_=s_, c_=c_: q.tensor_copy(out=cvt[c_], in_=wst[s_].rearrange("p k c -> p (k c)")), r=[B_wst[s_]], w=[B_cvt[c_]])
                else:
                    K.op("act", lambda q, s_=s_, c_=c_: q.activation(out=cvt[c_], in_=wst[s_].rearrange("p k c -> p (k c)"), func=AF.Copy), r=[B_wst[s_]], w=[B_cvt[c_]])
                K.dma("pool", Wb_d[l_, slot_], cvt[c_], r=[B_cvt[c_]], w=[B_wb[l_][slot_]])
                n_ += 1
        wctr = [0]
        cur = {"l": 0, "slot": 0}

        def wtile():
            i = wctr[0]
            wctr[0] += 1
            l, slot = cur["l"], cur["slot"]
            cur["slot"] += 1
            b_ = i % NB
            K.dma("sp", wbf[b_][:].rearrange("p k c -> p (k c)"), Wb_d[l, slot], r=[B_wb[l][slot]], w=[B_wbf[b_]])
            return wbf[b_], B_wbf[b_]

        def rms_T(nwoff, dstT, B_dst, hS, B_hS):
            for c in range(4):
                K.op("act", lambda q, c=c: q.activation(out=junk[:], in_=hS[:, c, :], func=AF.Square, accum_out=ssq[:, c:c + 1]),
                     r=[B_hS], w=[B_junk, B_ssq])
            K.op("act", lambda q: q.activation(out=ssq[:, 4:8], in_=ssq[:, 0:4], func=AF.Sqrt, scale=1.0 / D, bias=EPS), r=[B_ssq], w=[B_ssq])
            K.op("dve", lambda q: q.reciprocal(out=ssq[:, 4:8], in_=ssq[:, 4:8]), r=[B_ssq], w=[B_ssq])
            for c in range(4):
                xt, bx = xn_tok[c % 2], B_xn[c % 2]
                K.op("dve", lambda q, c=c, xt=xt: q.tensor_scalar(out=xt[:], in0=hS[:, c, :], scalar1=ssq[:, 4 + c:5 + c], scalar2=None, op0=ALU.mult),
                     r=[B_hS, B_ssq], w=[bx])
                for half in range(2):
                    b = bank()
                    for j in range(4):
                        kt = half * 4 + j
                        K.op("pe", lambda q, b=b, j=j, kt=kt, xt=xt: q.transpose(out=psb(b)[:, j * 128:(j + 1) * 128], in_=xt[:, kt * 128:(kt + 1) * 128], identity=identb[:]),
                             r=[bx, Bc], w=[PB[b]])
                    K.op("dve", lambda q, b=b, half=half, c=c: q.tensor_tensor(
                        out=dstT[:, half * 4:half * 4 + 4, c * 128:(c + 1) * 128],
                        in0=psb(b)[:, 0:512].rearrange("p (j t) -> p j t", j=4),
                        in1=pp[:, nwoff + half * 4:nwoff + half * 4 + 4].unsqueeze(2).to_broadcast([128, 4, 128]), op=ALU.mult),
                        r=[PB[b], B_par], w=[B_dst])

        def mm_fm(wt, bw, KT, rhsT, B_rhs, b, kt0=0, first=True, last=True, wkt0=0):
            for kt in range(KT):
                K.op("pe", lambda q, kt=kt: q.matmul(ps[:, b, :], lhsT=wt[:, wkt0 + kt, :], rhs=rhsT[:, kt0 + kt, :],
                                                    start=(first and kt == 0), stop=(last and kt == KT - 1)),
                     r=[bw, B_rhs], w=[PB[b]])

        def mm_tm(wt, bw, KT, lhsT_src, B_l, b, ncols=128, cstride=128, wkt0=0, kt0=0, first=True, last=True):
            for c in range(4):
                for kt in range(KT):
                    K.op("pe", lambda q, c=c, kt=kt: q.matmul(ps[:, b, c * cstride:c * cstride + ncols],
                                                              lhsT=lhsT_src[:, kt0 + kt, c * 128:(c + 1) * 128], rhs=wt[:, wkt0 + kt, 0:ncols],
                                                              start=(first and kt == 0), stop=(last and kt == KT - 1)),
                         r=[bw, B_l], w=[PB[b]])

        def mm_tm_wide(nkt_total, lhsT_src, B_l, evac):
            for cgp in range(2):
                b4 = bank4()
                nsl = (nkt_total + 1) // 2
                for si in range(nsl):
                    wt, bw = wtile()
                    ww = wt[:].rearrange("p k c -> p (k c)").rearrange("p (k c) -> p k c", k=2)
                    for c in range(4):
                        for k2 in range(2):
                            kt = si * 2 + k2
                            if kt >= nkt_total:
                                continue
                            K.op("pe", lambda q, c=c, k2=k2, kt=kt, ww=ww, b4=b4: q.matmul(ps[:, b4 + c, :], lhsT=lhsT_src[:, kt, c * 128:(c + 1) * 128], rhs=ww[:, k2, :],
                                                                                       start=(kt == 0), stop=(kt == nkt_total - 1)),
                                 r=[bw, B_l], w=[PB[b4 + c]])
                evac(cgp, b4)

        for l in range(L):
            src_d = x_d if l == 0 else hb_d
            last_layer = (l == L - 1)
            K.dma("sp", pp[:], pp_d[l], w=[B_par])
            K.dma("sp", rows[:], rows_d[l], w=[B_par])
            K.op("act", lambda q: q.activation(out=A_b[:], in_=rows[:, 16:32], func=AF.Exp), r=[B_par], w=[B_par])
            K.op("dve", lambda q: q.tensor_scalar(out=A_b[:], in0=A_b[:], scalar1=-1.0, scalar2=None, op0=ALU.mult), r=[B_par], w=[B_par])
            K.op("pool", lambda q: q.memset(state[:], 0.0), w=[B_state])
            K.op("pool", lambda q: q.memset(state_bf[:], 0.0), w=[B_statebf])
            K.op("pool", lambda q: q.memset(chalo[:], 0.0), w=[B_chalo])
            K.op("pool", lambda q: q.memset(phalo[:], 0.0), w=[B_phalo])

            def load_h(l_, sc_):
                n_ = l_ * NSC + sc_
                sd = x_d if l_ == 0 else hb_d
                K.dma("sp", hSb[n_ % 2][:], sd[sc_ * S:(sc_ + 1) * S, :].rearrange("(c p) d -> p c d", p=128), r=([B_hb[sc_]] if l_ > 0 else []), w=[B_hSb[n_ % 2]])

            def superchunk(sc, hS, B_hS):
                t0 = sc * S
                cg0 = sc * 4
                cur["l"], cur["slot"] = l, 0
                SB = ssd_bufs
                if l == 0 and sc == 0:
                    load_h(0, 0)
                rms_T(64, xnT, B_xnT, hS, B_hS)
                K.handoff(list(ffn_bufs.values()) + list(mrg_bufs.values()) + list(att_bufs.values()) + B_wst + B_cvt, list(SB.values()))
                dg = dbg and l == 0 and sc < 2
                if dg:
                    dump(f"xnT{sc}", xnT[:].rearrange("p k t -> p (k t)"), [B_xnT], 4096)

                wpool, bwpool = wtile()
                K.op("pool", lambda q, wpool=wpool: q.tensor_copy(out=junk[:, 0:512].rearrange("p (g c) -> p g c", g=4), in_=wpool[:, 0:4, :]), r=[bwpool], w=[B_junk])
                poolw = junk[:, 0:512].rearrange("p (g c) -> p g c", g=4)

                for j in range(12):
                    wt, bw = wtile()
                    b = bank()
                    mm_fm(wt, bw, 8, xnT, B_xnT, b)
                    up, bu = upre[j % 2], B_upre[j % 2]
                    K.op("act", lambda q, b=b, up=up: q.activation(out=up[:, 3:515], in_=ps[:, b, :], func=AF.Copy), r=[PB[b]], w=[bu])
                    K.op("pool", lambda q, j=j, up=up: q.tensor_copy(out=up[:, 0:3], in_=chalo[:, j, :]), r=[B_chalo], w=[bu])
                    K.op("dve", lambda q, j=j, up=up: q.tensor_scalar(out=ctmp, in0=up[:, 0:512], scalar1=pp[:, j * 4:j * 4 + 1], scalar2=None, op0=ALU.mult),
                         r=[bu, B_par], w=[SB["ctmp"]])
                    for k in range(1, 4):
                        K.op("dve", lambda q, j=j, k=k, up=up: q.scalar_tensor_tensor(out=ctmp, in0=up[:, k:k + 512], scalar=pp[:, j * 4 + k:j * 4 + k + 1], in1=ctmp, op0=ALU.mult, op1=ALU.add),
                             r=[bu, B_par, SB["ctmp"]], w=[SB["ctmp"]])
                    K.op("pool", lambda q, j=j, up=up: q.tensor_copy(out=chalo[:, j, :], in_=up[:, 512:515]), r=[bu], w=[B_chalo])
                    K.op("act", lambda q, j=j: q.activation(out=xbc_act[:, j, :], in_=ctmp, func=AF.Silu, bias=pp[:, 48 + j:49 + j]),
                         r=[SB["ctmp"], B_par], w=[SB["xbc_act"]])
                for c in range(4):
                    for half in range(2):
                        b = bank()
                        for j in range(4):
                            K.op("pe", lambda q, b=b, j=j, c=c, half=half: q.transpose(out=psb(b)[:, j * 128:(j + 1) * 128], in_=xbc_act[:, half * 4 + j, c * 128:(c + 1) * 128], identity=identb[:]),
                                 r=[SB["xbc_act"], Bc], w=[PB[b]])
                        K.op("act", lambda q, b=b, c=c, half=half: q.activation(out=xs_tok[:, c, half * 512:(half + 1) * 512], in_=psb(b)[:, 0:512], func=AF.Copy),
                             r=[PB[b]], w=[SB["xs_tok"]])
                    b = bank()
                    for g in range(2):
                        K.op("pe", lambda q, b=b, g=g, c=c: q.transpose(out=psb(b)[:, g * 128:(g + 1) * 128], in_=xbc_act[:, 8 + g, c * 128:(c + 1) * 128], identity=identb[:]),
                             r=[SB["xbc_act"], Bc], w=[PB[b]])
                    K.op("act", lambda q, b=b, c=c: q.activation(out=B_tok[:, c, :], in_=psb(b)[:, 0:256], func=AF.Copy), r=[PB[b]], w=[SB["B_tok"]])

                if dg:
                    dump(f"xbc{sc}a", xbc_act[:, 0:8, :].rearrange("p k t -> p (k t)"), [SB["xbc_act"]], 4096)
                    dump(f"xbc{sc}b", xbc_act[:, 8:12, :].rearrange("p k t -> p (k t)"), [SB["xbc_act"]], 2048)
                    dump(f"xstok{sc}", xs_tok.rearrange("p c d -> p (c d)"), [SB["xs_tok"]], 4096)
                def evac_z(cgp, b4):
                    K.op("act", lambda q: q.activation(out=zs[:, :, cgp * 512:(cgp + 1) * 512], in_=ps[:, b4:b4 + 4, :], func=AF.Silu),
                         r=[PB[b4], PB[b4 + 1], PB[b4 + 2], PB[b4 + 3]], w=[SB["zs"]])
                mm_tm_wide(8, xnT, B_xnT, evac_z)
                wt, bw = wtile()
                b = bank()
                mm_tm(wt, bw, 8, xnT, B_xnT, b, ncols=20, cstride=32)
                K.op("act", lambda q, b=b: q.activation(out=dw[:], in_=ps[:, b, 0:128].rearrange("p (c t) -> p c t", c=4)[:, :, 0:20], func=AF.Copy), r=[PB[b]], w=[B_dw])
                wt, bw = wtile()
                b = bank()
                mm_tm(wt, bw, 8, xnT, B_xnT, b)
                K.op("act", lambda q, b=b, cg0=cg0: q.activation(out=Vh[:, cg0:cg0 + 4, :], in_=ps[:, b, :].rearrange("p (c t) -> p c t", c=4), func=AF.Copy), r=[PB[b]], w=[B_V])
                for i in range(4):
                    wt, bw = wtile()
                    b = bank()
                    mm_fm(wt, bw, 8, xnT, B_xnT, b)
                    K.op("act", lambda q, b=b, i=i: q.activation(out=qT[:, i, :], in_=ps[:, b, :], func=AF.Copy, scale=0.125), r=[PB[b]], w=[B_qT])
                wt, bw = wtile()
                b = bank()
                mm_fm(wt, bw, 8, xnT, B_xnT, b)
                K.op("act", lambda q, b=b, t0=t0: q.activation(out=kT[:, t0:t0 + S], in_=ps[:, b, :], func=AF.Copy), r=[PB[b]], w=[B_kT])
                for i in range(2):
                    wt, bw = wtile()
                    b = bank()
                    mm_fm(wt, bw, 8, xnT, B_xnT, b)
                    K.op("act", lambda q, b=b, i=i: q.activation(out=qiT[:, i, :], in_=ps[:, b, :], func=AF.Copy, scale=0.0625), r=[PB[b]], w=[B_qiT])
                wt, bw = wtile()
                b = bank()
                mm_fm(wt, bw, 8, xnT, B_xnT, b)
                K.op("act", lambda q, b=b, t0=t0: q.activation(out=kiT[:, t0:t0 + S], in_=ps[:, b, :], func=AF.Copy), r=[PB[b]], w=[B_kiT])

                for g in range(4):
                    win = POOL_WINDOWS[g]
                    wt, bw = wtile()
                    b = bank()
                    mm_fm(wt, bw, 8, xnT, B_xnT, b)
                    ub, bub = ubuf[g % 2], B_ubuf[g % 2]
                    K.op("act", lambda q, b=b, ub=ub: q.activation(out=ub[:, 15:527], in_=ps[:, b, :], func=AF.Copy), r=[PB[b]], w=[bub])
                    K.op("pool", lambda q, g=g, ub=ub: q.tensor_copy(out=ub[:, 0:15], in_=phalo[:, g, :]), r=[B_phalo], w=[bub])
                    srcb, bsrc = ub, bub
                    lvl = 1
                    pi = 0
                    while lvl < win:
                        dst, bdst = ptmp[pi], B_ptmp[pi]
                        lo = 2 * lvl - 1
                        K.op("dve", lambda q, srcb=srcb, dst=dst, lo=lo, lvl=lvl: q.tensor_tensor(out=dst[:, lo:527], in0=srcb[:, lo:527], in1=srcb[:, lo - lvl:527 - lvl], op=ALU.add),
                             r=[bsrc], w=[bdst])
                        srcb, bsrc = dst, bdst
                        pi ^= 1
                        lvl *= 2
                    pl, bpl = pooled[g % 2], B_pooled[g % 2]
                    if sc == 0:
                        K.op("dve", lambda q, srcb=srcb, g=g: q.tensor_tensor(out=srcb[:, 15:31], in0=srcb[:, 15:31], in1=rc16[:, g, :], op=ALU.mult), r=[bsrc, Bc], w=[bsrc])
                        K.op("dve", lambda q, srcb=srcb, ub=ub, pl=pl: q.tensor_tensor(out=pl[:, 0:16], in0=srcb[:, 15:31], in1=ub[:, 15:31], op=ALU.subtract), r=[bsrc, bub], w=[bpl])
                        K.op("dve", lambda q, srcb=srcb, ub=ub, pl=pl, win=win: q.scalar_tensor_tensor(out=pl[:, 16:512], in0=srcb[:, 31:527], scalar=1.0 / win, in1=ub[:, 31:527], op0=ALU.mult, op1=ALU.subtract),
                             r=[bsrc, bub], w=[bpl])
                    else:
                        K.op("dve", lambda q, srcb=srcb, ub=ub, pl=pl, win=win: q.scalar_tensor_tensor(out=pl[:, 0:512], in0=srcb[:, 15:527], scalar=1.0 / win, in1=ub[:, 15:527], op0=ALU.mult, op1=ALU.subtract),
                             r=[bsrc, bub], w=[bpl])
                    K.op("pool", lambda q, g=g, ub=ub: q.tensor_copy(out=phalo[:, g, :], in_=ub[:, 512:527]), r=[bub], w=[B_phalo])
                    b2 = bank()
                    K.op("pe", lambda q, b2=b2, g=g, pl=pl: q.matmul(ps[:, b2, :], lhsT=poolw[:, g, :], rhs=pl[:], start=True, stop=True), r=[B_junk, bpl], w=[PB[b2]])
                    K.op("act", lambda q, b2=b2, g=g: q.activation(out=ypoolT[:, g, :], in_=ps[:, b2, :], func=AF.Copy, scale=pp[:, 60 + g:61 + g]), r=[PB[b2], B_par], w=[B_ypoolT])

                if dg:
                    dump(f"zs{sc}", zs.rearrange("p c d -> p (c d)"), [SB["zs"]], 4096)
                    dump(f"dw{sc}", dw[:].rearrange("p c d -> p (c d)"), [B_dw], 80)
                    dump(f"ypoolT{sc}", ypoolT[:].rearrange("p k t -> p (k t)"), [B_ypoolT], 2048)
                    dump(f"qT{sc}", qT[:].rearrange("p k t -> p (k t)"), [B_qT], 2048)
                sm = small
                for c in range(4):
                    cc = slice(c * 128, (c + 1) * 128)
                    K.op("dve", lambda q, c=c: q.tensor_tensor(out=sm[:, 0:16], in0=dw[:, c, 0:16], in1=rows[:, 0:16], op=ALU.add), r=[B_dw, B_par], w=[B_sm["dt"]])
                    K.op("act", lambda q: q.activation(out=sm[:, 0:16], in_=sm[:, 0:16], func=AF.Exp), r=[B_sm["dt"]], w=[B_sm["dt"]])
                    K.op("act", lambda q: q.activation(out=sm[:, 0:16], in_=sm[:, 0:16], func=AF.Ln, bias=1.0), r=[B_sm["dt"]], w=[B_sm["dt"]])
                    K.op("pool", lambda q: q.memset(sm[:, 32:80], 0.0), w=[B_sm["a48"]])
                    K.op("dve", lambda q: q.tensor_tensor(out=sm[:, 32:48], in0=sm[:, 0:16], in1=A_b[:], op=ALU.mult), r=[B_sm["dt"], B_par], w=[B_sm["a48"]])
                    K.op("dve", lambda q: q.tensor_tensor(out=sm[:, 64:80], in0=sm[:, 0:16], in1=A_b[:], op=ALU.mult), r=[B_sm["dt"], B_par], w=[B_sm["a48"]])
                    bs = bank()
                    a16 = sm[:, 32:48]
                    K.op("pe", lambda q, bs=bs: q.matmul(ps[:, bs, 0:16], lhsT=tri_le, rhs=a16, start=True, stop=True), r=[Bc, B_sm["a48"]], w=[PB[bs]])
                    K.op("pe", lambda q, bs=bs: q.matmul(ps[:, bs, 16:32], lhsT=tri_gt, rhs=a16, start=True, stop=True), r=[Bc, B_sm["a48"]], w=[PB[bs]])
                    K.op("pe", lambda q, bs=bs: q.matmul(ps[:, bs, 32:48], lhsT=onesf, rhs=a16, start=True, stop=True), r=[Bc, B_sm["a48"]], w=[PB[bs]])
                    K.op("act", lambda q, bs=bs: q.activation(out=sm[:, 80:128], in_=ps[:, bs, 0:48], func=AF.Exp), r=[PB[bs]], w=[B_sm["e3"]])
                    bt = bank()
                    K.op("pe", lambda q, bt=bt: q.matmul(ps[0:48, bt, 0:128], lhsT=sm[:, 32:80], rhs=tri_le, start=True, stop=True), r=[Bc, B_sm["a48"]], w=[PB[bt]])
                    K.op("act", lambda q, bt=bt: q.activation(out=RHS[0:16, :], in_=ps[0:16, bt, 0:128], func=AF.Copy), r=[PB[bt]], w=[B_RHS])
                    K.op("dve", lambda q, bt=bt: q.tensor_tensor(out=LHS[32:48, :, :], in0=ps[32:48, bt, 0:128].unsqueeze(1).to_broadcast([16, 16, 128]),
                                                                  in1=oh[32:48, 16:32].unsqueeze(2).to_broadcast([16, 16, 128]), op=ALU.mult),
                         r=[PB[bt], Bc], w=[B_LHS])
                    xs3 = xs_tok[:, c, :].rearrange("p (h d) -> p h d", h=16)
                    K.op("dve", lambda q: q.tensor_tensor(out=sm[:, 128:144], in0=sm[:, 0:16], in1=sm[:, 96:112], op=ALU.mult), r=[B_sm["dt"], B_sm["e3"]], w=[B_sm["dtdec"]])
                    K.op("dve", lambda q, xs3=xs3: q.tensor_tensor(out=x_dt.rearrange("p (h d) -> p h d", h=16), in0=xs3, in1=sm[:, 0:16].unsqueeze(2).to_broadcast([128, 16, 64]), op=ALU.mult),
                         r=[SB["xs_tok"], B_sm["dt"]], w=[SB["x_dt"]])
                    K.op("dve", lambda q, xs3=xs3: q.tensor_tensor(out=xdec.rearrange("p (h d) -> p h d", h=16), in0=xs3, in1=sm[:, 128:144].unsqueeze(2).to_broadcast([128, 16, 64]), op=ALU.mult),
                         r=[SB["xs_tok"], B_sm["dtdec"]], w=[SB["xdec"]])
                    K.op("dve", lambda q, xs3=xs3: q.tensor_tensor(out=xsd.rearrange("p (h d) -> p h d", h=16), in0=xs3, in1=rows[:, 32:48].unsqueeze(2).to_broadcast([128, 16, 64]), op=ALU.mult),
                         r=[SB["xs_tok"], B_par], w=[SB["xsd"]])
                    by = bank2()
                    live = (by, by + 1)
                    for g in range(2):
                        Lexp, Mt, cb = LexpG[g], MtG[g], cbG[g]
                        bL, bM, bC = SB[f"Lexp{g}"], SB[f"Mt{g}"], SB[f"cb{g}"]
                        bcb = bank(live)
                        K.op("pe", lambda q, bcb=bcb, g=g, cc=cc: q.matmul(ps[:, bcb, 0:128], lhsT=xbc_act[:, 8 + g, cc], rhs=xbc_act[:, 10 + g, cc], start=True, stop=True),
                             r=[SB["xbc_act"]], w=[PB[bcb]])
                        K.op("act", lambda q, bcb=bcb, cb=cb: q.activation(out=cb, in_=ps[:, bcb, 0:128], func=AF.Copy), r=[PB[bcb]], w=[bC])
                        be = bank2(live)
                        for j in range(8):
                            h = g * 8 + j
                            bb_, off = be + j // 4, (j % 4) * 128
                            K.op("pe", lambda q, bb_=bb_, off=off, h=h: q.matmul(ps[:, bb_, off:off + 128], lhsT=LHS[:, h, :], rhs=RHS[:], start=True, stop=False),
                                 r=[B_LHS, B_RHS], w=[PB[bb_]])
                            K.op("pe", lambda q, bb_=bb_, off=off: q.matmul(ps[:, bb_, off:off + 128], lhsT=identf, rhs=maskneg, start=False, stop=True),
                                 r=[Bc], w=[PB[bb_]])
                        K.op("act", lambda q, be=be, Lexp=Lexp: q.activation(out=Lexp, in_=ps[:, be:be + 2, :].rearrange("p b (j t) -> p (b j) t", j=4), func=AF.Exp),
                             r=[PB[be], PB[be + 1]], w=[bL])
                        K.op("dve", lambda q, Mt=Mt, Lexp=Lexp, cb=cb: q.tensor_tensor(out=Mt, in0=Lexp, in1=cb.unsqueeze(1).to_broadcast([128, 8, 128]), op=ALU.mult),
                             r=[bL, bC], w=[bM])
                        for j in range(8):
                            h = g * 8 + j
                            hb_, hoff = h // 8, (h % 8) * 64
                            K.op("pe", lambda q, j=j, hb_=hb_, hoff=hoff, h=h, by=by, Mt=Mt: q.matmul(ps[:, by + hb_, hoff:hoff + 64], lhsT=Mt[:, j, :], rhs=x_dt[:, h * 64:(h + 1) * 64], start=True, stop=False),
                                 r=[bM, SB["x_dt"]], w=[PB[by + hb_]])
                            K.op("pe", lambda q, hb_=hb_, hoff=hoff, h=h, by=by: q.matmul(ps[:, by + hb_, hoff:hoff + 64], lhsT=identb[:], rhs=xsd[:, h * 64:(h + 1) * 64], start=False, stop=True),
                                 r=[Bc, SB["xsd"]], w=[PB[by + hb_]])
                    bo = bank2(live)
                    for h in range(16):
                        g = h // 8
                        hb_, hoff = h // 8, (h % 8) * 64
                        K.op("pe", lambda q, hb_=hb_, hoff=hoff, h=h, g=g, cc=cc, bo=bo: q.matmul(ps[:, bo + hb_, hoff:hoff + 64], lhsT=xbc_act[:, 10 + g, cc], rhs=state_bf[:, h, :], start=True, stop=True),
                             r=[SB["xbc_act"], B_statebf], w=[PB[bo + hb_]])
                    bsn = bank2(live + (bo, bo + 1))
                    for h in range(16):
                        g = h // 8
                        hb_, hoff = h // 8, (h % 8) * 64
                        K.op("pe", lambda q, hb_=hb_, hoff=hoff, h=h, g=g, c=c, bsn=bsn: q.matmul(ps[:, bsn + hb_, hoff:hoff + 64], lhsT=B_tok[:, c, g * 128:(g + 1) * 128], rhs=xdec[:, h * 64:(h + 1) * 64], start=True, stop=True),
                             r=[SB["B_tok"], SB["xdec"]], w=[PB[bsn + hb_]])
                    cd_b = sm[:, 112:128].unsqueeze(2).to_broadcast([128, 16, 64])
                    K.op("dve", lambda q, cd_b=cd_b: q.tensor_tensor(out=state[:], in0=state[:], in1=cd_b, op=ALU.mult), r=[B_state, B_sm["e3"]], w=[B_state])
                    K.op("dve", lambda q, bsn=bsn: q.tensor_tensor(out=state[:].rearrange("p (b h) d -> p b (h d)", b=2), in0=ps[:, bsn:bsn + 2, :], in1=state[:].rearrange("p (b h) d -> p b (h d)", b=2), op=ALU.add),
                         r=[PB[bsn], PB[bsn + 1], B_state], w=[B_state])
                    K.op("pool", lambda q: q.tensor_copy(out=state_bf[:], in_=state[:]), r=[B_state], w=[B_statebf])
                    ea_b = sm[:, 80:96].unsqueeze(2).to_broadcast([128, 16, 64])
                    K.op("dve", lambda q, bo=bo, ea_b=ea_b: q.tensor_tensor(out=yacc.rearrange("p (h d) -> p h d", h=16), in0=ps[:, bo:bo + 2, :].rearrange("p b (h d) -> p (b h) d", h=8), in1=ea_b, op=ALU.mult),
                         r=[PB[bo], PB[bo + 1], B_sm["e3"]], w=[SB["yacc"]])
                    K.op("dve", lambda q, by=by: q.tensor_tensor(out=yacc.rearrange("p (b f) -> p b f", b=2), in0=ps[:, by:by + 2, :], in1=yacc.rearrange("p (b f) -> p b f", b=2), op=ALU.add),
                         r=[PB[by], PB[by + 1], SB["yacc"]], w=[SB["yacc"]])
                    if dg:
                        dump(f"y{sc}_{c}", yacc, [SB["yacc"]], 1024)
                        dump(f"sm{sc}_{c}", sm[:, 0:128], [B_sm["dt"], B_sm["e3"], B_sm["a48"]], 128)
                    K.op("dve", lambda q, c=c: q.tensor_tensor(out=yacc, in0=yacc, in1=zs[:, c, :], op=ALU.mult), r=[SB["yacc"], SB["zs"]], w=[SB["yacc"]])
                    K.op("act", lambda q: q.activation(out=junk[:], in_=yacc, func=AF.Square, accum_out=sm[:, 169:170]), r=[SB["yacc"]], w=[B_junk, B_sm["rs"]])
                    K.op("act", lambda q: q.activation(out=sm[:, 170:171], in_=sm[:, 169:170], func=AF.Sqrt, scale=1.0 / D, bias=EPS), r=[B_sm["rs"]], w=[B_sm["rs"]])
                    K.op("dve", lambda q: q.reciprocal(out=sm[:, 170:171], in_=sm[:, 170:171]), r=[B_sm["rs"]], w=[B_sm["rs"]])
                    K.op("dve", lambda q: q.tensor_scalar(out=ynb, in0=yacc, scalar1=sm[:, 170:171], scalar2=None, op0=ALU.mult), r=[SB["yacc"], B_sm["rs"]], w=[SB["ynb"]])
                    for half in range(2):
                        b = bank()
                        for j in range(4):
                            kt = half * 4 + j
                            K.op("pe", lambda q, b=b, j=j, kt=kt: q.transpose(out=psb(b)[:, j * 128:(j + 1) * 128], in_=ynb[:, kt * 128:(kt + 1) * 128], identity=identb[:]),
                                 r=[SB["ynb"], Bc], w=[PB[b]])
                        K.op("dve", lambda q, b=b, half=half, cc=cc: q.tensor_tensor(
                            out=ynT[:, half * 4:half * 4 + 4, cc], in0=psb(b)[:, 0:512].rearrange("p (j t) -> p j t", j=4),
                            in1=pp[:, 72 + half * 4:72 + half * 4 + 4].unsqueeze(2).to_broadcast([128, 4, 128]), op=ALU.mult),
                            r=[PB[b], B_par], w=[B_ynT])
                AB = att_bufs
                K.handoff(list(SB.values()), list(AB.values()))

                def idx_topk(c):
                    cg = cg0 + c
                    nk = (cg + 1) * 128
                    nb = (nk + S - 1) // S
                    cc = slice(c * 128, (c + 1) * 128)
                    for kb in range(nb):
                        w_ = min(S, nk - kb * S)
                        ks = slice(kb * S, kb * S + w_)
                        for h in range(4):
                            b = bank()
                            half, ti = h // 2, h % 2
                            prt = slice(half * 64, half * 64 + 64)
                            K.op("pe", lambda q, b=b, prt=prt, ti=ti, ks=ks, w_=w_, cc=cc: q.matmul(ps[:, b, 0:w_], lhsT=qiT[prt, ti, cc], rhs=kiT[prt, ks], start=True, stop=True),
                                 r=[B_qiT, B_kiT], w=[PB[b]])
                            rt, brt = rtmp[h % 2], AB[f"rtmp{h % 2}"]
                            K.op("act", lambda q, b=b, rt=rt, w_=w_: q.activation(out=rt[:, 0:w_], in_=ps[:, b, 0:w_], func=AF.Relu), r=[PB[b]], w=[brt])
                            if h == 0:
                                K.op("dve", lambda q, rt=rt, ks=ks, w_=w_, c=c: q.tensor_scalar(out=score[:, ks], in0=rt[:, 0:w_], scalar1=dw[:, c, 16:17], scalar2=None, op0=ALU.mult),
                                     r=[brt, B_dw], w=[AB["score"]])
                            else:
                                K.op("dve", lambda q, rt=rt, ks=ks, w_=w_, c=c, h=h: q.scalar_tensor_tensor(out=score[:, ks], in0=rt[:, 0:w_], scalar=dw[:, c, 16 + h:17 + h], in1=score[:, ks], op0=ALU.mult, op1=ALU.add),
                                     r=[brt, B_dw, AB["score"]], w=[AB["score"]])
                    K.op("dve", lambda q, nk=nk: q.tensor_tensor(out=score[:, nk - 128:nk], in0=score[:, nk - 128:nk], in1=cmaskneg, op=ALU.add), r=[AB["score"], Bc], w=[AB["score"]])
                    if cg >= 2:
                        NIT = 26
                        nlo = 256
                        K.op("dve", lambda q, nk=nk: q.max(out=sm[:, 144:152], in_=score[:, 0:nk]), r=[AB["score"]], w=[B_sm["m8"]])
                        K.op("dve", lambda q, nlo=nlo: q.tensor_reduce(out=bis[:, 59:60], in_=score[:, 0:nlo], axis=AX.X, op=ALU.min), r=[AB["score"]], w=[B_bis["lo"]])
                        K.op("dve", lambda q: q.tensor_tensor(out=bis[:, 60:61], in0=sm[:, 144:145], in1=bis[:, 59:60], op=ALU.subtract), r=[B_sm["m8"], B_bis["lo"]], w=[B_bis["d0"]])
                        K.op("dve", lambda q: q.tensor_scalar(out=bis[:, 0:56], in0=cst[:, 7, 0:56], scalar1=bis[:, 60:61], scalar2=None, op0=ALU.mult), r=[Bc, B_bis["d0"]], w=[B_bis["D"]])
                        K.op("dve", lambda q: q.tensor_tensor(out=bis[:, 58:59], in0=bis[:, 59:60], in1=bis[:, 0:1], op=ALU.add), r=[B_bis["lo"], B_bis["D"]], w=[B_bis["mid"]])
                        for it in range(NIT):
                            K.op("dve", lambda q, nk=nk: q.tensor_scalar(out=work[:, 0:nk], in0=score[:, 0:nk], scalar1=bis[:, 58:59], scalar2=None, op0=ALU.is_ge, op1=ALU.add, accum_out=bis[:, 56:57]),
                                 r=[AB["score"], B_bis["mid"]], w=[AB["work"], B_bis["cnt"]])
                            K.op("dve", lambda q, it=it: q.scalar_tensor_tensor(out=bis[:, 57:58], in0=bis[:, 56:57], scalar=255.5, in1=bis[:, it:it + 1], op0=ALU.is_ge, op1=ALU.mult),
                                 r=[B_bis["cnt"], B_bis["D"]], w=[B_bis["u"]])
                            K.op("dve", lambda q, it=it: q.scalar_tensor_tensor(out=bis[:, 58:59], in0=bis[:, 57:58], scalar=bis[:, 28 + it + 1:28 + it + 2], in1=bis[:, 58:59], op0=ALU.add, op1=ALU.add),
                                 r=[B_bis["u"], B_bis["D"], B_bis["mid"]], w=[B_bis["mid"]])
                        K.op("dve", lambda q: q.tensor_tensor(out=sm[:, 152:153], in0=bis[:, 58:59], in1=bis[:, 28 + NIT:28 + NIT + 1], op=ALU.add), r=[B_bis["mid"], B_bis["D"]], w=[B_sm["thr"]])
                    else:
                        K.op("dve", lambda q: q.memset(sm[:, 152:153], -1.0e29), w=[B_sm["thr"]])
                    mb = mbias[c % 2]
                    K.op("dve", lambda q, mb=mb, nk=nk: q.tensor_scalar(out=mb[:, 0:nk], in0=score[:, 0:nk], scalar1=sm[:, 152:153], scalar2=1.0, op0=ALU.is_ge, op1=ALU.subtract),
                         r=[AB["score"], B_sm["thr"]], w=[AB[f"mb{c % 2}"]])

                def heads(c):
                    cg = cg0 + c
                    nk = (cg + 1) * 128
                    nb = (nk + S - 1) // S
                    cc = slice(c * 128, (c + 1) * 128)
                    mb, bmb = mbias[c % 2], AB[f"mb{c % 2}"]
                    bO = bank()
                    K.op("pool", lambda q: q.memset(rsb[:], 0.0), w=B_rsb)
                    items = [(h, kb) for h in range(8) for kb in range(nb)]

                    def stA(i):
                        h, kb = items[i]
                        g, ti = h // 4, h % 4
                        prt = slice(g * 64, g * 64 + 64)
                        w_ = min(S, nk - kb * S)
                        ks = slice(kb * S, kb * S + w_)
                        b = bank((bO,))
                        i3 = i % 3
                        K.op("pe", lambda q: q.matmul(ps[:, b, 0:w_], lhsT=qT[prt, ti, cc], rhs=kT[prt, ks], start=True, stop=False),
                             r=[B_qT, B_kT], w=[PB[b]])
                        K.op("pe", lambda q: q.matmul(ps[:, b, 0:w_], lhsT=identBIG[:], rhs=mb[:, ks], start=False, stop=True),
                             r=[Bc, bmb], w=[PB[b]])
                        K.op("act", lambda q: q.activation(out=Pm[i3][:, 0:w_], in_=ps[:, b, 0:w_], func=AF.Exp, accum_out=rsb[:, h, kb:kb + 1]),
                             r=[PB[b]], w=[AB[f"Pm{i3}"], B_rsb[i % 8]])

                    def stB(i):
                        h, kb = items[i]
                        g = h // 4
                        w_ = min(S, nk - kb * S)
                        nsub = w_ // 128
                        i3 = i % 3
                        b2 = bank((bO,))
                        for ii in range(nsub):
                            K.op("pe", lambda q, ii=ii: q.transpose(out=psb(b2)[:, ii * 128:(ii + 1) * 128], in_=Pm[i3][:, ii * 128:(ii + 1) * 128], identity=identb[:]),
                                 r=[AB[f"Pm{i3}"], Bc], w=[PB[b2]])
                        K.op("act", lambda q: q.activation(out=PmT[i3][:, 0:w_], in_=psb(b2)[:, 0:w_], func=AF.Copy), r=[PB[b2]], w=[AB[f"PmT{i3}"]])
                        for ii in range(nsub):
                            kc = kb * 4 + ii
                            K.op("pe", lambda q, ii=ii, kc=kc, first=(kb == 0 and ii == 0), last=(kb == nb - 1 and ii == nsub - 1):
                                 q.matmul(ps[:, bO, h * 64:(h + 1) * 64], lhsT=PmT[i3][:, ii * 128:(ii + 1) * 128], rhs=Vh[:, kc, g * 64:(g + 1) * 64], start=first, stop=last),
                                 r=[AB[f"PmT{i3}"], B_V], w=[PB[bO]])

                    stA(0)
                    for i in range(len(items)):
                        if i + 1 < len(items):
                            stA(i + 1)
                        stB(i)
                    K.op("dve", lambda q: q.tensor_reduce(out=sm[:, 153:161], in_=rsb[:], axis=AX.X, op=ALU.add), r=B_rsb, w=[B_sm["rinv"]])
                    K.op("dve", lambda q: q.reciprocal(out=sm[:, 161:169], in_=sm[:, 153:161]), r=[B_sm["rinv"]], w=[B_sm["rinv"]])
                    K.op("dve", lambda q: q.tensor_tensor(out=yatt.rearrange("p (h d) -> p h d", h=8), in0=ps[:, bO, :].rearrange("p (h d) -> p h d", h=8),
                                                          in1=sm[:, 161:169].unsqueeze(2).to_broadcast([128, 8, 64]), op=ALU.mult),
                         r=[PB[bO], B_sm["rinv"]], w=[AB["yatt"]])
                    b = bank()
                    for j in range(4):
                        K.op("pe", lambda q, j=j: q.transpose(out=psb(b)[:, j * 128:(j + 1) * 128], in_=yatt[:, j * 128:(j + 1) * 128], identity=identb[:]), r=[AB["yatt"], Bc], w=[PB[b]])
                    K.op("act", lambda q: q.activation(out=yattT[:, :, cc], in_=psb(b)[:, 0:512].rearrange("p (j t) -> p j t", j=4), func=AF.Copy), r=[PB[b]], w=[B_yattT])

                idx_topk(0)
                for c in range(4):
                    if c + 1 < 4:
                        idx_topk(c + 1)
                    heads(c)

                if dg:
                    dump(f"ynT{sc}", ynT[:].rearrange("p k t -> p (k t)"), [B_ynT], 4096)
                    dump(f"yattT{sc}", yattT[:].rearrange("p k t -> p (k t)"), [B_yattT], 2048)
                if sc + 1 < NSC:
                    load_h(l, sc + 1)
                elif l + 1 < L:
                    load_h(l + 1, 0)
                MB = mrg_bufs
                K.handoff(list(AB.values()), list(MB.values()))
                for f in range(8):
                    w1, bw1 = wtile()
                    b1 = bank()
                    mm_fm(w1, bw1, 8, ynT, B_ynT, b1)
                    w2, bw2 = wtile()
                    b2 = bank()
                    mm_fm(w2, bw2, 4, yattT, B_yattT, b2)
                    b3 = bank()
                    mm_fm(w2, bw2, 4, ypoolT, B_ypoolT, b3, wkt0=4)
                    ups = [b1, b2, b3]
                    for br in range(3):
                        wg, bwg = wtile()
                        bg = bank()
                        mm_fm(wg, bwg, 8, xnT, B_xnT, bg)
                        K.op("act", lambda q, bg=bg, br=br: q.activation(out=sig[br], in_=ps[:, bg, :], func=AF.Sigmoid), r=[PB[bg]], w=[MB[f"sig{br}"]])
                        K.op("dve", lambda q, br=br, bu_=ups[br]: q.tensor_tensor(out=mt[br], in0=ps[:, bu_, :], in1=sig[br], op=ALU.mult), r=[PB[ups[br]], MB[f"sig{br}"]], w=[MB[f"mt{br}"]])
                    K.op("pool", lambda q: q.tensor_tensor(out=mt[0], in0=mt[0], in1=mt[1], op=ALU.add), r=[MB["mt0"], MB["mt1"]], w=[MB["mt0"]])
                    K.op("pool", lambda q, f=f: q.tensor_tensor(out=mergedT[:, f, :], in0=mt[0], in1=mt[2], op=ALU.add), r=[MB["mt0"], MB["mt2"]], w=[MB["mergedT"]])
                def evac_res(cgp, b4):
                    K.op("dve", lambda q: q.tensor_tensor(out=hS[:, :, cgp * 512:(cgp + 1) * 512], in0=ps[:, b4:b4 + 4, :], in1=hS[:, :, cgp * 512:(cgp + 1) * 512], op=ALU.add),
                         r=[PB[b4], PB[b4 + 1], PB[b4 + 2], PB[b4 + 3], B_hS], w=[B_hS])
                mm_tm_wide(8, mergedT, MB["mergedT"], evac_res)

                if dg:
                    dump(f"mergedT{sc}", mergedT.rearrange("p k t -> p (k t)"), [MB["mergedT"]], 4096)
                    dump(f"h1_{sc}", hS[:].rearrange("p c d -> p (c d)"), [B_hS], 4096)
                FB = ffn_bufs
                rms_T(80, xnT, B_xnT, hS, B_hS)
                K.handoff(list(MB.values()), list(FB.values()))
                for j in range(22):
                    wa, bwa = wtile()
                    ba = bank()
                    mm_fm(wa, bwa, 8, xnT, B_xnT, ba)
                    K.op("act", lambda q, ba=ba, j=j: q.activation(out=sa[j % 2], in_=ps[:, ba, :], func=AF.Silu), r=[PB[ba]], w=[FB[f"sa{j % 2}"]])
                    wb_, bwb = wtile()
                    bb = bank()
                    mm_fm(wb_, bwb, 8, xnT, B_xnT, bb)
                    K.op("dve", lambda q, bb=bb, j=j: q.tensor_tensor(out=hidT[:, j, :], in0=ps[:, bb, :], in1=sa[j % 2], op=ALU.mult), r=[PB[bb], FB[f"sa{j % 2}"]], w=[FB["hidT"]])
                mm_tm_wide(22, hidT, FB["hidT"], evac_res)
                assert cur["slot"] == NSLOT, cur["slot"]

                if last_layer:
                    for c in range(4):
                        K.op("act", lambda q, c=c: q.activation(out=junk[:], in_=hS[:, c, :], func=AF.Square, accum_out=ssq[:, c:c + 1]), r=[B_hS], w=[B_junk, B_ssq])
                    K.op("act", lambda q: q.activation(out=ssq[:, 4:8], in_=ssq[:, 0:4], func=AF.Sqrt, scale=1.0 / D, bias=EPS), r=[B_ssq], w=[B_ssq])
                    K.op("dve", lambda q: q.reciprocal(out=ssq[:, 4:8], in_=ssq[:, 4:8]), r=[B_ssq], w=[B_ssq])
                    for c in range(4):
                        K.op("dve", lambda q, c=c: q.scalar_tensor_tensor(out=hS[:, c, :], in0=hS[:, c, :], scalar=ssq[:, 4 + c:5 + c], in1=fnw[:], op0=ALU.mult, op1=ALU.mult),
                             r=[B_hS, B_ssq, Bc], w=[B_hS])
                    K.dma("act", y_d[t0:t0 + S, :].rearrange("(c p) d -> p c d", p=128), hS[:], r=[B_hS], w=[B_out])
                else:
                    K.dma("act", hb_d[t0:t0 + S, :].rearrange("(c p) d -> p c d", p=128), hS[:], r=[B_hS], w=[B_hb[sc]])
                    pass
            for sc in range(NSC):
                n_ = l * NSC + sc
                superchunk(sc, hSb[n_ % 2], B_hSb[n_ % 2])
        K.final_wait("act", [B_out])
        K.final_wait("sp", [B_out])
        K.emit()
    return nc, K


B_out = Buf()
B_hb = [Buf() for _ in range(64)]


def _tile_k(wcols, kt_total=8):
    Kd, ncol = wcols.shape
    nkt = Kd // 128
    out = np.zeros((128, kt_total, 128), np.float32)
    out[:, :nkt, :ncol] = wcols.reshape(nkt, 128, ncol).transpose(1, 0, 2)
    return out


def _tile_wide(w):
    Kd = w.shape[0]
    nkt = Kd // 128
    out = []
    for cgp in range(2):
        t = w[:, cgp * 512:(cgp + 1) * 512].reshape(nkt, 128, 512).transpose(1, 0, 2)
        for si in range((nkt + 1) // 2):
            s_ = np.zeros((128, 2, 512), np.float32)
            n = min(2, nkt - si * 2)
            s_[:, 0:n, :] = t[:, si * 2:si * 2 + n, :]
            out.append(s_.reshape(128, 8, 128))
    return out


def pack_layer(inp, l):
    w_in = inp["w_in"][l]
    slots = []
    pw = np.zeros((128, 8, 128), np.float32)
    for g in range(4):
        pw[:, g, :] = inp["pool_w"][l, g]
    slots.append(pw)
    for j in range(12):
        slots.append(_tile_k(w_in[:, 1024 + 128 * j:1024 + 128 * (j + 1)]))
    slots.extend(_tile_wide(w_in[:, 0:1024]))
    slots.append(_tile_k(np.concatenate([w_in[:, 2560:2576], w_in[:, 3664:3668]], axis=1)))
    slots.append(_tile_k(w_in[:, 3216:3344]))
    for i in range(4):
        slots.append(_tile_k(np.concatenate([w_in[:, 2576 + 64 * i:2576 + 64 * (i + 1)], w_in[:, 2576 + 64 * (4 + i):2576 + 64 * (5 + i)]], axis=1)))
    slots.append(_tile_k(w_in[:, 3088:3216]))
    for i in range(2):
        slots.append(_tile_k(np.concatenate([w_in[:, 3344 + 64 * i:3344 + 64 * (i + 1)], w_in[:, 3344 + 64 * (2 + i):3344 + 64 * (3 + i)]], axis=1)))
    slots.append(_tile_k(np.concatenate([w_in[:, 3600:3664], w_in[:, 3600:3664]], axis=1)))
    for g in range(4):
        slots.append(_tile_k(w_in[:, 3668 + 128 * g:3668 + 128 * (g + 1)]))
    for f in range(8):
        cs = slice(128 * f, 128 * (f + 1))
        slots.append(_tile_k(inp["w_up_ssd"][l][:, cs]))
        ap = np.zeros((128, 8, 128), np.float32)
        ap[:, 0:4, :] = inp["w_up_attn"][l][:, cs].reshape(4, 128, 128).transpose(1, 0, 2)
        ap[:, 4:8, :] = inp["w_up_pool"][l][:, cs].reshape(4, 128, 128).transpose(1, 0, 2)
        slots.append(ap)
        for br in range(3):
            c0 = 4180 + (br * 8 + f) * 128
            slots.append(_tile_k(w_in[:, c0:c0 + 128]))
    slots.extend(_tile_wide(inp["w_out"][l]))
    wfi = inp["w_ffn_in"][l]
    for j in range(22):
        slots.append(_tile_k(wfi[:, 128 * j:128 * (j + 1)]))
        slots.append(_tile_k(wfi[:, 2816 + 128 * j:2816 + 128 * (j + 1)]))
    slots.extend(_tile_wide(inp["w_ffn_out"][l]))
    assert len(slots) == NSLOT, len(slots)
    W = np.stack(slots).reshape(NSLOT, 128, 1024)
    pp = np.zeros((128, NPP), np.float32)
    cw = inp["conv_w"][l]
    pp[:, 0:48] = cw.reshape(4, 12, 128).transpose(2, 1, 0).reshape(128, 48)
    pp[:, 48:60] = inp["conv_b"][l].reshape(12, 128).T
    pp[:, 60:64] = inp["pool_scale"][l].reshape(4, 128).T
    pp[:, 64:72] = inp["norm1_w"][l].reshape(8, 128).T
    pp[:, 72:80] = inp["ssd_norm_w"][l].reshape(8, 128).T
    pp[:, 80:88] = inp["norm2_w"][l].reshape(8, 128).T
    rows = np.concatenate([inp["dt_bias"][l], inp["a_log"][l], inp["d_skip"][l]])[None, :].repeat(128, 0).astype(np.float32)
    return W, pp, rows


def make_consts():
    k = np.arange(128)
    cst = np.zeros((128, 8, 128), np.float32)
    cst[:, 0, :] = np.eye(128)
    cst[:, 1, :] = (k[:, None] <= k[None, :])
    cst[:, 2, :] = (k[:, None] > k[None, :])
    cst[:, 3, :] = 1.0
    cst[:, 4, :] = np.where(k[None, :] >= k[:, None], 0.0, -30000.0)
    cst[:, 5, :] = np.where(k[None, :] <= k[:, None], 0.0, NEG)
    oh = np.zeros((128, 128), np.float32)
    oh[0:16, 0:16] = np.eye(16)
    oh[32:48, 16:32] = -np.eye(16)
    cst[:, 6, :] = oh
    p2 = 2.0 ** -(np.arange(28) + 1.0)
    cst[:, 7, 0:28] = p2[None, :]
    cst[:, 7, 28:56] = -p2[None, :]
    rc16 = np.zeros((128, 4, 16), np.float32)
    for g, w in enumerate(POOL_WINDOWS):
        rc16[:, g, :] = 1.0 / np.minimum(np.arange(1, 17), w)
    return cst, rc16


_CACHE = {}


def run(inputs, T=SEQ, L=DEPTH, ncores=NCORES, dbg=False):
    inp = {k: np.asarray(v, np.float32) for k, v in inputs.items()}
    key = (T, L, dbg)
    if key not in _CACHE:
        _CACHE[key] = build(T, L, dbg)
    nc, _ = _CACHE[key]
    Ws, pps, rws = [], [], []
    for l in range(L):
        W, pp, rows = pack_layer(inp, l)
        Ws.append(W)
        pps.append(pp)
        rws.append(rows)
    W = np.stack(Ws)
    pp = np.stack(pps)
    rows = np.stack(rws)
    cst, rc16 = make_consts()
    fnw = np.ascontiguousarray(np.broadcast_to(inp["final_norm_w"][None, :], (128, D))).astype(np.float32)
    in_maps = []
    for b in range(ncores):
        in_maps.append({"x": np.ascontiguousarray(inp["x"][b, :T]), "W": W, "pp": pp, "rows": rows, "fnw": fnw, "cst": cst, "rc16": rc16})
    res = run_bass_kernel_spmd(nc, in_maps, core_ids=list(range(ncores)))
    if dbg:
        return res.results
    return np.stack([np.asarray(r["y"], np.float32) for r in res.results])


def kernel(**inputs):
    return run(inputs)
```
